# Optimizing a Trainium2 kernel written in Bass

```python
import math
import jax, jax.numpy as jnp
from jax import lax
import numpy as np

D_MODEL = 1024
BATCH = 4
SEQ = 4096
DEPTH = 4
DEC_BATCH = 128
DEC_SEQ = 4
PAST_LEN = 2048
PAGE_SIZE = 128

N_A_LAYERS = DEPTH // 2
N_B_LAYERS = DEPTH - N_A_LAYERS
EPS = 1e-6
CHUNK = 128
A_WIDTH = 2 * D_MODEL
A_GROUPS = 8
A_GROUP_DIM = A_WIDTH // A_GROUPS
N_HEADS = 16
HEAD_DIM = D_MODEL // N_HEADS
N_KV_HEADS = 4
GQA = N_HEADS // N_KV_HEADS
MOBA_BLOCK = 256
MOBA_TOPK = 3
Q_BLOCK = 64
ROPE_DIM = HEAD_DIM // 4
ROPE_THETA = 500000.0
PEER_HEADS = 8
PEER_N_KEYS = 128
PEER_EXPERTS = PEER_N_KEYS * PEER_N_KEYS
PEER_KEY_DIM = 256
PEER_TOPK = 16
PEER_TOK_BLOCK = 128

kernel_name = 'yoco_gmlp_moba_peer_step'

F32 = jnp.float32


def _rms(x, g):
    xf = x.astype(F32)
    y = xf * lax.rsqrt(jnp.mean(xf * xf, axis=-1, keepdims=True) + EPS)
    return (y * g.astype(F32)).astype(x.dtype)


def _ada(c, w, b):
    m = jax.nn.silu(c) @ w + b
    return jnp.split(m[:, None, :], 6, axis=-1)


def _modnorm(x, g, shift, scale):
    return _rms(x, g) * (1 + scale) + shift


def _rope(x, pos):
    half = ROPE_DIM // 2
    inv = ROPE_THETA ** (-jnp.arange(half, dtype=F32) / half)
    ang = pos.astype(F32)[:, None] * inv[None, :]
    cos = jnp.cos(ang)[:, None, :]
    sin = jnp.sin(ang)[:, None, :]
    xr = x[..., :ROPE_DIM].astype(F32)
    x1, x2 = xr[..., :half], xr[..., half:]
    rot = jnp.concatenate([x1 * cos - x2 * sin, x2 * cos + x1 * sin], axis=-1).astype(x.dtype)
    return jnp.concatenate([rot, x[..., ROPE_DIM:]], axis=-1)


def _chunk_gmlp(h, w_in, b_in, g_sgu, w_s, b_s, w_out):
    bsz, s, _ = h.shape
    z = jax.nn.gelu(h @ w_in + b_in)
    u, v = jnp.split(z, 2, axis=-1)
    v = _rms(v, g_sgu)
    L = min(s, CHUNK)
    nc = s // L
    vg = v.reshape(bsz, nc, L, A_GROUPS, A_GROUP_DIM)
    ws = w_s[:, :L, :L] * jnp.tril(jnp.ones((L, L), w_s.dtype))
    mix = jnp.einsum('gts,bcsgd->bctgd', ws, vg) + b_s[:, :L].T[None, None, :, :, None]
    y = u * mix.reshape(bsz, s, A_WIDTH)
    return y @ w_out, v


def _peer(h, w_pq, sub_keys, peer_u, peer_v):
    shp = h.shape
    x = h.reshape(-1, D_MODEL)
    n = x.shape[0]
    nblk = -(-n // PEER_TOK_BLOCK)
    xp = jnp.pad(x, ((0, nblk * PEER_TOK_BLOCK - n), (0, 0))).reshape(nblk, PEER_TOK_BLOCK, D_MODEL)
    kk = PEER_TOPK

    def step(xb):
        q = (xb @ w_pq).reshape(PEER_TOK_BLOCK, PEER_HEADS, 2, PEER_KEY_DIM // 2)
        s = jnp.einsum('thpd,pkd->thpk', q, sub_keys).astype(F32)
        sv, si = lax.top_k(s, kk)
        cand = sv[:, :, 0, :, None] + sv[:, :, 1, None, :]
        cidx = si[:, :, 0, :, None] * PEER_N_KEYS + si[:, :, 1, None, :]
        fv, fi = lax.top_k(cand.reshape(PEER_TOK_BLOCK, PEER_HEADS, kk * kk), kk)
        e = jnp.take_along_axis(cidx.reshape(PEER_TOK_BLOCK, PEER_HEADS, kk * kk), fi, axis=-1)
        g = jax.nn.softmax(fv, axis=-1)
        a = jax.nn.gelu(jnp.einsum('thkd,td->thk', peer_u[e], xb))
        w = (g * a.astype(F32)).astype(xb.dtype)
        return jnp.einsum('thk,thkd->td', w, peer_v[e])

    y = lax.map(step, xp).reshape(nblk * PEER_TOK_BLOCK, D_MODEL)[:n]
    return y.reshape(shp)


def _shared_kv(x, pos, kv_norm_g, w_kv, k_norm_g):
    bsz, s, _ = x.shape
    kv = _rms(x, kv_norm_g) @ w_kv
    k, v = jnp.split(kv, 2, axis=-1)
    k = k.reshape(bsz, s, N_KV_HEADS, HEAD_DIM)
    v = v.reshape(bsz, s, N_KV_HEADS, HEAD_DIM)
    k = _rope(_rms(k, k_norm_g), pos)
    return k, v


def _queries(h, pos, w_q, q_norm_g):
    bsz, s, _ = h.shape
    q = (h @ w_q).reshape(bsz, s, N_HEADS, HEAD_DIM)
    q = _rope(_rms(q, q_norm_g), pos)
    return q.reshape(bsz, s, N_KV_HEADS, GQA, HEAD_DIM)


def _moba_core(q, k_own, v_own, own_mask, k_sel, v_sel, sel_valid):
    scale = HEAD_DIM ** -0.5
    s_own = jnp.einsum('qkgd,lkd->qkgl', q, k_own).astype(F32) * scale
    s_own = jnp.where(own_mask[:, None, None, :], s_own, -jnp.inf)
    if k_sel is None:
        p = jax.nn.softmax(s_own, axis=-1).astype(v_own.dtype)
        return jnp.einsum('qkgl,lkd->qkgd', p, v_own)
    s_sel = jnp.einsum('qkgd,qkgnd->qkgn', q, k_sel).astype(F32) * scale
    if sel_valid is not None:
        s_sel = jnp.where(sel_valid, s_sel, -jnp.inf)
    L = k_own.shape[0]
    p = jax.nn.softmax(jnp.concatenate([s_own, s_sel], axis=-1), axis=-1).astype(v_own.dtype)
    return (jnp.einsum('qkgl,lkd->qkgd', p[..., :L], v_own)
            + jnp.einsum('qkgn,qkgnd->qkgd', p[..., L:], v_sel))


def _prompt_shared(x, pos, kv_norm_g, w_kv, k_norm_g):
    k, v = _shared_kv(x, pos, kv_norm_g, w_kv, k_norm_g)
    bsz, s = x.shape[:2]
    nb = -(-s // MOBA_BLOCK)
    pad = nb * MOBA_BLOCK - s
    k_pad = jnp.pad(k, ((0, 0), (0, pad), (0, 0), (0, 0)))
    v_pad = jnp.pad(v, ((0, 0), (0, pad), (0, 0), (0, 0)))
    kb = k_pad.reshape(bsz, nb, MOBA_BLOCK, N_KV_HEADS, HEAD_DIM)
    k_means = jnp.mean(kb.astype(F32), axis=2).astype(k.dtype)
    vb = v_pad.reshape(bsz, nb, MOBA_BLOCK, N_KV_HEADS, HEAD_DIM)
    return {'k': k, 'v': v, 'k_pad': k_pad, 'v_pad': v_pad,
            'kb': kb.transpose(0, 3, 1, 2, 4), 'vb': vb.transpose(0, 3, 1, 2, 4),
            'k_means': k_means}


def _moba_prompt(h, pos, w_q, q_norm_g, w_o, sh):
    bsz, s, _ = h.shape
    q = _queries(h, pos, w_q, q_norm_g)
    nb = sh['k_means'].shape[1]
    topk = min(MOBA_TOPK, nb - 1)
    nqb = s // Q_BLOCK

    def blockify(a):
        return a.reshape((bsz * nqb, Q_BLOCK) + a.shape[2:])

    xs = {'q': blockify(q),
          'b': jnp.repeat(jnp.arange(bsz, dtype=jnp.int32), nqb),
          'q0': jnp.tile(jnp.arange(nqb, dtype=jnp.int32) * Q_BLOCK, bsz)}
    if topk > 0:
        q_blk = pos // MOBA_BLOCK
        gate = jnp.einsum('bskgd,bnkd->bskgn', q, sh['k_means']).astype(F32)
        is_past = jnp.arange(nb)[None, :] < q_blk[:, None]
        gate = jnp.where(is_past[None, :, None, None, :], gate, -jnp.inf)
        _, idx = lax.top_k(gate, topk)
        xs['idx'] = blockify(idx)
        xs['valid'] = blockify(idx < q_blk[None, :, None, None, None])
    kvh = jnp.arange(N_KV_HEADS)[None, :, None, None]

    def step(blk):
        b, q0 = blk['b'], blk['q0']
        own0 = (q0 // MOBA_BLOCK) * MOBA_BLOCK
        k_own = lax.dynamic_slice(sh['k_pad'], (b, own0, 0, 0), (1, MOBA_BLOCK, N_KV_HEADS, HEAD_DIM))[0]
        v_own = lax.dynamic_slice(sh['v_pad'], (b, own0, 0, 0), (1, MOBA_BLOCK, N_KV_HEADS, HEAD_DIM))[0]
        own_mask = (own0 + jnp.arange(MOBA_BLOCK))[None, :] <= (q0 + jnp.arange(Q_BLOCK))[:, None]
        if topk > 0:
            sel_shape = (Q_BLOCK, N_KV_HEADS, GQA, topk * MOBA_BLOCK, HEAD_DIM)
            k_sel = sh['kb'][b, kvh, blk['idx']].reshape(sel_shape)
            v_sel = sh['vb'][b, kvh, blk['idx']].reshape(sel_shape)
            valid = jnp.repeat(blk['valid'], MOBA_BLOCK, axis=-1)
            return _moba_core(blk['q'], k_own, v_own, own_mask, k_sel, v_sel, valid)
        return _moba_core(blk['q'], k_own, v_own, own_mask, None, None, None)

    o = lax.map(step, xs)
    return o.reshape(bsz, s, N_HEADS * HEAD_DIM) @ w_o


def _sample_shared(x, pos, kv_norm_g, w_kv, k_norm_g, cache_k, cache_v, page_table):
    k_new, v_new = _shared_kv(x, pos, kv_norm_g, w_kv, k_norm_g)
    dbsz = x.shape[0]
    n_pages = page_table.shape[1]
    ppb = MOBA_BLOCK // PAGE_SIZE
    nbp = (n_pages * PAGE_SIZE) // MOBA_BLOCK
    full = nbp * ppb
    rem_rows = (n_pages - full) * PAGE_SIZE

    def rows(cache, pages):
        g = cache[pages]
        return g.transpose(0, 1, 3, 2, 4).reshape(dbsz, rem_rows, N_KV_HEADS, HEAD_DIM)

    out = {'k_new': k_new, 'v_new': v_new, 'nbp': nbp,
           'k_own': jnp.concatenate([rows(cache_k, page_table[:, full:]), k_new], axis=1),
           'v_own': jnp.concatenate([rows(cache_v, page_table[:, full:]), v_new], axis=1)}
    if nbp > 0:
        pm = jnp.mean(cache_k[page_table[:, :full]].astype(F32), axis=3)
        out['k_means'] = pm.reshape(dbsz, nbp, ppb, N_KV_HEADS, HEAD_DIM).mean(axis=2).astype(cache_k.dtype)
    return out


def _moba_sample(h, pos, w_q, q_norm_g, w_o, sh, cache_k, cache_v, page_table):
    dbsz, t, _ = h.shape
    q = _queries(h, pos, w_q, q_norm_g)
    topk = min(MOBA_TOPK, sh['nbp'])
    rem = sh['k_own'].shape[1] - t
    own_mask = jnp.concatenate([jnp.ones((t, rem), bool), jnp.tril(jnp.ones((t, t), bool))], axis=1)
    xs = {'q': q, 'k_own': sh['k_own'], 'v_own': sh['v_own'], 'prow': page_table}
    if topk > 0:
        gate = jnp.einsum('btkgd,bnkd->btkgn', q, sh['k_means']).astype(F32)
        _, xs['idx'] = lax.top_k(gate, topk)
    ppb = MOBA_BLOCK // PAGE_SIZE
    kvh = jnp.arange(N_KV_HEADS)[None, :, None, None, None]

    def step(seq):
        if topk > 0:
            pt = seq['prow'][seq['idx'][..., None] * ppb + jnp.arange(ppb)]
            sel_shape = (t, N_KV_HEADS, GQA, topk * MOBA_BLOCK, HEAD_DIM)
            k_sel = cache_k[pt, kvh].reshape(sel_shape)
            v_sel = cache_v[pt, kvh].reshape(sel_shape)
            return _moba_core(seq['q'], seq['k_own'], seq['v_own'], own_mask, k_sel, v_sel, None)
        return _moba_core(seq['q'], seq['k_own'], seq['v_own'], own_mask, None, None, None)

    o = lax.map(step, xs)
    return o.reshape(dbsz, t, N_HEADS * HEAD_DIM) @ w_o


def setup_inputs(seed: int = 0) -> dict:
    key = jax.random.key(seed)
    ks = jax.random.split(key, 28)

    def nrm(k, shape, scale=1.0):
        return jax.random.normal(k, shape, F32) * scale

    def gain(k, shape):
        return 1.0 + 0.02 * jax.random.normal(k, shape, F32)

    n_pages = PAST_LEN // PAGE_SIZE
    used = DEC_BATCH * n_pages
    n_phys = used + (used + 3) // 4
    perm = jax.random.permutation(ks[0], n_phys).astype(jnp.int32)
    kvw = 2 * N_KV_HEADS * HEAD_DIM
    qw = N_HEADS * HEAD_DIM
    return {
        'x_prompt': nrm(ks[1], (BATCH, SEQ, D_MODEL)),
        'x_sample': nrm(ks[2], (DEC_BATCH, DEC_SEQ, D_MODEL)),
        'cache_k': nrm(ks[3], (n_phys, N_KV_HEADS, PAGE_SIZE, HEAD_DIM)),
        'cache_v': nrm(ks[4], (n_phys, N_KV_HEADS, PAGE_SIZE, HEAD_DIM)),
        'page_table': perm[:used].reshape(DEC_BATCH, n_pages),
        'c_prompt': nrm(ks[5], (BATCH, D_MODEL)),
        'c_sample': nrm(ks[6], (DEC_BATCH, D_MODEL)),
        'ada_w': nrm(ks[7], (DEPTH, D_MODEL, 6 * D_MODEL), 0.5 * D_MODEL ** -0.5),
        'ada_b': nrm(ks[8], (DEPTH, 6 * D_MODEL), 0.02),
        'norm1_g': gain(ks[9], (DEPTH, D_MODEL)),
        'norm2_g': gain(ks[10], (DEPTH, D_MODEL)),
        'a_w_in': nrm(ks[11], (N_A_LAYERS, D_MODEL, 2 * A_WIDTH), D_MODEL ** -0.5),
        'a_b_in': nrm(ks[12], (N_A_LAYERS, 2 * A_WIDTH), 0.02),
        'a_g_sgu': gain(ks[13], (N_A_LAYERS, A_WIDTH)),
        'a_w_s': nrm(ks[14], (N_A_LAYERS, A_GROUPS, CHUNK, CHUNK), CHUNK ** -0.5),
        'a_b_s': nrm(ks[15], (N_A_LAYERS, A_GROUPS, CHUNK), 0.02),
        'a_w_out': nrm(ks[16], (N_A_LAYERS, A_WIDTH, D_MODEL), A_WIDTH ** -0.5),
        'kv_norm_g': gain(ks[17], (D_MODEL,)),
        'w_kv': nrm(ks[18], (D_MODEL, kvw), D_MODEL ** -0.5),
        'k_norm_g': gain(ks[19], (HEAD_DIM,)),
        'b_w_q': nrm(ks[20], (N_B_LAYERS, D_MODEL, qw), D_MODEL ** -0.5),
        'b_q_norm_g': gain(ks[21], (N_B_LAYERS, HEAD_DIM)),
        'b_w_o': nrm(ks[22], (N_B_LAYERS, qw, D_MODEL), qw ** -0.5),
        'peer_w_q': nrm(ks[23], (DEPTH, D_MODEL, PEER_HEADS * PEER_KEY_DIM), D_MODEL ** -0.5),
        'peer_sub_keys': nrm(ks[24], (DEPTH, 2, PEER_N_KEYS, PEER_KEY_DIM // 2), (PEER_KEY_DIM // 2) ** -0.5),
        'peer_u': nrm(ks[25], (DEPTH, PEER_EXPERTS, D_MODEL), D_MODEL ** -0.5),
        'peer_v': nrm(ks[26], (DEPTH, PEER_EXPERTS, D_MODEL), PEER_HEADS ** -0.5),
    }


def reference(x_prompt, x_sample, cache_k, cache_v, page_table, c_prompt, c_sample,
              ada_w, ada_b, norm1_g, norm2_g,
              a_w_in, a_b_in, a_g_sgu, a_w_s, a_b_s, a_w_out,
              kv_norm_g, w_kv, k_norm_g,
              b_w_q, b_q_norm_g, b_w_o,
              peer_w_q, peer_sub_keys, peer_u, peer_v):
    def trunk(x, c, make_shared, attend):
        a_rows = []
        shared = None
        for l in range(DEPTH):
            shift1, scale1, gate1, shift2, scale2, gate2 = _ada(c, ada_w[l], ada_b[l])
            h = _modnorm(x, norm1_g[l], shift1, scale1)
            if l < N_A_LAYERS:
                m, v_rows = _chunk_gmlp(h, a_w_in[l], a_b_in[l], a_g_sgu[l], a_w_s[l], a_b_s[l], a_w_out[l])
                a_rows.append(v_rows)
            else:
                j = l - N_A_LAYERS
                m = attend(h, b_w_q[j], b_q_norm_g[j], b_w_o[j], shared)
            x = x + gate1 * m
            h = _modnorm(x, norm2_g[l], shift2, scale2)
            x = x + gate2 * _peer(h, peer_w_q[l], peer_sub_keys[l], peer_u[l], peer_v[l])
            if l == N_A_LAYERS - 1:
                shared = make_shared(x)
        return x, a_rows, shared

    bsz, s_len, _ = x_prompt.shape
    pos_p = jnp.arange(s_len, dtype=jnp.int32)
    y_prompt, _, sp = trunk(
        x_prompt, c_prompt,
        lambda x: _prompt_shared(x, pos_p, kv_norm_g, w_kv, k_norm_g),
        lambda h, wq, gq, wo, sh: _moba_prompt(h, pos_p, wq, gq, wo, sh))

    dbsz, t_len, _ = x_sample.shape
    past_len = page_table.shape[1] * PAGE_SIZE
    pos_s = past_len + jnp.arange(t_len, dtype=jnp.int32)
    y_sample, a_rows_s, ss = trunk(
        x_sample, c_sample,
        lambda x: _sample_shared(x, pos_s, kv_norm_g, w_kv, k_norm_g, cache_k, cache_v, page_table),
        lambda h, wq, gq, wo, sh: _moba_sample(h, pos_s, wq, gq, wo, sh, cache_k, cache_v, page_table))

    n_pp = s_len // PAGE_SIZE
    k_prompt = sp['k'].reshape(bsz, n_pp, PAGE_SIZE, N_KV_HEADS, HEAD_DIM).transpose(0, 1, 3, 2, 4)
    v_prompt = sp['v'].reshape(bsz, n_pp, PAGE_SIZE, N_KV_HEADS, HEAD_DIM).transpose(0, 1, 3, 2, 4)
    k_sample = ss['k_new'].transpose(0, 2, 1, 3)
    v_sample = ss['v_new'].transpose(0, 2, 1, 3)
    a_v_sample = jnp.stack(a_rows_s, axis=0)
    return (y_prompt, y_sample, k_prompt, v_prompt, k_sample, v_sample, a_v_sample)
```

```python
import numpy as np
from contextlib import ExitStack
import concourse.bass as bass
import concourse.mybir as mybir
from concourse.bass_utils import run_bass_kernel_spmd

F32 = mybir.dt.float32
BF16 = mybir.dt.bfloat16
U32 = mybir.dt.uint32
AF = mybir.ActivationFunctionType
ALU = mybir.AluOpType
AX = mybir.AxisListType

NS = 33
SMP = 32
TILES_A = list(range(33))
TILES_B = list(range(16)) + [32]
MNEG = -30000.0
D = 1024
EPS = 1e-6
NEG = -1.0e30


class Sched:
    COMPUTE = ("pe", "act", "dve", "pool")

    def __init__(self, nc, n_dma_sems=8):
        self.nc = nc
        self.eng = {"pe": nc.tensor, "act": nc.scalar, "dve": nc.vector, "pool": nc.gpsimd, "sp": nc.sync}
        self.sem = {}
        self.cnt = {}
        for e in self.COMPUTE:
            self.sem[e] = nc.alloc_semaphore("sem_" + e)
            self.cnt[e] = 0
        self.dsem = {}
        self.dcnt = {}
        self.drot = {}
        for q in ("sp", "pool"):
            self.dsem[q] = [nc.alloc_semaphore("dsem_%s_%d" % (q, i)) for i in range(n_dma_sems)]
            self.dcnt[q] = [0] * n_dma_sems
            self.drot[q] = 0
        self.seen = {e: {} for e in self.eng}
        self.regions = {}
        self.n_instr = 0
        self.n_wait = 0
        self._pend_r = []
        self._pend_w = []

    def _semh(self, key):
        return self.sem[key[1]] if key[0] == "c" else self.dsem[key[1]][key[2]]

    def _wait(self, eng, deps):
        best = {}
        for k, v in deps:
            if v > best.get(k, 0):
                best[k] = v
        for k, v in best.items():
            if eng == "pe" and k == ("c", "pe"):
                continue
            if self.seen[eng].get(k, 0) >= v:
                continue
            self.eng[eng].wait_ge(self._semh(k), v)
            self.seen[eng][k] = v
            self.n_wait += 1

    def _deps(self, reads, writes):
        deps = []
        for r in reads:
            reg = self.regions.get(r)
            if reg is None:
                continue
            deps += reg[0]
            if reg[2]:
                deps += reg[1]
        for w in writes:
            reg = self.regions.get(w)
            if reg is None:
                continue
            deps += reg[0]
            deps += reg[1]
        return deps

    def _record(self, dep, reads, writes):
        for r in reads:
            reg = self.regions.setdefault(r, [[], [], isinstance(r, str) and r.startswith("ps")])
            if reg[2]:
                reg[0] = [dep]
                reg[1] = []
            else:
                reg[1] = [d for d in reg[1] if d[0] != dep[0]] + [dep]
        for w in writes:
            reg = self.regions.setdefault(w, [[], [], isinstance(w, str) and w.startswith("ps")])
            reg[0] = [dep]
            reg[1] = []

    def op(self, eng, fn, reads=(), writes=()):
        reads = list(reads)
        writes = list(writes)
        self._wait(eng, self._deps(reads, writes))
        ins = fn(self.eng[eng])
        self.cnt[eng] += 1
        ins.then_inc(self.sem[eng], 1)
        self._record((("c", eng), self.cnt[eng]), reads, writes)
        self.n_instr += 1
        return ins

    def mm(self, fn, reads=(), writes=(), last=True):
        reads = list(reads)
        writes = list(writes)
        self._wait("pe", self._deps(reads, writes))
        ins = fn(self.eng["pe"])
        self.n_instr += 1
        if last:
            self.cnt["pe"] += 1
            ins.then_inc(self.sem["pe"], 1)
            self._record((("c", "pe"), self.cnt["pe"]), reads + self._pend_r, writes + self._pend_w)
            self._pend_r = []
            self._pend_w = []
        else:
            self._pend_r += reads
            self._pend_w += writes
        return ins

    def dma(self, q, out, in_, reads=(), writes=(), **kw):
        reads = list(reads)
        writes = list(writes)
        i = self.drot[q]
        self.drot[q] = (i + 1) % len(self.dsem[q])
        key = ("d", q, i)
        deps = self._deps(reads, writes)
        if self.dcnt[q][i] > 0:
            deps.append((key, self.dcnt[q][i]))
        self._wait(q, deps)
        ins = self.eng[q].dma_start(out=out, in_=in_, **kw)
        self.dcnt[q][i] += 16
        ins.then_inc(self.dsem[q][i], 16)
        self._record((key, self.dcnt[q][i]), reads, writes)
        self.n_instr += 1

    def idma(self, out, in_, idx_ap, reads=(), writes=()):
        q = "pool"
        reads = list(reads)
        writes = list(writes)
        i = self.drot[q]
        self.drot[q] = (i + 1) % len(self.dsem[q])
        key = ("d", q, i)
        deps = self._deps(reads, writes)
        if self.dcnt[q][i] > 0:
            deps.append((key, self.dcnt[q][i]))
        self._wait(q, deps)
        ins = self.eng[q].indirect_dma_start(out=out, out_offset=None, in_=in_,
                                             in_offset=bass.IndirectOffsetOnAxis(ap=idx_ap, axis=0))
        self.dcnt[q][i] += 16
        ins.then_inc(self.dsem[q][i], 16)
        self._record((key, self.dcnt[q][i]), reads, writes)
        self.n_instr += 1

    def _all(self):
        deps = [(("c", e), self.cnt[e]) for e in self.COMPUTE if self.cnt[e] > 0]
        for q in self.dsem:
            for i, c in enumerate(self.dcnt[q]):
                if c > 0:
                    deps.append((("d", q, i), c))
        return deps

    def barrier(self):
        deps = self._all()
        for e in self.eng:
            self._wait(e, deps)
        self.regions = {}

    finish = barrier


def build_program(n_layers=4, do_peer=True, do_attn=True):
    nc = bass.Bass("TRN2", target_bir_lowering=False)

    def din(name, shape, dt=F32):
        return nc.dram_tensor(name, list(shape), dt, kind="ExternalInput").ap()

    def dout(name, shape, dt=F32):
        return nc.dram_tensor(name, list(shape), dt, kind="ExternalOutput").ap()

    xp = din("xp", [32, 128, D]); xs = din("xs", [128, D]); cT = din("cT", [D, 32])
    Ep = din("Ep", [32, 128]); Es = din("Es", [32, 128])
    ident = din("ident", [128, 128]); iota_in = din("iota", [128, 128])
    ada_w = din("ada_w", [4, D, 6 * D]); ada_b = din("ada_b", [4, 6 * D])
    norm1_g = din("norm1_g", [4, D]); norm2_g = din("norm2_g", [4, D])
    a_w_in = din("a_w_in", [2, D, 4096]); a_b_in = din("a_b_in", [2, 4096]); a_g_sgu = din("a_g_sgu", [2, 2048])
    wsTp = din("wsTp", [2, 128, 8, 128]); wsTs = din("wsTs", [2, 128, 8, 128])
    bsP = din("bsP", [2, 8, 128]); bsS = din("bsS", [2, 8, 128])
    maskP = din("maskP", [128, 128]); maskS = din("maskS", [128, 128])
    a_w_out = din("a_w_out", [2, 2048, D])
    kv_norm_g = din("kv_norm_g", [D]); w_kv = din("w_kv", [D, 512]); k_norm_g = din("k_norm_g", [64])
    peer_w_q = din("peer_w_q", [4, D, 2048]); skT = din("skT", [4, 2, 128, 128])
    peer_uT = din("peer_uT", [4, D, 16384]); peer_v = din("peer_v", [4, 16384, D])
    cosT = din("cosT", [NS, 128, 8]); sinT = din("sinT", [NS, 128, 8])
    b_w_q = din("b_w_q", [2, D, D]); b_q_norm_g = din("b_q_norm_g", [2, 64]); b_w_o = din("b_w_o", [2, D, D])
    cache_k = din("cache_k", [2560 * 512, 64]); cache_v = din("cache_v", [2560 * 512, 64])
    ptab = din("ptab", [256], mybir.dt.int32)
    iotap_in = din("iotap", [128, 1]); SelP_in = din("SelP", [32, 16]); SelS_in = din("SelS", [16, 8])
    maskOwn_in = din("maskOwn", [128, 256]); maskOwnS_in = din("maskOwnS", [128, 128]); rowmask_in = din("rowmask", [128, 16])

    y_p = dout("y_p", [16, 128, D]); y_s = dout("y_s", [128, D])
    kp_o = dout("kp_o", [16, 4, 128, 64]); vp_o = dout("vp_o", [16, 4, 128, 64])
    ks_o = dout("ks_o", [16, 4, 4, 64]); vs_o = dout("vs_o", [16, 4, 4, 64])
    av_o = dout("av_o", [2, 64, 2048])

    xd = nc.dram_tensor("xd", [NS, 128, D], F32, kind="Internal").ap()
    kT_d = nc.dram_tensor("kT_d", [NS, 2, 128, 128], BF16, kind="Internal").ap()
    v_d = nc.dram_tensor("v_d", [NS, 128, 256], BF16, kind="Internal").ap()
    ks_d = nc.dram_tensor("ks_d", [NS, 256], F32, kind="Internal").ap()
    kTs_d = nc.dram_tensor("kTs_d", [16, 16, 2, 128, 128], BF16, kind="Internal").ap()
    vS_d = nc.dram_tensor("vS_d", [16, 16, 128, 256], BF16, kind="Internal").ap()
    ksS_d = nc.dram_tensor("ksS_d", [16, 16, 256], F32, kind="Internal").ap()
    hT_d = nc.dram_tensor("hT_d", [NS, 128, 8, 128], BF16, kind="Internal").ap()
    G_d = nc.dram_tensor("G_d", [NS, 128, 128, 128], BF16, kind="Internal").ap()

    S = Sched(nc)
    V = lambda fn, r=(), w=(): S.op("dve", fn, r, w)
    A = lambda fn, r=(), w=(): S.op("act", fn, r, w)

    def sb(name, shape, dt=F32):
        return nc.alloc_sbuf_tensor(name, list(shape), dt)

    PS = [nc.alloc_psum_tensor("psb%d" % i, [128, 512], F32) for i in range(7)]
    PSB = nc.alloc_psum_tensor("psbf", [128, 1024], BF16)
    psk = lambda i: "ps%d" % i

    idf = sb("idf", [128, 128]); idb = sb("idb", [128, 128], BF16)
    iof = sb("iof", [128, 128]); iob = sb("iob", [128, 128], BF16)
    ones1 = sb("ones1", [1, 128])
    Ept = sb("Ept", [32, 128]); Est = sb("Est", [32, 128])
    cTt = sb("cTt", [128, 8, 32]); scT = sb("scT", [128, 8, 32], BF16)
    S.dma("sp", idf[:], ident, writes=["idf"])
    S.dma("sp", iof[:], iota_in, writes=["iof"])
    S.dma("sp", Ept[:], Ep, writes=["Ept"])
    S.dma("sp", Est[:], Es, writes=["Est"])
    S.dma("sp", cTt[:], cT.rearrange("(k p) m -> p k m", p=128), writes=["cTt"])
    V(lambda e: e.tensor_copy(out=idb[:], in_=idf[:]), ["idf"], ["idb"])
    V(lambda e: e.tensor_copy(out=iob[:], in_=iof[:]), ["iof"], ["iob"])
    V(lambda e: e.memset(ones1[:], 1.0), [], ["ones1"])
    A(lambda e: e.activation(out=scT[:], in_=cTt[:], func=AF.Silu), ["cTt"], ["scT"])

    modseq = sb("modseq", [32, 3 * D])
    Aseq = sb("Aseq", [32, D])
    modtok = [sb("modtok%d" % i, [128, D]) for i in range(6)]

    uid = [0]

    def phase_locals():
        ph = ExitStack()
        uid[0] += 1
        u = uid[0]
        return ph, (lambda name, shape, dt=F32: ph.enter_context(nc.sbuf_tensor("%s_%d" % (name, u), list(shape), dt)))

    def adaln(l, half):
        S.barrier()
        ph, L = phase_locals()
        adab = L("adab", [1, 512]); gbt = L("gbt", [32, D])
        wa = [L("wa0", [128, 8, 512], BF16), L("wa1", [128, 8, 512], BF16)]
        S.dma("sp", gbt[:], (norm1_g if half == 0 else norm2_g)[l].partition_broadcast(32), writes=["gbt"])
        for n in range(6):
            col = half * 3072 + n * 512
            w = wa[n % 2]
            S.dma("pool", w[:], ada_w[l, :, col:col + 512].rearrange("(k p) n -> p k n", p=128), writes=["wa%d" % (n % 2)])
            S.dma("sp", adab[:], ada_b[l:l + 1, col:col + 512], writes=["adab"])
            bank = PS[n % 2]
            for kc in range(8):
                S.mm(lambda e: e.matmul(bank[0:32, :], lhsT=scT[:, kc, :], rhs=w[:, kc, :], start=(kc == 0), stop=False),
                     ["scT", "wa%d" % (n % 2)], [psk(n % 2)], last=False)
            S.mm(lambda e: e.matmul(bank[0:32, :], lhsT=ones1[0:1, 0:32], rhs=adab[0:1, :], start=False, stop=True),
                 ["ones1", "adab"], [psk(n % 2)])
            A(lambda e: e.copy(out=modseq[:, n * 512:(n + 1) * 512], in_=bank[0:32, :]), [psk(n % 2)], ["modseq"])
        V(lambda e: e.scalar_tensor_tensor(out=Aseq[:], in0=modseq[:, D:2 * D], scalar=1.0, in1=gbt[:],
                                           op0=ALU.add, op1=ALU.mult), ["modseq", "gbt"], ["Aseq"])
        expand_mod()
        S.barrier()
        ph.close()

    def expand_mod():
        srcs = [Aseq[:], modseq[:, 0:D], modseq[:, 2 * D:3 * D]]
        k = 0
        for gi, Et in enumerate((Ept, Est)):
            for vi in range(3):
                for hf in range(2):
                    bank = PS[k % 2]
                    S.mm(lambda e: e.matmul(bank[:], lhsT=Et[:], rhs=srcs[vi][:, hf * 512:(hf + 1) * 512],
                                            start=True, stop=True),
                         ["Ept", "Est", "Aseq", "modseq"], [psk(k % 2)])
                    A(lambda e: e.copy(out=modtok[gi * 3 + vi][:, hf * 512:(hf + 1) * 512], in_=bank[:]),
                      [psk(k % 2)], ["modtok%d" % (gi * 3 + vi)])
                    k += 1

    xt = [sb("xt0", [128, D]), sb("xt1", [128, D])]
    st = sb("stat", [128, 8])
    hb = sb("hb", [128, D], BF16)
    hT = sb("hT", [128, 8, 128], BF16)
    tmp = sb("tmp", [128, D])

    def load_x(t, first):
        xb = xt[t % 2]
        src = (xp[t] if t < SMP else xs) if first else xd[t]
        S.dma("sp", xb[:], src, reads=["xd%d" % t], writes=["xt%d" % (t % 2)])
        return xb

    def store_x(t):
        S.dma("sp", xd[t], xt[t % 2][:], reads=["xt%d" % (t % 2)], writes=["xd%d" % t])

    def modnorm_T(xb, xkey, At, Bt, akey, bkey):
        A(lambda e: e.activation(out=tmp[:], in_=xb[:], func=AF.Square, accum_out=st[:, 0:1]), [xkey], ["tmp", "st"])
        A(lambda e: e.activation(out=st[:, 1:2], in_=st[:, 0:1], func=AF.Sqrt, scale=1.0 / D, bias=EPS), ["st"], ["st"])
        V(lambda e: e.reciprocal(out=st[:, 2:3], in_=st[:, 1:2]), ["st"], ["st"])
        if Bt is not None:
            V(lambda e: e.scalar_tensor_tensor(out=tmp[:], in0=xb[:], scalar=st[:, 2:3], in1=At, op0=ALU.mult, op1=ALU.mult),
              [xkey, "st", akey], ["tmp"])
            V(lambda e: e.tensor_tensor(out=hb[:], in0=tmp[:], in1=Bt, op=ALU.add), ["tmp", bkey], ["hb"])
        else:
            V(lambda e: e.scalar_tensor_tensor(out=hb[:], in0=xb[:], scalar=st[:, 2:3], in1=At, op0=ALU.mult, op1=ALU.mult),
              [xkey, "st", akey], ["hb"])
        for k in range(8):
            S.mm(lambda e: e.transpose(out=PSB[:, k * 128:(k + 1) * 128], in_=hb[:, k * 128:(k + 1) * 128], identity=idb[:]),
                 ["hb", "idb"], ["psT"], last=(k == 7))
        A(lambda e: e.copy(out=hT[:].rearrange("p k t -> p (k t)"), in_=PSB[:]), ["psT"], ["hT"])

    def resid_add(xb, xkey, banks, bkeys, Gt, gkey):
        for hf in range(2):
            V(lambda e: e.tensor_tensor(out=tmp[:, hf * 512:(hf + 1) * 512], in0=banks[hf][:], in1=Gt[:, hf * 512:(hf + 1) * 512],
                                        op=ALU.mult), [bkeys[hf], gkey], ["tmp"])
        V(lambda e: e.tensor_tensor(out=xb[:], in0=xb[:], in1=tmp[:], op=ALU.add), ["tmp", xkey], [xkey])

    def gmlp_phase(l, first):
        S.barrier()
        ph, L = phase_locals()
        w_in = L("w_in", [128, 8, 4096], BF16)
        wo = [L("wo0", [128, 4, D], BF16), L("wo1", [128, 4, D], BF16)]
        brow = L("brow", [1, 4096])
        binu = L("binu", [128, 16])
        gsg = L("gsg", [128, 2048])
        wsPb = L("wsPb", [128, 8, 128], BF16); wsSb = L("wsSb", [128, 8, 128], BF16)
        mP = L("mP", [128, 128], BF16); mS = L("mS", [128, 128], BF16)
        vf = L("vf", [128, 2048]); vb = L("vb", [128, 2048], BF16)
        uT = L("uT", [128, 16, 128], BF16); yT = L("yT", [128, 16, 128], BF16)
        binv = brow[0:1, 0:2048]
        bsPt = brow[0:1, 2048:3072].rearrange("p (g t) -> p g t", t=128)
        bsSt = brow[0:1, 3072:4096].rearrange("p (g t) -> p g t", t=128)
        for k in range(8):
            S.dma("pool", w_in[:, k, :], a_w_in[l, k * 128:(k + 1) * 128, :], writes=["w_in"], max_dma_last_dim=8192)
        S.dma("sp", brow[0:1, 0:2048], a_b_in[l:l + 1, 2048:4096], writes=["brow"])
        S.dma("sp", brow[0:1, 2048:3072], bsP[l:l + 1].rearrange("o g t -> o (g t)"), writes=["brow"])
        S.dma("sp", brow[0:1, 3072:4096], bsS[l:l + 1].rearrange("o g t -> o (g t)"), writes=["brow"])
        S.dma("sp", binu[:], a_b_in[l, 0:2048].rearrange("(c p) -> p c", p=128), writes=["binu"], allow_slow_non_contiguous=True)
        S.dma("sp", gsg[:], a_g_sgu[l].partition_broadcast(128), writes=["gsg"])
        S.dma("pool", wsPb[:], wsTp[l], writes=["wsPb"]); S.dma("pool", wsSb[:], wsTs[l], writes=["wsSb"])
        S.dma("pool", mP[:], maskP, writes=["mP"]); S.dma("pool", mS[:], maskS, writes=["mS"])
        S.barrier()
        V(lambda e: e.tensor_tensor(out=wsPb[:], in0=wsPb[:], in1=mP[:].unsqueeze(1).to_broadcast([128, 8, 128]), op=ALU.mult),
          ["wsPb", "mP"], ["wsPb"])
        V(lambda e: e.tensor_tensor(out=wsSb[:], in0=wsSb[:], in1=mS[:].unsqueeze(1).to_broadcast([128, 8, 128]), op=ALU.mult),
          ["wsSb", "mS"], ["wsSb"])
        wo_n = [0]

        def load_wo(j):
            S.dma("pool", wo[wo_n[0] % 2][:], a_w_out[l, j * 512:(j + 1) * 512, :].rearrange("(c p) d -> p c d", p=128),
                  writes=["wo%d" % (wo_n[0] % 2)])
            wo_n[0] += 1
        for t in TILES_A:
            g = 0 if t < SMP else 1
            At, Bt, Gt = modtok[3 * g], modtok[3 * g + 1], modtok[3 * g + 2]
            ak, bk, gk = "modtok%d" % (3 * g), "modtok%d" % (3 * g + 1), "modtok%d" % (3 * g + 2)
            xkey = "xt%d" % (t % 2)
            xb = load_x(t, first)
            modnorm_T(xb, xkey, At[:], Bt[:], ak, bk)
            for n in range(4):
                for kc in range(8):
                    S.mm(lambda e: e.matmul(PS[n][:], lhsT=hT[:, kc, :], rhs=w_in[:, kc, 2048 + n * 512:2048 + (n + 1) * 512],
                                            start=(kc == 0), stop=False), ["hT", "w_in"], [psk(n)], last=False)
                S.mm(lambda e: e.matmul(PS[n][:], lhsT=ones1[0:1, :], rhs=binv[0:1, n * 512:(n + 1) * 512], start=False, stop=True),
                     ["ones1", "brow"], [psk(n)])
                A(lambda e: e.activation(out=vf[:, n * 512:(n + 1) * 512], in_=PS[n][:], func=AF.Gelu_apprx_tanh), [psk(n)], ["vf"])
            A(lambda e: e.activation(out=tmp[:], in_=vf[:, 0:1024], func=AF.Square, accum_out=st[:, 3:4]), ["vf"], ["tmp", "st"])
            A(lambda e: e.activation(out=tmp[:], in_=vf[:, 1024:2048], func=AF.Square, accum_out=st[:, 4:5]), ["vf"], ["tmp", "st"])
            V(lambda e: e.tensor_tensor(out=st[:, 3:4], in0=st[:, 3:4], in1=st[:, 4:5], op=ALU.add), ["st"], ["st"])
            A(lambda e: e.activation(out=st[:, 4:5], in_=st[:, 3:4], func=AF.Sqrt, scale=1.0 / 2048, bias=EPS), ["st"], ["st"])
            V(lambda e: e.reciprocal(out=st[:, 5:6], in_=st[:, 4:5]), ["st"], ["st"])
            V(lambda e: e.scalar_tensor_tensor(out=vf[:], in0=vf[:], scalar=st[:, 5:6], in1=gsg[:], op0=ALU.mult, op1=ALU.mult),
              ["vf", "st", "gsg"], ["vf"])
            V(lambda e: e.tensor_copy(out=vb[:], in_=vf[:]), ["vf"], ["vb"])
            if t == SMP:
                S.dma("sp", av_o[l], vf[0:64, :], reads=["vf"])
            for c in range(16):
                bank = PS[3 + c // 4]
                for kc in range(8):
                    S.mm(lambda e: e.matmul(bank[:, (c % 4) * 128:(c % 4 + 1) * 128], lhsT=w_in[:, kc, c * 128:(c + 1) * 128],
                                            rhs=hT[:, kc, :], start=(kc == 0), stop=(kc == 7)),
                         ["hT", "w_in"], [psk(3 + c // 4)], last=(kc == 7))
                A(lambda e: e.activation(out=uT[:, c, :], in_=bank[:, (c % 4) * 128:(c % 4 + 1) * 128], func=AF.Gelu_apprx_tanh,
                                         bias=binu[:, c:c + 1]), [psk(3 + c // 4), "binu"], ["uT"])
            wsb = wsPb if t < SMP else wsSb
            bst = bsPt if t < SMP else bsSt
            for c in range(16):
                bank = PS[c // 4]
                gi = c // 2
                S.mm(lambda e: e.matmul(bank[:, (c % 4) * 128:(c % 4 + 1) * 128], lhsT=vb[:, c * 128:(c + 1) * 128],
                                        rhs=wsb[:, gi, :], start=True, stop=False), ["vb", "wsPb", "wsSb"], [psk(c // 4)], last=False)
                S.mm(lambda e: e.matmul(bank[:, (c % 4) * 128:(c % 4 + 1) * 128], lhsT=ones1[0:1, :], rhs=bst[0:1, gi, :],
                                        start=False, stop=True), ["ones1", "brow"], [psk(c // 4)], last=(c % 4 == 3))
                if c % 4 == 3:
                    cb = c // 4
                    V(lambda e: e.tensor_tensor(out=yT[:, cb * 4:(cb + 1) * 4, :].rearrange("p c t -> p (c t)"), in0=bank[:],
                                                in1=uT[:, cb * 4:(cb + 1) * 4, :].rearrange("p c t -> p (c t)"), op=ALU.mult),
                      [psk(cb), "uT"], ["yT"])
            for j in range(4):
                slot = wo_n[0] % 2
                load_wo(j)
                for cc in range(4):
                    c = j * 4 + cc
                    for hf in range(2):
                        S.mm(lambda e: e.matmul(PS[4 + hf][:], lhsT=yT[:, c, :], rhs=wo[slot][:, cc, hf * 512:(hf + 1) * 512],
                                                start=(c == 0), stop=(c == 15)), ["yT", "wo%d" % slot], [psk(4 + hf)],
                             last=(cc == 3 and hf == 1))
            resid_add(xb, xkey, [PS[4], PS[5]], [psk(4), psk(5)], Gt, gk)
            store_x(t)
        S.barrier()
        ph.close()

    def peer_phase(l):
        S.barrier()
        ph, L = phase_locals()
        wq = [L("wq0", [128, 8, 512], BF16), L("wq1", [128, 8, 512], BF16)]
        skt = L("skt", [128, 2, 128], BF16)
        RB = []
        for par in range(2):
            d = {}
            d["qT"] = L("qT%d" % par, [128, 16, 128], BF16)
            d["ssb"] = L("ssb%d" % par, [128, 16, 128]); d["ss2"] = L("ss2%d" % par, [128, 16, 128])
            d["sv"] = L("sv%d" % par, [128, 16, 16]); d["si"] = L("si%d" % par, [128, 16, 16], U32); d["sif"] = L("sif%d" % par, [128, 16, 16])
            d["fv"] = L("fv%d" % par, [128, 8, 16]); d["fi"] = L("fi%d" % par, [128, 8, 16], U32)
            d["fa"] = L("fa%d" % par, [128, 8, 16], U32); d["fb"] = L("fb%d" % par, [128, 8, 16], U32)
            d["faf"] = L("faf%d" % par, [128, 8, 16]); d["fbf"] = L("fbf%d" % par, [128, 8, 16])
            d["ex"] = L("ex%d" % par, [128, 8, 16]); d["zz"] = L("zz%d" % par, [128, 8]); d["rz"] = L("rz%d" % par, [128, 8])
            d["sel"] = L("sel%d" % par, [128, 3, 128]); d["selT"] = L("selT%d" % par, [128, 3, 128], BF16)
            RB.append(d)
        oh = L("oh", [128, 8, 16, 16])
        OIb = [L("OI0", [128, 32, 128], BF16), L("OI1", [128, 32, 128], BF16)]
        OJb = [L("OJ0", [128, 32, 128], BF16), L("OJ1", [128, 32, 128], BF16)]
        G = L("G", [128, 128, 128], BF16)
        S.dma("pool", skt[:], skT[l].rearrange("p d k -> d p k"), writes=["skt"])
        wq_n = [0]

        def load_wq(j):
            S.dma("pool", wq[wq_n[0] % 2][:], peer_w_q[l, :, j * 512:(j + 1) * 512].rearrange("(k p) n -> p k n", p=128),
                  writes=["wq%d" % (wq_n[0] % 2)])
            wq_n[0] += 1

        for ti, t in enumerate(TILES_A if l < 2 else TILES_B):
            par = ti % 2
            R = RB[par]
            K = lambda name: "%s_%d" % (name, par)
            qT, ssb, ss2, sv, si, sif = R["qT"], R["ssb"], R["ss2"], R["sv"], R["si"], R["sif"]
            fv, fi, fa, fb, faf, fbf, ex, zz, rz, sel, selT = (R["fv"], R["fi"], R["fa"], R["fb"], R["faf"], R["fbf"], R["ex"],
                                                                R["zz"], R["rz"], R["sel"], R["selT"])
            cand = ssb[:].rearrange("p g k -> p (g k)").rearrange("p (h c) -> p h c", c=256)
            cand2 = ss2[:].rearrange("p g k -> p (g k)").rearrange("p (h c) -> p h c", c=256)
            g = 0 if t < SMP else 1
            At, Bt = modtok[3 * g], modtok[3 * g + 1]
            ak, bk = "modtok%d" % (3 * g), "modtok%d" % (3 * g + 1)
            xkey = "xt%d" % (t % 2)
            xb = load_x(t, False)
            modnorm_T(xb, xkey, At[:], Bt[:], ak, bk)
            S.dma("sp", hT_d[t], hT[:], reads=["hT"])
            for b4 in range(4):
                slot = wq_n[0] % 2
                load_wq(b4)
                bank = PS[2 + b4]
                for g4 in range(4):
                    for kc in range(8):
                        S.mm(lambda e: e.matmul(bank[:, g4 * 128:(g4 + 1) * 128], lhsT=wq[slot][:, kc, g4 * 128:(g4 + 1) * 128],
                                                rhs=hT[:, kc, :], start=(kc == 0), stop=(kc == 7)),
                             ["wq%d" % slot, "hT"], [psk(2 + b4)], last=(kc == 7 and g4 == 3))
                A(lambda e: e.copy(out=qT[:, b4 * 4:(b4 + 1) * 4, :].rearrange("p g t -> p (g t)"), in_=bank[:]),
                  [psk(2 + b4)], [K("qT%d" % b4)])
            SSB = [K("ssb%d" % gbk) for gbk in range(16)]
            SS2 = [K("ss2_%d" % gbk) for gbk in range(16)]
            for gbk in range(16):
                bank = PS[2 + gbk // 4]
                S.mm(lambda e: e.matmul(bank[:, (gbk % 4) * 128:(gbk % 4 + 1) * 128], lhsT=qT[:, gbk, :], rhs=skt[:, gbk % 2, :],
                                        start=True, stop=True), [K("qT%d" % (gbk // 4)), "skt"], [psk(2 + gbk // 4)], last=(gbk % 4 == 3))
                if gbk % 4 == 3:
                    b4 = gbk // 4
                    A(lambda e: e.copy(out=ssb[:, b4 * 4:(b4 + 1) * 4, :].rearrange("p g k -> p (g k)"), in_=bank[:]),
                      [psk(2 + b4)], SSB[b4 * 4:(b4 + 1) * 4])
            SVA = [K("sva%d" % gbk) for gbk in range(16)]; SVB = [K("svb%d" % gbk) for gbk in range(16)]
            SIA = [K("sia%d" % gbk) for gbk in range(16)]; SIB = [K("sib%d" % gbk) for gbk in range(16)]
            for gbk in range(16):
                V(lambda e: e.max(out=sv[:, gbk, 0:8], in_=ssb[:, gbk, :]), [SSB[gbk]], [SVA[gbk]])
            for gbk in range(16):
                V(lambda e: e.max_index(out=si[:, gbk, 0:8], in_max=sv[:, gbk, 0:8], in_values=ssb[:, gbk, :]), [SSB[gbk], SVA[gbk]], [SIA[gbk]])
            for gbk in range(16):
                V(lambda e: e.match_replace(out=ss2[:, gbk, :], in_to_replace=sv[:, gbk, 0:8], in_values=ssb[:, gbk, :], imm_value=NEG),
                  [SSB[gbk], SVA[gbk]], [SS2[gbk]])
            for gbk in range(16):
                V(lambda e: e.max(out=sv[:, gbk, 8:16], in_=ss2[:, gbk, :]), [SS2[gbk]], [SVB[gbk]])
            for gbk in range(16):
                V(lambda e: e.max_index(out=si[:, gbk, 8:16], in_max=sv[:, gbk, 8:16], in_values=ss2[:, gbk, :]), [SS2[gbk], SVB[gbk]], [SIB[gbk]])
            V(lambda e: e.tensor_copy(out=sif[:], in_=si[:]), SIA + SIB, [K("sif")])
            sv4 = sv[:].rearrange("p (h two) k -> p h two k", two=2)
            sif4 = sif[:].rearrange("p (h two) k -> p h two k", two=2)
            CND = [K("cand%d" % h) for h in range(8)]
            V(lambda e: e.tensor_tensor(out=cand.rearrange("p h (a b) -> p h a b", b=16),
                                        in0=sv4[:, :, 0, :].unsqueeze(3).to_broadcast([128, 8, 16, 16]),
                                        in1=sv4[:, :, 1, :].unsqueeze(2).to_broadcast([128, 8, 16, 16]), op=ALU.add),
              SVA + SVB, SSB + CND)
            FVA = [K("fva%d" % h) for h in range(8)]; FVB = [K("fvb%d" % h) for h in range(8)]
            FIA = [K("fia%d" % h) for h in range(8)]; FIB = [K("fib%d" % h) for h in range(8)]
            C2 = [K("c2_%d" % h) for h in range(8)]
            for h in range(8):
                V(lambda e: e.max(out=fv[:, h, 0:8], in_=cand[:, h, :]), [CND[h], SSB[2 * h], SSB[2 * h + 1]], [FVA[h]])
            for h in range(8):
                V(lambda e: e.max_index(out=fi[:, h, 0:8], in_max=fv[:, h, 0:8], in_values=cand[:, h, :]), [CND[h], FVA[h], SSB[2 * h], SSB[2 * h + 1]], [FIA[h]])
            for h in range(8):
                V(lambda e: e.match_replace(out=cand2[:, h, :], in_to_replace=fv[:, h, 0:8], in_values=cand[:, h, :], imm_value=NEG),
                  [CND[h], FVA[h], SSB[2 * h], SSB[2 * h + 1]], [C2[h], SS2[2 * h], SS2[2 * h + 1]])
            for h in range(8):
                V(lambda e: e.max(out=fv[:, h, 8:16], in_=cand2[:, h, :]), [C2[h], SS2[2 * h], SS2[2 * h + 1]], [FVB[h]])
            for h in range(8):
                V(lambda e: e.max_index(out=fi[:, h, 8:16], in_max=fv[:, h, 8:16], in_values=cand2[:, h, :]), [C2[h], FVB[h], SS2[2 * h], SS2[2 * h + 1]], [FIB[h]])
            V(lambda e: e.tensor_tensor(out=ex[:], in0=fv[:], in1=fv[:, :, 0:1].to_broadcast([128, 8, 16]), op=ALU.subtract),
              FVA + FVB, [K("ex")])
            A(lambda e: e.activation(out=ex[:], in_=ex[:], func=AF.Exp), [K("ex")], [K("ex")])
            V(lambda e: e.tensor_reduce(out=zz[:], in_=ex[:], axis=AX.X, op=ALU.add), [K("ex")], [K("zz")])
            V(lambda e: e.reciprocal(out=rz[:], in_=zz[:]), [K("zz")], [K("rz")])
            V(lambda e: e.tensor_tensor(out=sel[:, 2, :].rearrange("p (h k) -> p h k", k=16), in0=ex[:],
                                        in1=rz[:].unsqueeze(2).to_broadcast([128, 8, 16]), op=ALU.mult), [K("ex"), K("rz")], [K("sel2")])
            V(lambda e: e.tensor_single_scalar(out=fa[:], in_=fi[:], scalar=4, op=ALU.logical_shift_right), FIA + FIB, [K("fa")])
            V(lambda e: e.tensor_single_scalar(out=fb[:], in_=fi[:], scalar=15, op=ALU.bitwise_and), FIA + FIB, [K("fb")])
            V(lambda e: e.tensor_copy(out=faf[:], in_=fa[:]), [K("fa")], [K("faf")])
            V(lambda e: e.tensor_copy(out=fbf[:], in_=fb[:]), [K("fb")], [K("fbf")])
            io16 = iof[:, 0:16].unsqueeze(1).unsqueeze(1).to_broadcast([128, 8, 16, 16])
            for which, (ff, fk) in enumerate(((faf, K("faf")), (fbf, K("fbf")))):
                V(lambda e: e.tensor_tensor(out=oh[:], in0=ff[:].unsqueeze(3).to_broadcast([128, 8, 16, 16]), in1=io16, op=ALU.is_equal),
                  [fk, "iof"], ["oh"])
                V(lambda e: e.tensor_tensor(out=oh[:], in0=oh[:], in1=sif4[:, :, which, :].unsqueeze(2).to_broadcast([128, 8, 16, 16]),
                                            op=ALU.mult), ["oh", K("sif")], ["oh"])
                V(lambda e: e.tensor_reduce(out=sel[:, which, :].rearrange("p (h k) -> p h k", k=16), in_=oh[:], axis=AX.X, op=ALU.add),
                  ["oh"], [K("sel%d" % which)])
            for w3 in range(3):
                S.mm(lambda e: e.transpose(out=PS[6][:, w3 * 128:(w3 + 1) * 128], in_=sel[:, w3, :], identity=idf[:]),
                     [K("sel%d" % w3), "idf"], [psk(6)], last=(w3 == 2))
            A(lambda e: e.copy(out=selT[:].rearrange("p w t -> p (w t)"), in_=PS[6][:, 0:384]), [psk(6)], [K("selT")])
            for hf in range(4):
                t0 = hf * 32
                OI, OJ = OIb[hf % 2], OJb[hf % 2]
                ki, kj = "OI%d" % (hf % 2), "OJ%d" % (hf % 2)
                V(lambda e: e.tensor_tensor(out=OI[:], in0=iob[:].unsqueeze(1).to_broadcast([128, 32, 128]),
                                            in1=selT[:, 0, t0:t0 + 32].unsqueeze(2).to_broadcast([128, 32, 128]), op=ALU.is_equal),
                  ["iob", K("selT")], [ki])
                V(lambda e: e.tensor_tensor(out=OJ[:], in0=iob[:].unsqueeze(1).to_broadcast([128, 32, 128]),
                                            in1=selT[:, 1, t0:t0 + 32].unsqueeze(2).to_broadcast([128, 32, 128]), op=ALU.is_equal),
                  ["iob", K("selT")], [kj])
                S.op("pool", lambda e: e.tensor_tensor(out=OJ[:], in0=OJ[:], in1=selT[:, 2, t0:t0 + 32].unsqueeze(2).to_broadcast([128, 32, 128]),
                                                       op=ALU.mult), [kj, K("selT")], [kj])
                for q4 in range(8):
                    bank = PS[q4 % 2]
                    for tt in range(4):
                        tl = q4 * 4 + tt
                        S.mm(lambda e: e.matmul(bank[:, tt * 128:(tt + 1) * 128], lhsT=OJ[:, tl, :], rhs=OI[:, tl, :], start=True, stop=True),
                             [ki, kj], [psk(q4 % 2)], last=(tt == 3))
                    tg = t0 + q4 * 4
                    if q4 % 2 == 0:
                        A(lambda e: e.copy(out=G[:, :, tg:tg + 4].rearrange("p i t -> p t i"),
                                           in_=bank[:].rearrange("p (t i) -> p t i", i=128)), [psk(q4 % 2)], ["G"])
                    else:
                        V(lambda e: e.tensor_copy(out=G[:, :, tg:tg + 4].rearrange("p i t -> p t i"),
                                                  in_=bank[:].rearrange("p (t i) -> p t i", i=128)), [psk(q4 % 2)], ["G"])
            S.dma("sp", G_d[t], G[:], reads=["G"])
        S.barrier()
        ph.close()

        tiles = TILES_A if l < 2 else TILES_B
        groups = []
        k0 = 0
        while k0 < len(tiles):
            gsz = 9 if (len(tiles) - k0) % 8 == 1 else 8
            groups.append(tiles[k0:k0 + gsz])
            k0 += gsz
        ph, L = phase_locals()
        hTg = L("hTg", [128, 8, 9 * 128], BF16)
        Yacc = L("Yacc", [128, 9, D])
        Gc = [L("Gc0", [128, 4, 9 * 128], BF16), L("Gc1", [128, 4, 9 * 128], BF16)]
        Ub = [L("Ub%d" % i_, [128, 8, 512], BF16) for i_ in range(3)]
        Vb = [L("Vb%d" % i_, [128, 4, D], BF16) for i_ in range(3)]
        ga = [L("ga0", [128, 512], BF16), L("ga1", [128, 512], BF16)]
        WT = L("WT", [128, 4, 512], BF16)
        cnt = [0, 0, 0]
        steps = [(gi, sc) for gi in range(len(groups)) for sc in range(32)]

        def prefetch(k):
            if k >= len(steps):
                return
            gi, sc = steps[k]
            su, sg = k % 3, k % 2
            S.dma("pool", Ub[su][:], peer_uT[l, :, sc * 512:(sc + 1) * 512].rearrange("(k p) e -> p k e", p=128), writes=["Ub%d" % su])
            S.dma("pool", Vb[su][:], peer_v[l, sc * 512:(sc + 1) * 512, :].rearrange("(c p) d -> p c d", p=128), writes=["Vb%d" % su])
            for a_, t in enumerate(groups[gi]):
                S.dma("sp", Gc[sg][:, :, a_ * 128:(a_ + 1) * 128], G_d[t][:, sc * 4:(sc + 1) * 4, :], writes=["Gc%d" % sg])

        prefetch(0)
        for gi, grp in enumerate(groups):
            ntl = len(grp)
            for a_, t in enumerate(grp):
                S.dma("sp", hTg[:, :, a_ * 128:(a_ + 1) * 128], hT_d[t], writes=["hTg"])
            V(lambda e: e.memset(Yacc[:], 0.0), [], ["Yacc"])
            for sc in range(32):
                k = cnt[0]
                cnt[0] += 1
                s_ = k % 3
                sg_ = k % 2
                prefetch(k + 1)
                for sub in range(0, ntl, 4):
                    nsub = min(4, ntl - sub)
                    N = nsub * 128
                    tok0 = sub * 128
                    for ci in range(4):
                        bi = cnt[1] % 2
                        cnt[1] += 1
                        bank = PS[2 + bi]
                        for kc in range(8):
                            S.mm(lambda e: e.matmul(bank[:, 0:N], lhsT=Ub[s_][:, kc, ci * 128:(ci + 1) * 128], rhs=hTg[:, kc, tok0:tok0 + N],
                                                    start=(kc == 0), stop=(kc == 7)), ["Ub%d" % s_, "hTg"], [psk(2 + bi)], last=(kc == 7))
                        A(lambda e: e.activation(out=ga[bi][:, 0:N], in_=bank[:, 0:N], func=AF.Gelu_apprx_tanh), [psk(2 + bi)], ["ga%d" % bi])
                        S.op("pool", lambda e: e.tensor_tensor(out=WT[:, ci, 0:N], in0=ga[bi][:, 0:N], in1=Gc[sg_][:, ci, tok0:tok0 + N], op=ALU.mult),
                             ["ga%d" % bi, "Gc%d" % sg_], ["WT%d" % ci])
                    for a_ in range(nsub):
                        yb = cnt[2] % 2
                        cnt[2] += 1
                        ybank = [PS[0], PS[1]] if yb == 0 else [PS[4], PS[5]]
                        ykey = [psk(0), psk(1)] if yb == 0 else [psk(4), psk(5)]
                        for ci in range(4):
                            for hf in range(2):
                                S.mm(lambda e: e.matmul(ybank[hf][:], lhsT=WT[:, ci, a_ * 128:(a_ + 1) * 128], rhs=Vb[s_][:, ci, hf * 512:(hf + 1) * 512],
                                                        start=(ci == 0), stop=(ci == 3)), ["WT%d" % ci, "Vb%d" % s_], [ykey[hf]],
                                     last=(ci == 3 and hf == 1))
                        ta = sub + a_
                        for hf in range(2):
                            V(lambda e: e.tensor_tensor(out=Yacc[:, ta, hf * 512:(hf + 1) * 512], in0=ybank[hf][:], in1=Yacc[:, ta, hf * 512:(hf + 1) * 512],
                                                        op=ALU.add), [ykey[hf], "Yacc"], ["Yacc"])
            for a_, t in enumerate(grp):
                g = 0 if t < SMP else 1
                Gt, gk = modtok[3 * g + 2], "modtok%d" % (3 * g + 2)
                xkey = "xt%d" % (t % 2)
                xb = load_x(t, False)
                V(lambda e: e.tensor_tensor(out=tmp[:], in0=Yacc[:, a_, :], in1=Gt[:], op=ALU.mult), ["Yacc", gk], ["tmp"])
                V(lambda e: e.tensor_tensor(out=xb[:], in0=xb[:], in1=tmp[:], op=ALU.add), ["tmp", xkey], [xkey])
                store_x(t)
        S.barrier()
        ph.close()

    def kv_phase():
        S.barrier()
        ph, L = phase_locals()
        wkv = L("wkv", [128, 8, 512], BF16)
        gkv = L("gkv", [128, D]); gk64 = L("gk64", [128, 64])
        kvf = L("kvf", [128, 512]); ksq = L("ksq", [128, 256]); kst = L("kst", [128, 8])
        cs = L("cs", [128, 8]); sn = L("sn", [128, 8])
        r1 = L("r1", [128, 4, 8]); r2 = L("r2", [128, 4, 8]); r3 = L("r3", [128, 4, 8]); r4 = L("r4", [128, 4, 8])
        S.dma("pool", wkv[:], w_kv.rearrange("(k p) n -> p k n", p=128), writes=["wkv"])
        S.dma("sp", gkv[:], kv_norm_g.partition_broadcast(128), writes=["gkv"])
        S.dma("sp", gk64[:], k_norm_g.partition_broadcast(128), writes=["gk64"])
        kvb = L("kvb", [128, 512], BF16); ktl = L("ktl", [128, 2, 128], BF16); ksr = L("ksr", [1, 256])
        onesc = L("onesc", [128, 1])
        V(lambda e: e.memset(onesc[:], 1.0), [], ["onesc"])
        for t in TILES_A:
            xkey = "xt%d" % (t % 2)
            xb = load_x(t, False)
            modnorm_T(xb, xkey, gkv[:], None, "gkv", None)
            S.dma("sp", cs[:], cosT[t], writes=["cs"]); S.dma("sp", sn[:], sinT[t], writes=["sn"])
            for kc in range(8):
                S.mm(lambda e: e.matmul(PS[0][:], lhsT=hT[:, kc, :], rhs=wkv[:, kc, :], start=(kc == 0), stop=(kc == 7)),
                     ["hT", "wkv"], [psk(0)], last=(kc == 7))
            A(lambda e: e.copy(out=kvf[:], in_=PS[0][:]), [psk(0)], ["kvf"])
            k3 = kvf[:, 0:256].rearrange("p (h d) -> p h d", d=64)
            V(lambda e: e.tensor_tensor(out=ksq[:], in0=kvf[:, 0:256], in1=kvf[:, 0:256], op=ALU.mult), ["kvf"], ["ksq"])
            V(lambda e: e.tensor_reduce(out=kst[:, 0:4], in_=ksq[:].rearrange("p (h d) -> p h d", d=64), axis=AX.X, op=ALU.add), ["ksq"], ["kst"])
            A(lambda e: e.activation(out=kst[:, 4:8], in_=kst[:, 0:4], func=AF.Sqrt, scale=1.0 / 64, bias=EPS), ["kst"], ["kst"])
            V(lambda e: e.reciprocal(out=kst[:, 0:4], in_=kst[:, 4:8]), ["kst"], ["kst"])
            V(lambda e: e.tensor_tensor(out=k3, in0=k3, in1=kst[:, 0:4].unsqueeze(2).to_broadcast([128, 4, 64]), op=ALU.mult), ["kvf", "kst"], ["kvf"])
            V(lambda e: e.tensor_tensor(out=k3, in0=k3, in1=gk64[:].unsqueeze(1).to_broadcast([128, 4, 64]), op=ALU.mult), ["kvf", "gk64"], ["kvf"])
            cb = cs[:].unsqueeze(1).to_broadcast([128, 4, 8]); snb = sn[:].unsqueeze(1).to_broadcast([128, 4, 8])
            V(lambda e: e.tensor_tensor(out=r1[:], in0=k3[:, :, 0:8], in1=cb, op=ALU.mult), ["kvf", "cs"], ["r1"])
            V(lambda e: e.tensor_tensor(out=r2[:], in0=k3[:, :, 8:16], in1=snb, op=ALU.mult), ["kvf", "sn"], ["r2"])
            V(lambda e: e.tensor_tensor(out=r3[:], in0=k3[:, :, 8:16], in1=cb, op=ALU.mult), ["kvf", "cs"], ["r3"])
            V(lambda e: e.tensor_tensor(out=r4[:], in0=k3[:, :, 0:8], in1=snb, op=ALU.mult), ["kvf", "sn"], ["r4"])
            V(lambda e: e.tensor_tensor(out=k3[:, :, 0:8], in0=r1[:], in1=r2[:], op=ALU.subtract), ["r1", "r2", "kvf"], ["kvf"])
            V(lambda e: e.tensor_tensor(out=k3[:, :, 8:16], in0=r3[:], in1=r4[:], op=ALU.add), ["r3", "r4", "kvf"], ["kvf"])
            v3 = kvf[:, 256:512].rearrange("p (h d) -> p h d", d=64)
            V(lambda e: e.tensor_copy(out=kvb[:], in_=kvf[:]), ["kvf"], ["kvb"])
            for pr in range(2):
                S.mm(lambda e: e.transpose(out=PSB[:, pr * 128:(pr + 1) * 128], in_=kvb[:, pr * 128:(pr + 1) * 128], identity=idb[:]),
                     ["kvb", "idb"], ["psT"], last=(pr == 1))
            A(lambda e: e.copy(out=ktl[:].rearrange("p a t -> p (a t)"), in_=PSB[:, 0:256]), ["psT"], ["ktl"])
            S.dma("sp", kT_d[t].rearrange("a p t -> p a t"), ktl[:], reads=["ktl"])
            S.dma("sp", v_d[t], kvb[:, 256:512], reads=["kvb"])
            S.mm(lambda e: e.matmul(PS[1][0:1, 0:256], lhsT=onesc[:], rhs=kvf[:, 0:256], start=True, stop=True), ["onesc", "kvf"], [psk(1)])
            A(lambda e: e.copy(out=ksr[:], in_=PS[1][0:1, 0:256]), [psk(1)], ["ksr"])
            S.dma("sp", ks_d[t:t + 1, :], ksr[:], reads=["ksr"])
            if t >= 16 and t < SMP:
                continue
            if t < 16:
                S.dma("sp", kp_o[t].rearrange("h t d -> t h d"), k3, reads=["kvf"])
                S.dma("sp", vp_o[t].rearrange("h t d -> t h d"), v3, reads=["kvf"])
            else:
                for s_ in range(16):
                    S.dma("sp", ks_o[s_].rearrange("h t d -> t h d"), k3[s_ * 4:(s_ + 1) * 4], reads=["kvf"])
                    S.dma("sp", vs_o[s_].rearrange("h t d -> t h d"), v3[s_ * 4:(s_ + 1) * 4], reads=["kvf"])
        S.barrier()
        ph.close()

    def cache_phase():
        S.barrier()
        ph, L = phase_locals()
        I32 = mybir.dt.int32
        ptb = L("ptb", [128, 256], I32); ptf = L("ptf", [128, 256]); iop = L("iop", [128, 1])
        idxf = L("idxf", [128, 256, 4]); idxu = L("idxu", [128, 256, 4], U32)
        kc = [L("kc0", [128, 256]), L("kc1", [128, 256])]
        vc = [L("vc0", [128, 256]), L("vc1", [128, 256])]
        kcb = L("kcb", [128, 256], BF16); vcb = L("vcb", [128, 256], BF16)
        ktl = L("ktlc", [128, 2, 128], BF16); ksr = L("ksrc", [1, 256]); onesc = L("onescc", [128, 1])
        V(lambda e: e.memset(onesc[:], 1.0), [], ["onesc"])
        S.dma("sp", ptb[:], ptab.partition_broadcast(128), writes=["ptb"])
        S.dma("sp", iop[:], iotap_in, writes=["iop"])
        V(lambda e: e.tensor_copy(out=ptf[:], in_=ptb[:]), ["ptb"], ["ptf"])
        for kvh in range(4):
            V(lambda e: e.tensor_scalar(out=idxf[:, :, kvh], in0=ptf[:], scalar1=512.0, scalar2=float(kvh * 128), op0=ALU.mult, op1=ALU.add),
              ["ptf"], ["idxf"])
        V(lambda e: e.tensor_scalar(out=idxf[:], in0=idxf[:], scalar1=iop[:, 0:1], scalar2=None, op0=ALU.add), ["idxf", "iop"], ["idxf"])
        V(lambda e: e.tensor_copy(out=idxu[:], in_=idxf[:]), ["idxf"], ["idxu"])
        S.barrier()
        n = 0
        for sq_ in range(16):
            for pg in range(16):
                col = sq_ * 16 + pg
                sl = n % 2
                for kvh in range(4):
                    S.idma(kc[sl][:, kvh * 64:(kvh + 1) * 64], cache_k, idxu[:, col, kvh:kvh + 1], ["idxu"], ["kc%d" % sl])
                    S.idma(vc[sl][:, kvh * 64:(kvh + 1) * 64], cache_v, idxu[:, col, kvh:kvh + 1], ["idxu"], ["vc%d" % sl])
                V(lambda e: e.tensor_copy(out=kcb[:], in_=kc[sl][:]), ["kc%d" % sl], ["kcb"])
                A(lambda e: e.copy(out=vcb[:], in_=vc[sl][:]), ["vc%d" % sl], ["vcb"])
                for pr in range(2):
                    S.mm(lambda e: e.transpose(out=PSB[:, pr * 128:(pr + 1) * 128], in_=kcb[:, pr * 128:(pr + 1) * 128], identity=idb[:]),
                         ["kcb", "idb"], ["psT"], last=(pr == 1))
                A(lambda e: e.copy(out=ktl[:].rearrange("p a t -> p (a t)"), in_=PSB[:, 0:256]), ["psT"], ["ktl"])
                S.dma("sp", kTs_d[sq_, pg].rearrange("a p t -> p a t"), ktl[:], reads=["ktl"])
                S.dma("sp", vS_d[sq_, pg], vcb[:], reads=["vcb"])
                S.mm(lambda e: e.matmul(PS[1][0:1, 0:256], lhsT=onesc[:], rhs=kc[sl][:], start=True, stop=True), ["onesc", "kc%d" % sl], [psk(1)])
                A(lambda e: e.copy(out=ksr[:], in_=PS[1][0:1, 0:256]), [psk(1)], ["ksr"])
                S.dma("sp", ksS_d[sq_, pg:pg + 1, :], ksr[:], reads=["ksr"])
                n += 1
        S.barrier()
        ph.close()

    def attn_phase(l):
        j = l - 2
        S.barrier()
        ph, L = phase_locals()
        wq = L("awq", [128, 8, D], BF16); wo = L("awo", [128, 8, D], BF16)
        KT = L("KT", [128, 4, 32, 128], BF16)
        VV = L("VV", [128, 32, 256], BF16)
        KM = L("KM", [128, 4, 32], BF16)
        kss = L("kss", [32, 256]); ks2 = L("ks2", [32, 4, 2, 64])
        SelP = L("SelP", [32, 16]); SelS = L("SelS", [16, 8])
        gq64 = L("gq64", [128, 64]); mOwn = L("mOwn", [128, 256]); mOwnS = L("mOwnS", [128, 128]); rowm = L("rowm", [128, 16])
        qf = L("qf", [128, D]); qb = L("qb", [128, D], BF16); qT = L("qTa", [128, 8, 128], BF16)
        qst = L("qst", [128, 48])
        cs = L("acs", [128, 8]); sn = L("asn", [128, 8])
        r1 = L("ar1", [128, 16, 8]); r2 = L("ar2", [128, 16, 8]); r3 = L("ar3", [128, 16, 8]); r4 = L("ar4", [128, 16, 8])
        gate = L("gate", [128, 16, 16]); gm = L("gm", [128, 16, 16]); m8 = L("m8", [128, 16, 8]); mb = L("mb", [128, 16, 16])
        P = L("P", [128, 4096], BF16); PT = L("PT", [128, 32, 128], BF16)
        so = L("so", [128, 256])
        den = L("den", [128, 16, 136]); dsum = L("dsum", [128, 16]); drec = L("drec", [128, 16])
        Osb = L("Osb", [128, D]); ob = L("ob", [128, D], BF16); oT = L("oT", [128, 8, 128], BF16)
        S.dma("pool", wq[:], b_w_q[j].rearrange("(k p) n -> p k n", p=128), writes=["awq"])
        S.dma("pool", wo[:], b_w_o[j].rearrange("(k p) n -> p k n", p=128), writes=["awo"])
        S.dma("sp", gq64[:], b_q_norm_g[j].partition_broadcast(128), writes=["gq64"])
        S.dma("sp", mOwn[:], maskOwn_in, writes=["mOwn"]); S.dma("sp", mOwnS[:], maskOwnS_in, writes=["mOwnS"])
        S.dma("sp", rowm[:], rowmask_in, writes=["rowm"])
        S.dma("sp", SelP[:], SelP_in, writes=["SelP"]); S.dma("sp", SelS[:], SelS_in, writes=["SelS"])
        S.barrier()

        def load_ctx(kT_src, v_src, ks_src, nslots, interleave, Sel, nblk):
            parts = [(0, 16, 0, 2), (16, 32, 1, 2)] if interleave else [(0, nslots, 0, 1)]
            for (s0, s1, k0, kstep) in parts:
                nk = s1 - s0
                for kvh in range(4):
                    for base in (0, 64):
                        S.dma("sp", KT[base:base + 64, kvh, k0:k0 + kstep * (nk - 1) + 1:kstep, :],
                              kT_src[s0:s1, kvh // 2, (kvh % 2) * 64:(kvh % 2) * 64 + 64, :].rearrange("s d t -> d s t"),
                              reads=["kscr"], writes=["KT"])
                S.dma("sp", VV[:, k0:k0 + kstep * (nk - 1) + 1:kstep, :], v_src[s0:s1].rearrange("s k c -> k s c"), reads=["kscr"], writes=["VV"])
            S.dma("sp", kss[0:nslots, :], ks_src, reads=["kscr"], writes=["kss"])
            k3s = kss[0:nslots, :].rearrange("p (h d) -> p h d", d=64)
            V(lambda e: e.tensor_copy(out=ks2[0:nslots, :, 0, :], in_=k3s), ["kss"], ["ks2"])
            V(lambda e: e.tensor_copy(out=ks2[0:nslots, :, 1, :], in_=k3s), ["kss"], ["ks2"])
            V(lambda e: e.memset(KM[:], 0.0), [], ["KM"])
            for kvh in range(4):
                S.mm(lambda e: e.matmul(PS[2][:, 0:nblk], lhsT=ks2[0:nslots, kvh, :, :].rearrange("p a d -> p (a d)"), rhs=Sel[0:nslots, 0:nblk],
                                        start=True, stop=True), ["ks2", "SelP", "SelS"], [psk(2)])
                A(lambda e: e.copy(out=KM[0:64, kvh, 0:nblk], in_=PS[2][0:64, 0:nblk]), [psk(2)], ["KM"])
                A(lambda e: e.copy(out=KM[64:128, kvh, 16:16 + nblk], in_=PS[2][64:128, 0:nblk]), [psk(2)], ["KM"])

        def compute_q(t):
            for hf in range(2):
                for kc_ in range(8):
                    S.mm(lambda e: e.matmul(PS[hf][:], lhsT=hT[:, kc_, :], rhs=wq[:, kc_, hf * 512:(hf + 1) * 512], start=(kc_ == 0), stop=(kc_ == 7)),
                         ["hT", "awq"], [psk(hf)], last=(kc_ == 7))
                A(lambda e: e.copy(out=qf[:, hf * 512:(hf + 1) * 512], in_=PS[hf][:]), [psk(hf)], ["qf"])
            q3 = qf[:].rearrange("p (h d) -> p h d", d=64)
            V(lambda e: e.tensor_tensor(out=tmp[:], in0=qf[:], in1=qf[:], op=ALU.mult), ["qf"], ["tmp"])
            V(lambda e: e.tensor_reduce(out=qst[:, 0:16], in_=tmp[:].rearrange("p (h d) -> p h d", d=64), axis=AX.X, op=ALU.add), ["tmp"], ["qst"])
            A(lambda e: e.activation(out=qst[:, 16:32], in_=qst[:, 0:16], func=AF.Sqrt, scale=1.0 / 64, bias=EPS), ["qst"], ["qst"])
            V(lambda e: e.reciprocal(out=qst[:, 32:48], in_=qst[:, 16:32]), ["qst"], ["qst"])
            V(lambda e: e.tensor_tensor(out=q3, in0=q3, in1=qst[:, 32:48].unsqueeze(2).to_broadcast([128, 16, 64]), op=ALU.mult), ["qf", "qst"], ["qf"])
            V(lambda e: e.tensor_tensor(out=q3, in0=q3, in1=gq64[:].unsqueeze(1).to_broadcast([128, 16, 64]), op=ALU.mult), ["qf", "gq64"], ["qf"])
            S.dma("sp", cs[:], cosT[t], writes=["acs"]); S.dma("sp", sn[:], sinT[t], writes=["asn"])
            cb = cs[:].unsqueeze(1).to_broadcast([128, 16, 8]); snb = sn[:].unsqueeze(1).to_broadcast([128, 16, 8])
            V(lambda e: e.tensor_tensor(out=r1[:], in0=q3[:, :, 0:8], in1=cb, op=ALU.mult), ["qf", "acs"], ["ar1"])
            V(lambda e: e.tensor_tensor(out=r2[:], in0=q3[:, :, 8:16], in1=snb, op=ALU.mult), ["qf", "asn"], ["ar2"])
            V(lambda e: e.tensor_tensor(out=r3[:], in0=q3[:, :, 8:16], in1=cb, op=ALU.mult), ["qf", "acs"], ["ar3"])
            V(lambda e: e.tensor_tensor(out=r4[:], in0=q3[:, :, 0:8], in1=snb, op=ALU.mult), ["qf", "asn"], ["ar4"])
            V(lambda e: e.tensor_tensor(out=q3[:, :, 0:8], in0=r1[:], in1=r2[:], op=ALU.subtract), ["ar1", "ar2", "qf"], ["qf"])
            V(lambda e: e.tensor_tensor(out=q3[:, :, 8:16], in0=r3[:], in1=r4[:], op=ALU.add), ["ar3", "ar4", "qf"], ["qf"])
            V(lambda e: e.tensor_copy(out=qb[:], in_=qf[:]), ["qf"], ["qb"])
            for k in range(8):
                S.mm(lambda e: e.transpose(out=PSB[:, k * 128:(k + 1) * 128], in_=qb[:, k * 128:(k + 1) * 128], identity=idb[:]),
                     ["qb", "idb"], ["psT"], last=(k == 7))
            A(lambda e: e.copy(out=qT[:].rearrange("p k t -> p (k t)"), in_=PSB[:]), ["psT"], ["qTa"])

        def gates(nblk, nvalid, rowbias):
            for pr in range(8):
                S.mm(lambda e: e.matmul(PS[2][:, pr * 32:pr * 32 + 32], lhsT=qT[:, pr, :], rhs=KM[:, pr // 2, :], start=True, stop=True),
                     ["qTa", "KM"], [psk(2)], last=(pr == 7))
            V(lambda e: e.memset(gm[:], NEG), [], ["gm"])
            V(lambda e: e.tensor_copy(out=gm[:, :, 0:nvalid],
                                      in_=PS[2][:, 0:256].rearrange("p (pr two n) -> p (pr two) n", two=2, n=16)[:, :, 0:nvalid]),
              [psk(2)], ["gm"])
            for h in range(16):
                V(lambda e: e.max(out=m8[:, h, :], in_=gm[:, h, :]), ["gm"], ["m8"])
            V(lambda e: e.tensor_tensor(out=mb[:, :, 0:nvalid], in0=gm[:, :, 0:nvalid], in1=m8[:, :, 2:3].to_broadcast([128, 16, nvalid]), op=ALU.is_ge),
              ["gm", "m8"], ["mb"])
            V(lambda e: e.tensor_scalar(out=mb[:, :, 0:nvalid], in0=mb[:, :, 0:nvalid], scalar1=-1.0, scalar2=-MNEG, op0=ALU.add, op1=ALU.mult),
              ["mb"], ["mb"])
            if rowbias is not None:
                V(lambda e: e.tensor_scalar(out=mb[:, :, 0:nvalid], in0=mb[:, :, 0:nvalid], scalar1=rowbias, scalar2=None, op0=ALU.add), ["mb", "rowm"], ["mb"])

        def attend_head(h, blocks, dcol0, accumulate):
            pr, base, kvh = h // 2, (h % 2) * 64, h // 4
            ntile = 0
            bi = 0
            for (kt0, nkt, bias, emask) in blocks:
                bank = PS[2 + bi % 2]
                S.mm(lambda e: e.matmul(bank[:, 0:nkt * 128], lhsT=qT[base:base + 64, pr, :],
                                        rhs=KT[base:base + 64, kvh, kt0:kt0 + nkt, :].rearrange("p a k -> p (a k)"), start=True, stop=True),
                     ["qTa", "KT"], [psk(2 + bi % 2)])
                pdst = P[:, ntile * 128:(ntile + nkt) * 128]
                dcol = den[:, h, dcol0 + bi:dcol0 + bi + 1]
                if emask is not None:
                    V(lambda e: e.tensor_tensor(out=so[:, 0:nkt * 128], in0=bank[:, 0:nkt * 128], in1=emask, op=ALU.add), [psk(2 + bi % 2), "mOwn", "mOwnS"], ["so"])
                    A(lambda e: e.activation(out=pdst, in_=so[:, 0:nkt * 128], func=AF.Exp, scale=0.125, accum_out=dcol), ["so"], ["P", "den"])
                else:
                    A(lambda e: e.activation(out=pdst, in_=bank[:, 0:nkt * 128], func=AF.Exp, scale=0.125, bias=bias, accum_out=dcol),
                      [psk(2 + bi % 2), "mb"], ["P", "den"])
                ntile += nkt
                bi += 1
            for kt in range(ntile):
                S.mm(lambda e: e.transpose(out=PSB[:, (kt % 8) * 128:(kt % 8 + 1) * 128], in_=P[:, kt * 128:(kt + 1) * 128], identity=idb[:]),
                     ["P", "idb"], ["psT"], last=(kt % 8 == 7 or kt == ntile - 1))
                if kt % 8 == 7 or kt == ntile - 1:
                    k0 = (kt // 8) * 8
                    nk = kt - k0 + 1
                    V(lambda e: e.tensor_copy(out=PT[:, k0:k0 + nk, :].rearrange("p a t -> p (a t)"), in_=PSB[:, 0:nk * 128]), ["psT"], ["PT"])
            kti = 0
            for (kt0, nkt, bias, emask) in blocks:
                for a_ in range(nkt):
                    S.mm(lambda e: e.matmul(PS[6][:, 0:64], lhsT=PT[:, kti, :], rhs=VV[:, kt0 + a_, kvh * 64:(kvh + 1) * 64],
                                            start=(kti == 0), stop=(kti == ntile - 1)), ["PT", "VV"], [psk(6)], last=(kti == ntile - 1))
                    kti += 1
            if accumulate:
                V(lambda e: e.tensor_tensor(out=Osb[:, h * 64:(h + 1) * 64], in0=PS[6][:, 0:64], in1=Osb[:, h * 64:(h + 1) * 64], op=ALU.add),
                  [psk(6), "Osb"], ["Osb"])
            else:
                V(lambda e: e.tensor_copy(out=Osb[:, h * 64:(h + 1) * 64], in_=PS[6][:, 0:64]), [psk(6)], ["Osb"])

        def finish_tile(xb, xkey, Gt, gk):
            V(lambda e: e.tensor_reduce(out=dsum[:], in_=den[:], axis=AX.X, op=ALU.add), ["den"], ["dsum"])
            V(lambda e: e.tensor_scalar(out=dsum[:], in0=dsum[:], scalar1=1e-30, scalar2=None, op0=ALU.add), ["dsum"], ["dsum"])
            V(lambda e: e.reciprocal(out=drec[:], in_=dsum[:]), ["dsum"], ["drec"])
            V(lambda e: e.tensor_tensor(out=ob[:].rearrange("p (h d) -> p h d", d=64), in0=Osb[:].rearrange("p (h d) -> p h d", d=64),
                                        in1=drec[:].unsqueeze(2).to_broadcast([128, 16, 64]), op=ALU.mult), ["Osb", "drec"], ["ob"])
            for k in range(8):
                S.mm(lambda e: e.transpose(out=PSB[:, k * 128:(k + 1) * 128], in_=ob[:, k * 128:(k + 1) * 128], identity=idb[:]),
                     ["ob", "idb"], ["psT"], last=(k == 7))
            A(lambda e: e.copy(out=oT[:].rearrange("p k t -> p (k t)"), in_=PSB[:]), ["psT"], ["oT"])
            for hf in range(2):
                for c in range(8):
                    S.mm(lambda e: e.matmul(PS[4 + hf][:], lhsT=oT[:, c, :], rhs=wo[:, c, hf * 512:(hf + 1) * 512], start=(c == 0), stop=(c == 7)),
                         ["oT", "awo"], [psk(4 + hf)], last=(c == 7))
            resid_add(xb, xkey, [PS[4], PS[5]], [psk(4), psk(5)], Gt, gk)

        load_ctx(kT_d, v_d, ks_d[0:32, :], 32, True, SelP, 16)
        for i in range(16):
            xkey = "xt%d" % (i % 2)
            xb = load_x(i, False)
            modnorm_T(xb, xkey, modtok[0][:], modtok[1][:], "modtok0", "modtok1")
            compute_q(i)
            V(lambda e: e.memset(den[:], 0.0), [], ["den"])
            if i > 0:
                gates(16, i, None)
            for h in range(16):
                blocks = []
                n = 0
                while n < i:
                    if n + 1 < i:
                        blocks.append((2 * n, 2, mb[:, h, n:n + 1], None))
                        blocks.append((2 * n + 2, 2, mb[:, h, n + 1:n + 2], None))
                        n += 2
                    else:
                        blocks.append((2 * n, 2, mb[:, h, n:n + 1], None))
                        n += 1
                blocks.append((2 * i, 2, None, mOwn[:, 0:256]))
                attend_head(h, blocks, 0, False)
            finish_tile(xb, xkey, modtok[2], "modtok2")
            store_x(i)
        xkey = "xt%d" % (SMP % 2)
        xb = load_x(SMP, False)
        modnorm_T(xb, xkey, modtok[3][:], modtok[4][:], "modtok3", "modtok4")
        compute_q(SMP)
        V(lambda e: e.memset(den[:], 0.0), [], ["den"])
        for kvh in range(4):
            for base in (0, 64):
                S.dma("sp", KT[base:base + 64, kvh, 0, :], kT_d[SMP, kvh // 2, (kvh % 2) * 64:(kvh % 2) * 64 + 64, :], writes=["KT"])
        S.dma("sp", VV[:, 0, :], v_d[SMP], writes=["VV"])
        for h in range(16):
            attend_head(h, [(0, 1, None, mOwnS[:, 0:128])], 0, False)
        for sq_ in range(16):
            load_ctx(kTs_d[sq_], vS_d[sq_], ksS_d[sq_], 16, False, SelS, 8)
            gates(8, 8, rowm[:, sq_:sq_ + 1])
            for h in range(16):
                blocks = [(2 * n, 2, mb[:, h, n:n + 1], None) for n in range(8)]
                attend_head(h, blocks, 1 + sq_ * 8, True)
        finish_tile(xb, xkey, modtok[5], "modtok5")
        store_x(SMP)
        S.barrier()
        ph.close()

    first = True
    for l in range(n_layers):
        adaln(l, 0)
        if l < 2:
            gmlp_phase(l, first)
            first = False
        elif do_attn:
            attn_phase(l)
        if do_peer:
            adaln(l, 1)
            peer_phase(l)
        if l == 1:
            kv_phase()
            if do_attn and n_layers > 2:
                cache_phase()
    S.barrier()
    for t in TILES_B:
        xb = load_x(t, False)
        S.dma("sp", y_p[t] if t < SMP else y_s, xb[:], reads=["xt%d" % (t % 2)])
    S.finish()
    return nc, S


def _host_inputs(inp):
    f = np.float32
    c = lambda a: np.ascontiguousarray(a, dtype=f)
    x_prompt = inp["x_prompt"]; x_sample = inp["x_sample"]
    a_w_s = np.asarray(inp["a_w_s"], dtype=f); a_b_s = np.asarray(inp["a_b_s"], dtype=f)
    shared = {}
    for k in ("ada_w", "ada_b", "norm1_g", "norm2_g", "a_w_in", "a_b_in", "a_g_sgu", "a_w_out", "kv_norm_g", "w_kv",
              "k_norm_g", "peer_w_q", "peer_v", "b_w_q", "b_q_norm_g", "b_w_o"):
        shared[k] = c(inp[k])
    shared["cache_k"] = c(inp["cache_k"]).reshape(-1, 64)
    shared["cache_v"] = c(inp["cache_v"]).reshape(-1, 64)
    shared["peer_uT"] = c(np.transpose(inp["peer_u"], (0, 2, 1)))
    shared["skT"] = c(np.transpose(inp["peer_sub_keys"], (0, 1, 3, 2)))
    shared["wsTp"] = c(np.transpose(a_w_s, (0, 3, 1, 2)))
    wsTs = np.zeros((2, 128, 8, 128), f)
    bsS = np.zeros((2, 8, 128), f)
    for q in range(16):
        wsTs[:, q * 4:(q + 1) * 4, :, q * 4:(q + 1) * 4] = np.transpose(a_w_s[:, :, :4, :4], (0, 3, 1, 2))
        bsS[:, :, q * 4:(q + 1) * 4] = a_b_s[:, :, :4]
    shared["wsTs"] = wsTs
    shared["bsP"] = c(a_b_s)
    shared["bsS"] = bsS
    s_idx = np.arange(128)[:, None]; t_idx = np.arange(128)[None, :]
    shared["maskP"] = (s_idx <= t_idx).astype(f)
    shared["maskS"] = ((s_idx // 4 == t_idx // 4) & (s_idx % 4 <= t_idx % 4) & (s_idx < 64)).astype(f)
    shared["ident"] = np.eye(128, dtype=f)
    shared["iota"] = np.tile(np.arange(128, dtype=f)[None, :], (128, 1))
    shared["iotap"] = np.arange(128, dtype=f)[:, None].copy()
    Ep = np.zeros((32, 128), f); Ep[0, :] = 1.0
    Es = np.zeros((32, 128), f)
    for t in range(64):
        Es[1 + t // 4, t] = 1.0
    shared["Ep"] = Ep; shared["Es"] = Es
    SelP = np.zeros((32, 16), f)
    for sl in range(32):
        SelP[sl, sl % 16] = 1.0 / 256
    SelS = np.zeros((16, 8), f)
    for pg in range(16):
        SelS[pg, pg // 2] = 1.0 / 256
    shared["SelP"] = SelP; shared["SelS"] = SelS
    tq = np.arange(128)[:, None]; kk = np.arange(128)[None, :]
    shared["maskOwnS"] = np.where((kk // 4 == tq // 4) & (kk % 4 <= tq % 4) & (tq < 64) & (kk < 64), 0.0, MNEG).astype(f)
    rowmask = np.full((128, 16), MNEG, f)
    for t in range(64):
        rowmask[t, t // 4] = 0.0
    shared["rowmask"] = rowmask
    half = 8
    inv = (500000.0 ** (-np.arange(half, dtype=np.float64) / half)).astype(f)
    maps = []
    for core in range(8):
        b, p = core // 2, core % 2
        m = dict(shared)
        xt32 = x_prompt[b].reshape(32, 128, D)
        m["xp"] = c(np.concatenate([xt32[p::2], xt32[(1 - p)::2]], axis=0))
        xs = np.zeros((128, D), f); xs[:64] = x_sample[16 * core:16 * core + 16].reshape(64, D)
        m["xs"] = xs
        cT = np.zeros((D, 32), f); cT[:, 0] = inp["c_prompt"][b]; cT[:, 1:17] = inp["c_sample"][16 * core:16 * core + 16].T
        m["cT"] = cT
        m["ptab"] = np.ascontiguousarray(inp["page_table"][16 * core:16 * core + 16], dtype=np.int32).reshape(256)
        pos = np.zeros((NS, 128), f)
        for i in range(16):
            pos[i] = (2 * i + p) * 128 + np.arange(128)
            pos[16 + i] = (2 * i + 1 - p) * 128 + np.arange(128)
        pos[SMP, :64] = 2048 + (np.arange(64) % 4)
        ang = pos[:, :, None].astype(f) * inv[None, None, :]
        m["cosT"] = np.cos(ang).astype(f); m["sinT"] = np.sin(ang).astype(f)
        mo = np.zeros((128, 256), f)
        mo[:, 0:128] = np.where(kk <= tq, 0.0, MNEG)
        mo[:, 128:256] = 0.0 if p == 1 else MNEG
        m["maskOwn"] = mo
        maps.append(m)
    return maps


_CACHE = {}


def kernel(**inputs):
    inp = {k: np.asarray(v) for k, v in inputs.items()}
    maps = _host_inputs(inp)
    if "nc" not in _CACHE:
        _CACHE["nc"] = build_program()[0]
    nc = _CACHE["nc"]
    res = run_bass_kernel_spmd(nc, maps, core_ids=list(range(8)))
    R = res.results
    y_prompt = np.zeros((4, 4096, D), np.float32); y_sample = np.zeros((128, 4, D), np.float32)
    k_prompt = np.zeros((4, 32, 4, 128, 64), np.float32); v_prompt = np.zeros_like(k_prompt)
    k_sample = np.zeros((128, 4, 4, 64), np.float32); v_sample = np.zeros_like(k_sample)
    a_v = np.zeros((2, 128, 4, 2048), np.float32)
    for core in range(8):
        b, p = core // 2, core % 2
        r = R[core]
        y_prompt[b].reshape(32, 128, D)[p::2] = r["y_p"]
        y_sample[16 * core:16 * core + 16] = r["y_s"][:64].reshape(16, 4, D)
        k_prompt[b, p::2] = r["kp_o"]; v_prompt[b, p::2] = r["vp_o"]
        k_sample[16 * core:16 * core + 16] = r["ks_o"]; v_sample[16 * core:16 * core + 16] = r["vs_o"]
        a_v[:, 16 * core:16 * core + 16] = r["av_o"].reshape(2, 16, 4, 2048)
    return (y_prompt, y_sample, k_prompt, v_prompt, k_sample, v_sample, a_v)
```

```python
import numpy as np
from contextlib import ExitStack
import concourse.bass as bass
import concourse.mybir as mybir
from concourse.bass_utils import run_bass_kernel_spmd

F32 = mybir.dt.float32
BF16 = mybir.dt.bfloat16
U32 = mybir.dt.uint32
AF = mybir.ActivationFunctionType
ALU = mybir.AluOpType
AX = mybir.AxisListType

NS = 33
SMP = 32
TILES_A = list(range(33))
TILES_B = list(range(16)) + [32]
MNEG = -30000.0
D = 1024
EPS = 1e-6
NEG = -1.0e30


class Sched:
    COMPUTE = ("pe", "act", "dve", "pool")

    def __init__(self, nc, n_dma_sems=8):
        self.nc = nc
        self.eng = {"pe": nc.tensor, "act": nc.scalar, "dve": nc.vector, "pool": nc.gpsimd, "sp": nc.sync}
        self.sem = {}
        self.cnt = {}
        for e in self.COMPUTE:
            self.sem[e] = nc.alloc_semaphore("sem_" + e)
            self.cnt[e] = 0
        self.dsem = {}
        self.dcnt = {}
        self.drot = {}
        for q in ("sp", "pool"):
            self.dsem[q] = [nc.alloc_semaphore("dsem_%s_%d" % (q, i)) for i in range(n_dma_sems)]
            self.dcnt[q] = [0] * n_dma_sems
            self.drot[q] = 0
        self.seen = {e: {} for e in self.eng}
        self.regions = {}
        self.n_instr = 0
        self.n_wait = 0
        self._pend_r = []
        self._pend_w = []

    def _semh(self, key):
        return self.sem[key[1]] if key[0] == "c" else self.dsem[key[1]][key[2]]

    def _wait(self, eng, deps):
        best = {}
        for k, v in deps:
            if v > best.get(k, 0):
                best[k] = v
        for k, v in best.items():
            if eng == "pe" and k == ("c", "pe"):
                continue
            if self.seen[eng].get(k, 0) >= v:
                continue
            if eng in ("dve", "act") and k == ("c", eng) and self.cnt[eng] - v >= 3:
                continue
            self.eng[eng].wait_ge(self._semh(k), v)
            self.seen[eng][k] = v
            self.n_wait += 1

    def _deps(self, reads, writes):
        deps = []
        for r in reads:
            reg = self.regions.get(r)
            if reg is None:
                continue
            deps += reg[0]
            if reg[2]:
                deps += reg[1]
        for w in writes:
            reg = self.regions.get(w)
            if reg is None:
                continue
            deps += reg[0]
            deps += reg[1]
        return deps

    def _record(self, dep, reads, writes):
        for r in reads:
            reg = self.regions.setdefault(r, [[], [], isinstance(r, str) and r.startswith("ps")])
            if reg[2]:
                reg[0] = [dep]
                reg[1] = []
            else:
                reg[1] = [d for d in reg[1] if d[0] != dep[0]] + [dep]
        for w in writes:
            reg = self.regions.setdefault(w, [[], [], isinstance(w, str) and w.startswith("ps")])
            reg[0] = [dep]
            reg[1] = []

    def op(self, eng, fn, reads=(), writes=()):
        reads = list(reads)
        writes = list(writes)
        self._wait(eng, self._deps(reads, writes))
        ins = fn(self.eng[eng])
        self.cnt[eng] += 1
        ins.then_inc(self.sem[eng], 1)
        self._record((("c", eng), self.cnt[eng]), reads, writes)
        self.n_instr += 1
        return ins

    def mm(self, fn, reads=(), writes=(), last=True):
        reads = list(reads)
        writes = list(writes)
        self._wait("pe", self._deps(reads, writes))
        ins = fn(self.eng["pe"])
        self.n_instr += 1
        if last:
            self.cnt["pe"] += 1
            ins.then_inc(self.sem["pe"], 1)
            self._record((("c", "pe"), self.cnt["pe"]), reads + self._pend_r, writes + self._pend_w)
            self._pend_r = []
            self._pend_w = []
        else:
            self._pend_r += reads
            self._pend_w += writes
        return ins

    def dma(self, q, out, in_, reads=(), writes=(), **kw):
        reads = list(reads)
        writes = list(writes)
        i = self.drot[q]
        self.drot[q] = (i + 1) % len(self.dsem[q])
        key = ("d", q, i)
        deps = self._deps(reads, writes)
        if self.dcnt[q][i] > 0:
            deps.append((key, self.dcnt[q][i]))
        self._wait(q, deps)
        ins = self.eng[q].dma_start(out=out, in_=in_, **kw)
        self.dcnt[q][i] += 16
        ins.then_inc(self.dsem[q][i], 16)
        self._record((key, self.dcnt[q][i]), reads, writes)
        self.n_instr += 1

    def idma(self, out, in_, idx_ap, reads=(), writes=()):
        q = "pool"
        reads = list(reads)
        writes = list(writes)
        i = self.drot[q]
        self.drot[q] = (i + 1) % len(self.dsem[q])
        key = ("d", q, i)
        deps = self._deps(reads, writes)
        if self.dcnt[q][i] > 0:
            deps.append((key, self.dcnt[q][i]))
        self._wait(q, deps)
        ins = self.eng[q].indirect_dma_start(out=out, out_offset=None, in_=in_,
                                             in_offset=bass.IndirectOffsetOnAxis(ap=idx_ap, axis=0))
        self.dcnt[q][i] += 16
        ins.then_inc(self.dsem[q][i], 16)
        self._record((key, self.dcnt[q][i]), reads, writes)
        self.n_instr += 1

    def _all(self):
        deps = [(("c", e), self.cnt[e]) for e in self.COMPUTE if self.cnt[e] > 0]
        for q in self.dsem:
            for i, c in enumerate(self.dcnt[q]):
                if c > 0:
                    deps.append((("d", q, i), c))
        return deps

    def barrier(self):
        deps = self._all()
        for e in self.eng:
            self._wait(e, deps)
        self.regions = {}

    finish = barrier


def build_program(n_layers=4, do_peer=True, do_attn=True):
    nc = bass.Bass("TRN2", target_bir_lowering=False)

    def din(name, shape, dt=F32):
        return nc.dram_tensor(name, list(shape), dt, kind="ExternalInput").ap()

    def dout(name, shape, dt=F32):
        return nc.dram_tensor(name, list(shape), dt, kind="ExternalOutput").ap()

    xp = din("xp", [32, 128, D]); xs = din("xs", [128, D]); cT = din("cT", [D, 32])
    Ep = din("Ep", [32, 128]); Es = din("Es", [32, 128])
    ident = din("ident", [128, 128]); iota_in = din("iota", [128, 128])
    ada_w = din("ada_w", [4, D, 6 * D]); ada_b = din("ada_b", [4, 6 * D])
    norm1_g = din("norm1_g", [4, D]); norm2_g = din("norm2_g", [4, D])
    a_w_in = din("a_w_in", [2, D, 4096]); a_b_in = din("a_b_in", [2, 4096]); a_g_sgu = din("a_g_sgu", [2, 2048])
    wsTp = din("wsTp", [2, 128, 8, 128]); wsTs = din("wsTs", [2, 128, 8, 128])
    bsP = din("bsP", [2, 8, 128]); bsS = din("bsS", [2, 8, 128])
    maskP = din("maskP", [128, 128]); maskS = din("maskS", [128, 128])
    a_w_out = din("a_w_out", [2, 2048, D])
    kv_norm_g = din("kv_norm_g", [D]); w_kv = din("w_kv", [D, 512]); k_norm_g = din("k_norm_g", [64])
    peer_w_q = din("peer_w_q", [4, D, 2048]); skT = din("skT", [4, 2, 128, 128])
    peer_uT = din("peer_uT", [4, D, 16384]); peer_v = din("peer_v", [4, 16384, D])
    cosT = din("cosT", [NS, 128, 8]); sinT = din("sinT", [NS, 128, 8])
    b_w_q = din("b_w_q", [2, D, D]); b_q_norm_g = din("b_q_norm_g", [2, 64]); b_w_o = din("b_w_o", [2, D, D])
    cache_k = din("cache_k", [2560 * 512, 64]); cache_v = din("cache_v", [2560 * 512, 64])
    ptab = din("ptab", [256], mybir.dt.int32)
    iotap_in = din("iotap", [128, 1]); SelP_in = din("SelP", [32, 16]); SelS_in = din("SelS", [16, 8])
    maskOwn_in = din("maskOwn", [128, 256]); maskOwnS_in = din("maskOwnS", [128, 128]); rowmask_in = din("rowmask", [128, 16])

    y_p = dout("y_p", [16, 128, D]); y_s = dout("y_s", [128, D])
    kp_o = dout("kp_o", [16, 4, 128, 64]); vp_o = dout("vp_o", [16, 4, 128, 64])
    ks_o = dout("ks_o", [16, 4, 4, 64]); vs_o = dout("vs_o", [16, 4, 4, 64])
    av_o = dout("av_o", [2, 64, 2048])

    xd = nc.dram_tensor("xd", [NS, 128, D], F32, kind="Internal").ap()
    kT_d = nc.dram_tensor("kT_d", [NS, 2, 128, 128], BF16, kind="Internal").ap()
    v_d = nc.dram_tensor("v_d", [NS, 128, 256], BF16, kind="Internal").ap()
    ks_d = nc.dram_tensor("ks_d", [NS, 256], F32, kind="Internal").ap()
    kTs_d = nc.dram_tensor("kTs_d", [16, 16, 2, 128, 128], BF16, kind="Internal").ap()
    vS_d = nc.dram_tensor("vS_d", [16, 16, 128, 256], BF16, kind="Internal").ap()
    ksS_d = nc.dram_tensor("ksS_d", [16, 16, 256], F32, kind="Internal").ap()
    hT_d = nc.dram_tensor("hT_d", [NS, 128, 8, 128], BF16, kind="Internal").ap()
    G_d = nc.dram_tensor("G_d", [NS, 128, 128, 128], BF16, kind="Internal").ap()

    S = Sched(nc)
    V = lambda fn, r=(), w=(): S.op("dve", fn, r, w)
    A = lambda fn, r=(), w=(): S.op("act", fn, r, w)

    def sb(name, shape, dt=F32):
        return nc.alloc_sbuf_tensor(name, list(shape), dt)

    PS = [nc.alloc_psum_tensor("psb%d" % i, [128, 512], F32) for i in range(7)]
    PSB = nc.alloc_psum_tensor("psbf", [128, 1024], BF16)
    psk = lambda i: "ps%d" % i

    idf = sb("idf", [128, 128]); idb = sb("idb", [128, 128], BF16)
    iof = sb("iof", [128, 128]); iob = sb("iob", [128, 128], BF16)
    ones1 = sb("ones1", [1, 128])
    Ept = sb("Ept", [32, 128]); Est = sb("Est", [32, 128])
    cTt = sb("cTt", [128, 8, 32]); scT = sb("scT", [128, 8, 32], BF16)
    S.dma("sp", idf[:], ident, writes=["idf"])
    S.dma("sp", iof[:], iota_in, writes=["iof"])
    S.dma("sp", Ept[:], Ep, writes=["Ept"])
    S.dma("sp", Est[:], Es, writes=["Est"])
    S.dma("sp", cTt[:], cT.rearrange("(k p) m -> p k m", p=128), writes=["cTt"])
    V(lambda e: e.tensor_copy(out=idb[:], in_=idf[:]), ["idf"], ["idb"])
    V(lambda e: e.tensor_copy(out=iob[:], in_=iof[:]), ["iof"], ["iob"])
    V(lambda e: e.memset(ones1[:], 1.0), [], ["ones1"])
    A(lambda e: e.activation(out=scT[:], in_=cTt[:], func=AF.Silu), ["cTt"], ["scT"])

    modseq = sb("modseq", [32, 3 * D])
    Aseq = sb("Aseq", [32, D])
    modtok = [sb("modtok%d" % i, [128, D]) for i in range(6)]

    uid = [0]

    def phase_locals():
        ph = ExitStack()
        uid[0] += 1
        u = uid[0]
        return ph, (lambda name, shape, dt=F32: ph.enter_context(nc.sbuf_tensor("%s_%d" % (name, u), list(shape), dt)))

    def adaln(l, half):
        S.barrier()
        ph, L = phase_locals()
        adab = L("adab", [1, 512]); gbt = L("gbt", [32, D])
        wa = [L("wa0", [128, 8, 512], BF16), L("wa1", [128, 8, 512], BF16)]
        S.dma("sp", gbt[:], (norm1_g if half == 0 else norm2_g)[l].partition_broadcast(32), writes=["gbt"])
        for n in range(6):
            col = half * 3072 + n * 512
            w = wa[n % 2]
            S.dma("pool", w[:], ada_w[l, :, col:col + 512].rearrange("(k p) n -> p k n", p=128), writes=["wa%d" % (n % 2)])
            S.dma("sp", adab[:], ada_b[l:l + 1, col:col + 512], writes=["adab"])
            bank = PS[n % 2]
            for kc in range(8):
                S.mm(lambda e: e.matmul(bank[0:32, :], lhsT=scT[:, kc, :], rhs=w[:, kc, :], start=(kc == 0), stop=False),
                     ["scT", "wa%d" % (n % 2)], [psk(n % 2)], last=False)
            S.mm(lambda e: e.matmul(bank[0:32, :], lhsT=ones1[0:1, 0:32], rhs=adab[0:1, :], start=False, stop=True),
                 ["ones1", "adab"], [psk(n % 2)])
            A(lambda e: e.copy(out=modseq[:, n * 512:(n + 1) * 512], in_=bank[0:32, :]), [psk(n % 2)], ["modseq"])
        V(lambda e: e.scalar_tensor_tensor(out=Aseq[:], in0=modseq[:, D:2 * D], scalar=1.0, in1=gbt[:],
                                           op0=ALU.add, op1=ALU.mult), ["modseq", "gbt"], ["Aseq"])
        expand_mod()
        S.barrier()
        ph.close()

    def expand_mod():
        srcs = [Aseq[:], modseq[:, 0:D], modseq[:, 2 * D:3 * D]]
        k = 0
        for gi, Et in enumerate((Ept, Est)):
            for vi in range(3):
                for hf in range(2):
                    bank = PS[k % 2]
                    S.mm(lambda e: e.matmul(bank[:], lhsT=Et[:], rhs=srcs[vi][:, hf * 512:(hf + 1) * 512],
                                            start=True, stop=True),
                         ["Ept", "Est", "Aseq", "modseq"], [psk(k % 2)])
                    A(lambda e: e.copy(out=modtok[gi * 3 + vi][:, hf * 512:(hf + 1) * 512], in_=bank[:]),
                      [psk(k % 2)], ["modtok%d" % (gi * 3 + vi)])
                    k += 1

    xt = [sb("xt0", [128, D]), sb("xt1", [128, D])]
    st = sb("stat", [128, 8])
    hb = sb("hb", [128, D], BF16)
    hT = sb("hT", [128, 8, 128], BF16)
    tmp = sb("tmp", [128, D])

    def load_x(t, first):
        xb = xt[t % 2]
        src = (xp[t] if t < SMP else xs) if first else xd[t]
        S.dma("sp", xb[:], src, reads=["xd%d" % t], writes=["xt%d" % (t % 2)])
        return xb

    def store_x(t):
        S.dma("sp", xd[t], xt[t % 2][:], reads=["xt%d" % (t % 2)], writes=["xd%d" % t])

    def modnorm_T(xb, xkey, At, Bt, akey, bkey):
        A(lambda e: e.activation(out=tmp[:], in_=xb[:], func=AF.Square, accum_out=st[:, 0:1]), [xkey], ["tmp", "st"])
        A(lambda e: e.activation(out=st[:, 1:2], in_=st[:, 0:1], func=AF.Sqrt, scale=1.0 / D, bias=EPS), ["st"], ["st"])
        V(lambda e: e.reciprocal(out=st[:, 2:3], in_=st[:, 1:2]), ["st"], ["st"])
        if Bt is not None:
            V(lambda e: e.scalar_tensor_tensor(out=tmp[:], in0=xb[:], scalar=st[:, 2:3], in1=At, op0=ALU.mult, op1=ALU.mult),
              [xkey, "st", akey], ["tmp"])
            V(lambda e: e.tensor_tensor(out=hb[:], in0=tmp[:], in1=Bt, op=ALU.add), ["tmp", bkey], ["hb"])
        else:
            V(lambda e: e.scalar_tensor_tensor(out=hb[:], in0=xb[:], scalar=st[:, 2:3], in1=At, op0=ALU.mult, op1=ALU.mult),
              [xkey, "st", akey], ["hb"])
        for k in range(8):
            S.mm(lambda e: e.transpose(out=PSB[:, k * 128:(k + 1) * 128], in_=hb[:, k * 128:(k + 1) * 128], identity=idb[:]),
                 ["hb", "idb"], ["psT"], last=(k == 7))
        A(lambda e: e.copy(out=hT[:].rearrange("p k t -> p (k t)"), in_=PSB[:]), ["psT"], ["hT"])

    def resid_add(xb, xkey, banks, bkeys, Gt, gkey):
        for hf in range(2):
            V(lambda e: e.tensor_tensor(out=tmp[:, hf * 512:(hf + 1) * 512], in0=banks[hf][:], in1=Gt[:, hf * 512:(hf + 1) * 512],
                                        op=ALU.mult), [bkeys[hf], gkey], ["tmp"])
        V(lambda e: e.tensor_tensor(out=xb[:], in0=xb[:], in1=tmp[:], op=ALU.add), ["tmp", xkey], [xkey])

    def gmlp_phase(l, first):
        S.barrier()
        ph, L = phase_locals()
        w_in = L("w_in", [128, 8, 4096], BF16)
        wo = [L("wo0", [128, 4, D], BF16), L("wo1", [128, 4, D], BF16)]
        brow = L("brow", [1, 4096])
        binu = L("binu", [128, 16])
        gsg = L("gsg", [128, 2048])
        wsPb = L("wsPb", [128, 8, 128], BF16); wsSb = L("wsSb", [128, 8, 128], BF16)
        mP = L("mP", [128, 128], BF16); mS = L("mS", [128, 128], BF16)
        vf = L("vf", [128, 2048]); vb = L("vb", [128, 2048], BF16)
        uT = L("uT", [128, 16, 128], BF16); yT = L("yT", [128, 16, 128], BF16)
        binv = brow[0:1, 0:2048]
        bsPt = brow[0:1, 2048:3072].rearrange("p (g t) -> p g t", t=128)
        bsSt = brow[0:1, 3072:4096].rearrange("p (g t) -> p g t", t=128)
        for k in range(8):
            S.dma("pool", w_in[:, k, :], a_w_in[l, k * 128:(k + 1) * 128, :], writes=["w_in"], max_dma_last_dim=8192)
        S.dma("sp", brow[0:1, 0:2048], a_b_in[l:l + 1, 2048:4096], writes=["brow"])
        S.dma("sp", brow[0:1, 2048:3072], bsP[l:l + 1].rearrange("o g t -> o (g t)"), writes=["brow"])
        S.dma("sp", brow[0:1, 3072:4096], bsS[l:l + 1].rearrange("o g t -> o (g t)"), writes=["brow"])
        S.dma("sp", binu[:], a_b_in[l, 0:2048].rearrange("(c p) -> p c", p=128), writes=["binu"], allow_slow_non_contiguous=True)
        S.dma("sp", gsg[:], a_g_sgu[l].partition_broadcast(128), writes=["gsg"])
        S.dma("pool", wsPb[:], wsTp[l], writes=["wsPb"]); S.dma("pool", wsSb[:], wsTs[l], writes=["wsSb"])
        S.dma("pool", mP[:], maskP, writes=["mP"]); S.dma("pool", mS[:], maskS, writes=["mS"])
        S.barrier()
        V(lambda e: e.tensor_tensor(out=wsPb[:], in0=wsPb[:], in1=mP[:].unsqueeze(1).to_broadcast([128, 8, 128]), op=ALU.mult),
          ["wsPb", "mP"], ["wsPb"])
        V(lambda e: e.tensor_tensor(out=wsSb[:], in0=wsSb[:], in1=mS[:].unsqueeze(1).to_broadcast([128, 8, 128]), op=ALU.mult),
          ["wsSb", "mS"], ["wsSb"])
        wo_n = [0]

        def load_wo(j):
            S.dma("pool", wo[wo_n[0] % 2][:], a_w_out[l, j * 512:(j + 1) * 512, :].rearrange("(c p) d -> p c d", p=128),
                  writes=["wo%d" % (wo_n[0] % 2)])
            wo_n[0] += 1
        for t in TILES_A:
            g = 0 if t < SMP else 1
            At, Bt, Gt = modtok[3 * g], modtok[3 * g + 1], modtok[3 * g + 2]
            ak, bk, gk = "modtok%d" % (3 * g), "modtok%d" % (3 * g + 1), "modtok%d" % (3 * g + 2)
            xkey = "xt%d" % (t % 2)
            xb = load_x(t, first)
            modnorm_T(xb, xkey, At[:], Bt[:], ak, bk)
            for n in range(4):
                for kc in range(8):
                    S.mm(lambda e: e.matmul(PS[n][:], lhsT=hT[:, kc, :], rhs=w_in[:, kc, 2048 + n * 512:2048 + (n + 1) * 512],
                                            start=(kc == 0), stop=False), ["hT", "w_in"], [psk(n)], last=False)
                S.mm(lambda e: e.matmul(PS[n][:], lhsT=ones1[0:1, :], rhs=binv[0:1, n * 512:(n + 1) * 512], start=False, stop=True),
                     ["ones1", "brow"], [psk(n)])
                A(lambda e: e.activation(out=vf[:, n * 512:(n + 1) * 512], in_=PS[n][:], func=AF.Gelu_apprx_tanh), [psk(n)], ["vf"])
            A(lambda e: e.activation(out=tmp[:], in_=vf[:, 0:1024], func=AF.Square, accum_out=st[:, 3:4]), ["vf"], ["tmp", "st"])
            A(lambda e: e.activation(out=tmp[:], in_=vf[:, 1024:2048], func=AF.Square, accum_out=st[:, 4:5]), ["vf"], ["tmp", "st"])
            V(lambda e: e.tensor_tensor(out=st[:, 3:4], in0=st[:, 3:4], in1=st[:, 4:5], op=ALU.add), ["st"], ["st"])
            A(lambda e: e.activation(out=st[:, 4:5], in_=st[:, 3:4], func=AF.Sqrt, scale=1.0 / 2048, bias=EPS), ["st"], ["st"])
            V(lambda e: e.reciprocal(out=st[:, 5:6], in_=st[:, 4:5]), ["st"], ["st"])
            V(lambda e: e.scalar_tensor_tensor(out=vf[:], in0=vf[:], scalar=st[:, 5:6], in1=gsg[:], op0=ALU.mult, op1=ALU.mult),
              ["vf", "st", "gsg"], ["vf"])
            V(lambda e: e.tensor_copy(out=vb[:], in_=vf[:]), ["vf"], ["vb"])
            if t == SMP:
                S.dma("sp", av_o[l], vf[0:64, :], reads=["vf"])
            for c in range(16):
                bank = PS[3 + c // 4]
                for kc in range(8):
                    S.mm(lambda e: e.matmul(bank[:, (c % 4) * 128:(c % 4 + 1) * 128], lhsT=w_in[:, kc, c * 128:(c + 1) * 128],
                                            rhs=hT[:, kc, :], start=(kc == 0), stop=(kc == 7)),
                         ["hT", "w_in"], [psk(3 + c // 4)], last=(kc == 7))
                A(lambda e: e.activation(out=uT[:, c, :], in_=bank[:, (c % 4) * 128:(c % 4 + 1) * 128], func=AF.Gelu_apprx_tanh,
                                         bias=binu[:, c:c + 1]), [psk(3 + c // 4), "binu"], ["uT"])
            wsb = wsPb if t < SMP else wsSb
            bst = bsPt if t < SMP else bsSt
            for c in range(16):
                bank = PS[c // 4]
                gi = c // 2
                S.mm(lambda e: e.matmul(bank[:, (c % 4) * 128:(c % 4 + 1) * 128], lhsT=vb[:, c * 128:(c + 1) * 128],
                                        rhs=wsb[:, gi, :], start=True, stop=False), ["vb", "wsPb", "wsSb"], [psk(c // 4)], last=False)
                S.mm(lambda e: e.matmul(bank[:, (c % 4) * 128:(c % 4 + 1) * 128], lhsT=ones1[0:1, :], rhs=bst[0:1, gi, :],
                                        start=False, stop=True), ["ones1", "brow"], [psk(c // 4)], last=(c % 4 == 3))
                if c % 4 == 3:
                    cb = c // 4
                    V(lambda e: e.tensor_tensor(out=yT[:, cb * 4:(cb + 1) * 4, :].rearrange("p c t -> p (c t)"), in0=bank[:],
                                                in1=uT[:, cb * 4:(cb + 1) * 4, :].rearrange("p c t -> p (c t)"), op=ALU.mult),
                      [psk(cb), "uT"], ["yT"])
            for j in range(4):
                slot = wo_n[0] % 2
                load_wo(j)
                for cc in range(4):
                    c = j * 4 + cc
                    for hf in range(2):
                        S.mm(lambda e: e.matmul(PS[4 + hf][:], lhsT=yT[:, c, :], rhs=wo[slot][:, cc, hf * 512:(hf + 1) * 512],
                                                start=(c == 0), stop=(c == 15)), ["yT", "wo%d" % slot], [psk(4 + hf)],
                             last=(cc == 3 and hf == 1))
            resid_add(xb, xkey, [PS[4], PS[5]], [psk(4), psk(5)], Gt, gk)
            store_x(t)
        S.barrier()
        ph.close()

    def peer_phase(l):
        S.barrier()
        ph, L = phase_locals()
        wq = [L("wq0", [128, 8, 512], BF16), L("wq1", [128, 8, 512], BF16)]
        skt = L("skt", [128, 2, 128], BF16)
        RB = []
        for par in range(2):
            d = {}
            d["qT"] = L("qT%d" % par, [128, 16, 128], BF16)
            d["ssb"] = L("ssb%d" % par, [128, 16, 128]); d["ss2"] = L("ss2%d" % par, [128, 16, 128])
            d["sv"] = L("sv%d" % par, [128, 16, 16]); d["si"] = L("si%d" % par, [128, 16, 16], U32); d["sif"] = L("sif%d" % par, [128, 16, 16])
            d["fv"] = L("fv%d" % par, [128, 8, 16]); d["fi"] = L("fi%d" % par, [128, 8, 16], U32)
            d["fa"] = L("fa%d" % par, [128, 8, 16], U32); d["fb"] = L("fb%d" % par, [128, 8, 16], U32)
            d["faf"] = L("faf%d" % par, [128, 8, 16]); d["fbf"] = L("fbf%d" % par, [128, 8, 16])
            d["ex"] = L("ex%d" % par, [128, 8, 16]); d["zz"] = L("zz%d" % par, [128, 8]); d["rz"] = L("rz%d" % par, [128, 8])
            d["sel"] = L("sel%d" % par, [128, 3, 128]); d["selT"] = L("selT%d" % par, [128, 3, 128], BF16)
            RB.append(d)
        oh = L("oh", [128, 8, 16, 16])
        OIb = [L("OI0", [128, 32, 128], BF16), L("OI1", [128, 32, 128], BF16)]
        OJb = [L("OJ0", [128, 32, 128], BF16), L("OJ1", [128, 32, 128], BF16)]
        G = L("G", [128, 128, 128], BF16)
        S.dma("pool", skt[:], skT[l].rearrange("p d k -> d p k"), writes=["skt"])
        wq_n = [0]

        def load_wq(j):
            S.dma("pool", wq[wq_n[0] % 2][:], peer_w_q[l, :, j * 512:(j + 1) * 512].rearrange("(k p) n -> p k n", p=128),
                  writes=["wq%d" % (wq_n[0] % 2)])
            wq_n[0] += 1

        for ti, t in enumerate(TILES_A if l < 2 else TILES_B):
            par = ti % 2
            R = RB[par]
            K = lambda name: "%s_%d" % (name, par)
            qT, ssb, ss2, sv, si, sif = R["qT"], R["ssb"], R["ss2"], R["sv"], R["si"], R["sif"]
            fv, fi, fa, fb, faf, fbf, ex, zz, rz, sel, selT = (R["fv"], R["fi"], R["fa"], R["fb"], R["faf"], R["fbf"], R["ex"],
                                                                R["zz"], R["rz"], R["sel"], R["selT"])
            cand = ssb[:].rearrange("p g k -> p (g k)").rearrange("p (h c) -> p h c", c=256)
            cand2 = ss2[:].rearrange("p g k -> p (g k)").rearrange("p (h c) -> p h c", c=256)
            g = 0 if t < SMP else 1
            At, Bt = modtok[3 * g], modtok[3 * g + 1]
            ak, bk = "modtok%d" % (3 * g), "modtok%d" % (3 * g + 1)
            xkey = "xt%d" % (t % 2)
            xb = load_x(t, False)
            modnorm_T(xb, xkey, At[:], Bt[:], ak, bk)
            S.dma("sp", hT_d[t], hT[:], reads=["hT"])
            for b4 in range(4):
                slot = wq_n[0] % 2
                load_wq(b4)
                bank = PS[2 + b4]
                for g4 in range(4):
                    for kc in range(8):
                        S.mm(lambda e: e.matmul(bank[:, g4 * 128:(g4 + 1) * 128], lhsT=wq[slot][:, kc, g4 * 128:(g4 + 1) * 128],
                                                rhs=hT[:, kc, :], start=(kc == 0), stop=(kc == 7)),
                             ["wq%d" % slot, "hT"], [psk(2 + b4)], last=(kc == 7 and g4 == 3))
                A(lambda e: e.copy(out=qT[:, b4 * 4:(b4 + 1) * 4, :].rearrange("p g t -> p (g t)"), in_=bank[:]),
                  [psk(2 + b4)], [K("qT%d" % b4)])
            SSB = [K("ssb%d" % gbk) for gbk in range(16)]
            SS2 = [K("ss2_%d" % gbk) for gbk in range(16)]
            for gbk in range(16):
                bank = PS[2 + gbk // 4]
                S.mm(lambda e: e.matmul(bank[:, (gbk % 4) * 128:(gbk % 4 + 1) * 128], lhsT=qT[:, gbk, :], rhs=skt[:, gbk % 2, :],
                                        start=True, stop=True), [K("qT%d" % (gbk // 4)), "skt"], [psk(2 + gbk // 4)], last=(gbk % 4 == 3))
                if gbk % 4 == 3:
                    b4 = gbk // 4
                    A(lambda e: e.copy(out=ssb[:, b4 * 4:(b4 + 1) * 4, :].rearrange("p g k -> p (g k)"), in_=bank[:]),
                      [psk(2 + b4)], SSB[b4 * 4:(b4 + 1) * 4])
            SVA = [K("sva%d" % gbk) for gbk in range(16)]; SVB = [K("svb%d" % gbk) for gbk in range(16)]
            SIA = [K("sia%d" % gbk) for gbk in range(16)]; SIB = [K("sib%d" % gbk) for gbk in range(16)]
            for gbk in range(16):
                V(lambda e: e.max(out=sv[:, gbk, 0:8], in_=ssb[:, gbk, :]), [SSB[gbk]], [SVA[gbk]])
            for gbk in range(16):
                V(lambda e: e.max_index(out=si[:, gbk, 0:8], in_max=sv[:, gbk, 0:8], in_values=ssb[:, gbk, :]), [SSB[gbk], SVA[gbk]], [SIA[gbk]])
            for gbk in range(16):
                V(lambda e: e.match_replace(out=ss2[:, gbk, :], in_to_replace=sv[:, gbk, 0:8], in_values=ssb[:, gbk, :], imm_value=NEG),
                  [SSB[gbk], SVA[gbk]], [SS2[gbk]])
            for gbk in range(16):
                V(lambda e: e.max(out=sv[:, gbk, 8:16], in_=ss2[:, gbk, :]), [SS2[gbk]], [SVB[gbk]])
            for gbk in range(16):
                V(lambda e: e.max_index(out=si[:, gbk, 8:16], in_max=sv[:, gbk, 8:16], in_values=ss2[:, gbk, :]), [SS2[gbk], SVB[gbk]], [SIB[gbk]])
            V(lambda e: e.tensor_copy(out=sif[:], in_=si[:]), SIA + SIB, [K("sif")])
            sv4 = sv[:].rearrange("p (h two) k -> p h two k", two=2)
            sif4 = sif[:].rearrange("p (h two) k -> p h two k", two=2)
            CND = [K("cand%d" % h) for h in range(8)]
            V(lambda e: e.tensor_tensor(out=cand.rearrange("p h (a b) -> p h a b", b=16),
                                        in0=sv4[:, :, 0, :].unsqueeze(3).to_broadcast([128, 8, 16, 16]),
                                        in1=sv4[:, :, 1, :].unsqueeze(2).to_broadcast([128, 8, 16, 16]), op=ALU.add),
              SVA + SVB, SSB + CND)
            FVA = [K("fva%d" % h) for h in range(8)]; FVB = [K("fvb%d" % h) for h in range(8)]
            FIA = [K("fia%d" % h) for h in range(8)]; FIB = [K("fib%d" % h) for h in range(8)]
            C2 = [K("c2_%d" % h) for h in range(8)]
            for h in range(8):
                V(lambda e: e.max(out=fv[:, h, 0:8], in_=cand[:, h, :]), [CND[h], SSB[2 * h], SSB[2 * h + 1]], [FVA[h]])
            for h in range(8):
                V(lambda e: e.max_index(out=fi[:, h, 0:8], in_max=fv[:, h, 0:8], in_values=cand[:, h, :]), [CND[h], FVA[h], SSB[2 * h], SSB[2 * h + 1]], [FIA[h]])
            for h in range(8):
                V(lambda e: e.match_replace(out=cand2[:, h, :], in_to_replace=fv[:, h, 0:8], in_values=cand[:, h, :], imm_value=NEG),
                  [CND[h], FVA[h], SSB[2 * h], SSB[2 * h + 1]], [C2[h], SS2[2 * h], SS2[2 * h + 1]])
            for h in range(8):
                V(lambda e: e.max(out=fv[:, h, 8:16], in_=cand2[:, h, :]), [C2[h], SS2[2 * h], SS2[2 * h + 1]], [FVB[h]])
            for h in range(8):
                V(lambda e: e.max_index(out=fi[:, h, 8:16], in_max=fv[:, h, 8:16], in_values=cand2[:, h, :]), [C2[h], FVB[h], SS2[2 * h], SS2[2 * h + 1]], [FIB[h]])
            V(lambda e: e.tensor_tensor(out=ex[:], in0=fv[:], in1=fv[:, :, 0:1].to_broadcast([128, 8, 16]), op=ALU.subtract),
              FVA + FVB, [K("ex")])
            A(lambda e: e.activation(out=ex[:], in_=ex[:], func=AF.Exp), [K("ex")], [K("ex")])
            V(lambda e: e.tensor_reduce(out=zz[:], in_=ex[:], axis=AX.X, op=ALU.add), [K("ex")], [K("zz")])
            V(lambda e: e.reciprocal(out=rz[:], in_=zz[:]), [K("zz")], [K("rz")])
            V(lambda e: e.tensor_tensor(out=sel[:, 2, :].rearrange("p (h k) -> p h k", k=16), in0=ex[:],
                                        in1=rz[:].unsqueeze(2).to_broadcast([128, 8, 16]), op=ALU.mult), [K("ex"), K("rz")], [K("sel2")])
            V(lambda e: e.tensor_single_scalar(out=fa[:], in_=fi[:], scalar=4, op=ALU.logical_shift_right), FIA + FIB, [K("fa")])
            V(lambda e: e.tensor_single_scalar(out=fb[:], in_=fi[:], scalar=15, op=ALU.bitwise_and), FIA + FIB, [K("fb")])
            V(lambda e: e.tensor_copy(out=faf[:], in_=fa[:]), [K("fa")], [K("faf")])
            V(lambda e: e.tensor_copy(out=fbf[:], in_=fb[:]), [K("fb")], [K("fbf")])
            io16 = iof[:, 0:16].unsqueeze(1).unsqueeze(1).to_broadcast([128, 8, 16, 16])
            for which, (ff, fk) in enumerate(((faf, K("faf")), (fbf, K("fbf")))):
                V(lambda e: e.tensor_tensor(out=oh[:], in0=ff[:].unsqueeze(3).to_broadcast([128, 8, 16, 16]), in1=io16, op=ALU.is_equal),
                  [fk, "iof"], ["oh"])
                V(lambda e: e.tensor_tensor(out=oh[:], in0=oh[:], in1=sif4[:, :, which, :].unsqueeze(2).to_broadcast([128, 8, 16, 16]),
                                            op=ALU.mult), ["oh", K("sif")], ["oh"])
                V(lambda e: e.tensor_reduce(out=sel[:, which, :].rearrange("p (h k) -> p h k", k=16), in_=oh[:], axis=AX.X, op=ALU.add),
                  ["oh"], [K("sel%d" % which)])
            for w3 in range(3):
                S.mm(lambda e: e.transpose(out=PS[6][:, w3 * 128:(w3 + 1) * 128], in_=sel[:, w3, :], identity=idf[:]),
                     [K("sel%d" % w3), "idf"], [psk(6)], last=(w3 == 2))
            A(lambda e: e.copy(out=selT[:].rearrange("p w t -> p (w t)"), in_=PS[6][:, 0:384]), [psk(6)], [K("selT")])
            for hf in range(4):
                t0 = hf * 32
                OI, OJ = OIb[hf % 2], OJb[hf % 2]
                ki, kj = "OI%d" % (hf % 2), "OJ%d" % (hf % 2)
                V(lambda e: e.tensor_tensor(out=OI[:], in0=iob[:].unsqueeze(1).to_broadcast([128, 32, 128]),
                                            in1=selT[:, 0, t0:t0 + 32].unsqueeze(2).to_broadcast([128, 32, 128]), op=ALU.is_equal),
                  ["iob", K("selT")], [ki])
                V(lambda e: e.tensor_tensor(out=OJ[:], in0=iob[:].unsqueeze(1).to_broadcast([128, 32, 128]),
                                            in1=selT[:, 1, t0:t0 + 32].unsqueeze(2).to_broadcast([128, 32, 128]), op=ALU.is_equal),
                  ["iob", K("selT")], [kj])
                S.op("pool", lambda e: e.tensor_tensor(out=OJ[:], in0=OJ[:], in1=selT[:, 2, t0:t0 + 32].unsqueeze(2).to_broadcast([128, 32, 128]),
                                                       op=ALU.mult), [kj, K("selT")], [kj])
                for q4 in range(8):
                    bank = PS[q4 % 2]
                    for tt in range(4):
                        tl = q4 * 4 + tt
                        S.mm(lambda e: e.matmul(bank[:, tt * 128:(tt + 1) * 128], lhsT=OJ[:, tl, :], rhs=OI[:, tl, :], start=True, stop=True),
                             [ki, kj], [psk(q4 % 2)], last=(tt == 3))
                    tg = t0 + q4 * 4
                    A(lambda e: e.copy(out=G[:, :, tg:tg + 4].rearrange("p i t -> p t i"),
                                       in_=bank[:].rearrange("p (t i) -> p t i", i=128)), [psk(q4 % 2)], ["G"])
            S.dma("sp", G_d[t], G[:], reads=["G"])
        S.barrier()
        ph.close()

        tiles = TILES_A if l < 2 else TILES_B
        groups = []
        k0 = 0
        while k0 < len(tiles):
            gsz = 9 if (len(tiles) - k0) % 8 == 1 else 8
            groups.append(tiles[k0:k0 + gsz])
            k0 += gsz
        ph, L = phase_locals()
        hTg = L("hTg", [128, 8, 9 * 128], BF16)
        Yacc = L("Yacc", [128, 9, D])
        Gc = [L("Gc0", [128, 4, 9 * 128], BF16), L("Gc1", [128, 4, 9 * 128], BF16)]
        Ub = [L("Ub%d" % i_, [128, 8, 512], BF16) for i_ in range(3)]
        Vb = [L("Vb%d" % i_, [128, 4, D], BF16) for i_ in range(3)]
        ga = [L("ga0", [128, 512], BF16), L("ga1", [128, 512], BF16)]
        WT = L("WT", [128, 4, 512], BF16)
        cnt = [0, 0, 0]
        steps = [(gi, sc) for gi in range(len(groups)) for sc in range(32)]

        def prefetch(k):
            if k >= len(steps):
                return
            gi, sc = steps[k]
            su, sg = k % 3, k % 2
            S.dma("pool", Ub[su][:], peer_uT[l, :, sc * 512:(sc + 1) * 512].rearrange("(k p) e -> p k e", p=128), writes=["Ub%d" % su])
            S.dma("pool", Vb[su][:], peer_v[l, sc * 512:(sc + 1) * 512, :].rearrange("(c p) d -> p c d", p=128), writes=["Vb%d" % su])
            for a_, t in enumerate(groups[gi]):
                S.dma("sp", Gc[sg][:, :, a_ * 128:(a_ + 1) * 128], G_d[t][:, sc * 4:(sc + 1) * 4, :], writes=["Gc%d" % sg])

        prefetch(0)
        for gi, grp in enumerate(groups):
            ntl = len(grp)
            for a_, t in enumerate(grp):
                S.dma("sp", hTg[:, :, a_ * 128:(a_ + 1) * 128], hT_d[t], writes=["hTg"])
            V(lambda e: e.memset(Yacc[:], 0.0), [], ["Yacc"])
            for sc in range(32):
                k = cnt[0]
                cnt[0] += 1
                s_ = k % 3
                sg_ = k % 2
                prefetch(k + 1)
                for sub in range(0, ntl, 4):
                    nsub = min(4, ntl - sub)
                    N = nsub * 128
                    tok0 = sub * 128
                    for ci in range(4):
                        bi = cnt[1] % 2
                        cnt[1] += 1
                        bank = PS[2 + bi]
                        for kc in range(8):
                            S.mm(lambda e: e.matmul(bank[:, 0:N], lhsT=Ub[s_][:, kc, ci * 128:(ci + 1) * 128], rhs=hTg[:, kc, tok0:tok0 + N],
                                                    start=(kc == 0), stop=(kc == 7)), ["Ub%d" % s_, "hTg"], [psk(2 + bi)], last=(kc == 7))
                        A(lambda e: e.activation(out=ga[bi][:, 0:N], in_=bank[:, 0:N], func=AF.Gelu_apprx_tanh), [psk(2 + bi)], ["ga%d" % bi])
                        S.op("pool", lambda e: e.tensor_tensor(out=WT[:, ci, 0:N], in0=ga[bi][:, 0:N], in1=Gc[sg_][:, ci, tok0:tok0 + N], op=ALU.mult),
                             ["ga%d" % bi, "Gc%d" % sg_], ["WT%d" % ci])
                    for a_ in range(nsub):
                        yb = cnt[2] % 2
                        cnt[2] += 1
                        ybank = [PS[0], PS[1]] if yb == 0 else [PS[4], PS[5]]
                        ykey = [psk(0), psk(1)] if yb == 0 else [psk(4), psk(5)]
                        for ci in range(4):
                            for hf in range(2):
                                S.mm(lambda e: e.matmul(ybank[hf][:], lhsT=WT[:, ci, a_ * 128:(a_ + 1) * 128], rhs=Vb[s_][:, ci, hf * 512:(hf + 1) * 512],
                                                        start=(ci == 0), stop=(ci == 3)), ["WT%d" % ci, "Vb%d" % s_], [ykey[hf]],
                                     last=(ci == 3 and hf == 1))
                        ta = sub + a_
                        for hf in range(2):
                            V(lambda e: e.tensor_tensor(out=Yacc[:, ta, hf * 512:(hf + 1) * 512], in0=ybank[hf][:], in1=Yacc[:, ta, hf * 512:(hf + 1) * 512],
                                                        op=ALU.add), [ykey[hf], "Yacc"], ["Yacc"])
            for a_, t in enumerate(grp):
                g = 0 if t < SMP else 1
                Gt, gk = modtok[3 * g + 2], "modtok%d" % (3 * g + 2)
                xkey = "xt%d" % (t % 2)
                xb = load_x(t, False)
                V(lambda e: e.tensor_tensor(out=tmp[:], in0=Yacc[:, a_, :], in1=Gt[:], op=ALU.mult), ["Yacc", gk], ["tmp"])
                V(lambda e: e.tensor_tensor(out=xb[:], in0=xb[:], in1=tmp[:], op=ALU.add), ["tmp", xkey], [xkey])
                store_x(t)
        S.barrier()
        ph.close()

    def kv_phase():
        S.barrier()
        ph, L = phase_locals()
        wkv = L("wkv", [128, 8, 512], BF16)
        gkv = L("gkv", [128, D]); gk64 = L("gk64", [128, 64])
        kvf = L("kvf", [128, 512]); ksq = L("ksq", [128, 256]); kst = L("kst", [128, 8])
        cs = L("cs", [128, 8]); sn = L("sn", [128, 8])
        r1 = L("r1", [128, 4, 8]); r2 = L("r2", [128, 4, 8]); r3 = L("r3", [128, 4, 8]); r4 = L("r4", [128, 4, 8])
        S.dma("pool", wkv[:], w_kv.rearrange("(k p) n -> p k n", p=128), writes=["wkv"])
        S.dma("sp", gkv[:], kv_norm_g.partition_broadcast(128), writes=["gkv"])
        S.dma("sp", gk64[:], k_norm_g.partition_broadcast(128), writes=["gk64"])
        kvb = L("kvb", [128, 512], BF16); ktl = L("ktl", [128, 2, 128], BF16); ksr = L("ksr", [1, 256])
        onesc = L("onesc", [128, 1])
        V(lambda e: e.memset(onesc[:], 1.0), [], ["onesc"])
        for t in TILES_A:
            xkey = "xt%d" % (t % 2)
            xb = load_x(t, False)
            modnorm_T(xb, xkey, gkv[:], None, "gkv", None)
            S.dma("sp", cs[:], cosT[t], writes=["cs"]); S.dma("sp", sn[:], sinT[t], writes=["sn"])
            for kc in range(8):
                S.mm(lambda e: e.matmul(PS[0][:], lhsT=hT[:, kc, :], rhs=wkv[:, kc, :], start=(kc == 0), stop=(kc == 7)),
                     ["hT", "wkv"], [psk(0)], last=(kc == 7))
            A(lambda e: e.copy(out=kvf[:], in_=PS[0][:]), [psk(0)], ["kvf"])
            k3 = kvf[:, 0:256].rearrange("p (h d) -> p h d", d=64)
            V(lambda e: e.tensor_tensor(out=ksq[:], in0=kvf[:, 0:256], in1=kvf[:, 0:256], op=ALU.mult), ["kvf"], ["ksq"])
            V(lambda e: e.tensor_reduce(out=kst[:, 0:4], in_=ksq[:].rearrange("p (h d) -> p h d", d=64), axis=AX.X, op=ALU.add), ["ksq"], ["kst"])
            A(lambda e: e.activation(out=kst[:, 4:8], in_=kst[:, 0:4], func=AF.Sqrt, scale=1.0 / 64, bias=EPS), ["kst"], ["kst"])
            V(lambda e: e.reciprocal(out=kst[:, 0:4], in_=kst[:, 4:8]), ["kst"], ["kst"])
            V(lambda e: e.tensor_tensor(out=k3, in0=k3, in1=kst[:, 0:4].unsqueeze(2).to_broadcast([128, 4, 64]), op=ALU.mult), ["kvf", "kst"], ["kvf"])
            V(lambda e: e.tensor_tensor(out=k3, in0=k3, in1=gk64[:].unsqueeze(1).to_broadcast([128, 4, 64]), op=ALU.mult), ["kvf", "gk64"], ["kvf"])
            cb = cs[:].unsqueeze(1).to_broadcast([128, 4, 8]); snb = sn[:].unsqueeze(1).to_broadcast([128, 4, 8])
            V(lambda e: e.tensor_tensor(out=r1[:], in0=k3[:, :, 0:8], in1=cb, op=ALU.mult), ["kvf", "cs"], ["r1"])
            V(lambda e: e.tensor_tensor(out=r2[:], in0=k3[:, :, 8:16], in1=snb, op=ALU.mult), ["kvf", "sn"], ["r2"])
            V(lambda e: e.tensor_tensor(out=r3[:], in0=k3[:, :, 8:16], in1=cb, op=ALU.mult), ["kvf", "cs"], ["r3"])
            V(lambda e: e.tensor_tensor(out=r4[:], in0=k3[:, :, 0:8], in1=snb, op=ALU.mult), ["kvf", "sn"], ["r4"])
            V(lambda e: e.tensor_tensor(out=k3[:, :, 0:8], in0=r1[:], in1=r2[:], op=ALU.subtract), ["r1", "r2", "kvf"], ["kvf"])
            V(lambda e: e.tensor_tensor(out=k3[:, :, 8:16], in0=r3[:], in1=r4[:], op=ALU.add), ["r3", "r4", "kvf"], ["kvf"])
            v3 = kvf[:, 256:512].rearrange("p (h d) -> p h d", d=64)
            V(lambda e: e.tensor_copy(out=kvb[:], in_=kvf[:]), ["kvf"], ["kvb"])
            for pr in range(2):
                S.mm(lambda e: e.transpose(out=PSB[:, pr * 128:(pr + 1) * 128], in_=kvb[:, pr * 128:(pr + 1) * 128], identity=idb[:]),
                     ["kvb", "idb"], ["psT"], last=(pr == 1))
            A(lambda e: e.copy(out=ktl[:].rearrange("p a t -> p (a t)"), in_=PSB[:, 0:256]), ["psT"], ["ktl"])
            S.dma("sp", kT_d[t].rearrange("a p t -> p a t"), ktl[:], reads=["ktl"])
            S.dma("sp", v_d[t], kvb[:, 256:512], reads=["kvb"])
            S.mm(lambda e: e.matmul(PS[1][0:1, 0:256], lhsT=onesc[:], rhs=kvf[:, 0:256], start=True, stop=True), ["onesc", "kvf"], [psk(1)])
            A(lambda e: e.copy(out=ksr[:], in_=PS[1][0:1, 0:256]), [psk(1)], ["ksr"])
            S.dma("sp", ks_d[t:t + 1, :], ksr[:], reads=["ksr"])
            if t >= 16 and t < SMP:
                continue
            if t < 16:
                S.dma("sp", kp_o[t].rearrange("h t d -> t h d"), k3, reads=["kvf"])
                S.dma("sp", vp_o[t].rearrange("h t d -> t h d"), v3, reads=["kvf"])
            else:
                for s_ in range(16):
                    S.dma("sp", ks_o[s_].rearrange("h t d -> t h d"), k3[s_ * 4:(s_ + 1) * 4], reads=["kvf"])
                    S.dma("sp", vs_o[s_].rearrange("h t d -> t h d"), v3[s_ * 4:(s_ + 1) * 4], reads=["kvf"])
        S.barrier()
        ph.close()

    def cache_phase():
        S.barrier()
        ph, L = phase_locals()
        I32 = mybir.dt.int32
        ptb = L("ptb", [128, 256], I32); ptf = L("ptf", [128, 256]); iop = L("iop", [128, 1])
        idxf = L("idxf", [128, 256, 4]); idxu = L("idxu", [128, 256, 4], U32)
        kc = [L("kc0", [128, 256]), L("kc1", [128, 256])]
        vc = [L("vc0", [128, 256]), L("vc1", [128, 256])]
        kcb = L("kcb", [128, 256], BF16); vcb = L("vcb", [128, 256], BF16)
        ktl = L("ktlc", [128, 2, 128], BF16); ksr = L("ksrc", [1, 256]); onesc = L("onescc", [128, 1])
        V(lambda e: e.memset(onesc[:], 1.0), [], ["onesc"])
        S.dma("sp", ptb[:], ptab.partition_broadcast(128), writes=["ptb"])
        S.dma("sp", iop[:], iotap_in, writes=["iop"])
        V(lambda e: e.tensor_copy(out=ptf[:], in_=ptb[:]), ["ptb"], ["ptf"])
        for kvh in range(4):
            V(lambda e: e.tensor_scalar(out=idxf[:, :, kvh], in0=ptf[:], scalar1=512.0, scalar2=float(kvh * 128), op0=ALU.mult, op1=ALU.add),
              ["ptf"], ["idxf"])
        V(lambda e: e.tensor_scalar(out=idxf[:], in0=idxf[:], scalar1=iop[:, 0:1], scalar2=None, op0=ALU.add), ["idxf", "iop"], ["idxf"])
        V(lambda e: e.tensor_copy(out=idxu[:], in_=idxf[:]), ["idxf"], ["idxu"])
        S.barrier()
        n = 0
        for sq_ in range(16):
            for pg in range(16):
                col = sq_ * 16 + pg
                sl = n % 2
                for kvh in range(4):
                    S.idma(kc[sl][:, kvh * 64:(kvh + 1) * 64], cache_k, idxu[:, col, kvh:kvh + 1], ["idxu"], ["kc%d" % sl])
                    S.idma(vc[sl][:, kvh * 64:(kvh + 1) * 64], cache_v, idxu[:, col, kvh:kvh + 1], ["idxu"], ["vc%d" % sl])
                V(lambda e: e.tensor_copy(out=kcb[:], in_=kc[sl][:]), ["kc%d" % sl], ["kcb"])
                A(lambda e: e.copy(out=vcb[:], in_=vc[sl][:]), ["vc%d" % sl], ["vcb"])
                for pr in range(2):
                    S.mm(lambda e: e.transpose(out=PSB[:, pr * 128:(pr + 1) * 128], in_=kcb[:, pr * 128:(pr + 1) * 128], identity=idb[:]),
                         ["kcb", "idb"], ["psT"], last=(pr == 1))
                A(lambda e: e.copy(out=ktl[:].rearrange("p a t -> p (a t)"), in_=PSB[:, 0:256]), ["psT"], ["ktl"])
                S.dma("sp", kTs_d[sq_, pg].rearrange("a p t -> p a t"), ktl[:], reads=["ktl"])
                S.dma("sp", vS_d[sq_, pg], vcb[:], reads=["vcb"])
                S.mm(lambda e: e.matmul(PS[1][0:1, 0:256], lhsT=onesc[:], rhs=kc[sl][:], start=True, stop=True), ["onesc", "kc%d" % sl], [psk(1)])
                A(lambda e: e.copy(out=ksr[:], in_=PS[1][0:1, 0:256]), [psk(1)], ["ksr"])
                S.dma("sp", ksS_d[sq_, pg:pg + 1, :], ksr[:], reads=["ksr"])
                n += 1
        S.barrier()
        ph.close()

    def attn_phase(l):
        j = l - 2
        S.barrier()
        ph, L = phase_locals()
        wq = L("awq", [128, 8, D], BF16); wo = L("awo", [128, 8, D], BF16)
        KT = L("KT", [128, 4, 32, 128], BF16)
        VV = L("VV", [128, 32, 256], BF16)
        KM = L("KM", [128, 4, 32], BF16)
        kss = L("kss", [32, 256]); ks2 = L("ks2", [32, 4, 2, 64])
        SelP = L("SelP", [32, 16]); SelS = L("SelS", [16, 8])
        gq64 = L("gq64", [128, 64]); mOwn = L("mOwn", [128, 256]); mOwnS = L("mOwnS", [128, 128]); rowm = L("rowm", [128, 16])
        qf = L("qf", [128, D]); qb = L("qb", [128, D], BF16); qT = L("qTa", [128, 8, 128], BF16)
        qst = L("qst", [128, 48])
        cs = L("acs", [128, 8]); sn = L("asn", [128, 8])
        r1 = L("ar1", [128, 16, 8]); r2 = L("ar2", [128, 16, 8]); r3 = L("ar3", [128, 16, 8]); r4 = L("ar4", [128, 16, 8])
        gate = L("gate", [128, 16, 16]); gm = L("gm", [128, 16, 16]); m8 = L("m8", [128, 16, 8]); mb = L("mb", [128, 16, 16])
        P = L("P", [128, 4096], BF16); PT = L("PT", [128, 32, 128], BF16)
        so = L("so", [128, 256])
        den = L("den", [128, 16, 136]); dsum = L("dsum", [128, 16]); drec = L("drec", [128, 16])
        Osb = L("Osb", [128, D]); ob = L("ob", [128, D], BF16); oT = L("oT", [128, 8, 128], BF16)
        S.dma("pool", wq[:], b_w_q[j].rearrange("(k p) n -> p k n", p=128), writes=["awq"])
        S.dma("pool", wo[:], b_w_o[j].rearrange("(k p) n -> p k n", p=128), writes=["awo"])
        S.dma("sp", gq64[:], b_q_norm_g[j].partition_broadcast(128), writes=["gq64"])
        S.dma("sp", mOwn[:], maskOwn_in, writes=["mOwn"]); S.dma("sp", mOwnS[:], maskOwnS_in, writes=["mOwnS"])
        S.dma("sp", rowm[:], rowmask_in, writes=["rowm"])
        S.dma("sp", SelP[:], SelP_in, writes=["SelP"]); S.dma("sp", SelS[:], SelS_in, writes=["SelS"])
        S.barrier()

        def load_ctx(kT_src, v_src, ks_src, nslots, interleave, Sel, nblk):
            parts = [(0, 16, 0, 2), (16, 32, 1, 2)] if interleave else [(0, nslots, 0, 1)]
            for (s0, s1, k0, kstep) in parts:
                nk = s1 - s0
                for kvh in range(4):
                    for base in (0, 64):
                        S.dma("sp", KT[base:base + 64, kvh, k0:k0 + kstep * (nk - 1) + 1:kstep, :],
                              kT_src[s0:s1, kvh // 2, (kvh % 2) * 64:(kvh % 2) * 64 + 64, :].rearrange("s d t -> d s t"),
                              reads=["kscr"], writes=["KT"])
                S.dma("sp", VV[:, k0:k0 + kstep * (nk - 1) + 1:kstep, :], v_src[s0:s1].rearrange("s k c -> k s c"), reads=["kscr"], writes=["VV"])
            S.dma("sp", kss[0:nslots, :], ks_src, reads=["kscr"], writes=["kss"])
            k3s = kss[0:nslots, :].rearrange("p (h d) -> p h d", d=64)
            V(lambda e: e.tensor_copy(out=ks2[0:nslots, :, 0, :], in_=k3s), ["kss"], ["ks2"])
            V(lambda e: e.tensor_copy(out=ks2[0:nslots, :, 1, :], in_=k3s), ["kss"], ["ks2"])
            V(lambda e: e.memset(KM[:], 0.0), [], ["KM"])
            for kvh in range(4):
                S.mm(lambda e: e.matmul(PS[2][:, 0:nblk], lhsT=ks2[0:nslots, kvh, :, :].rearrange("p a d -> p (a d)"), rhs=Sel[0:nslots, 0:nblk],
                                        start=True, stop=True), ["ks2", "SelP", "SelS"], [psk(2)])
                A(lambda e: e.copy(out=KM[0:64, kvh, 0:nblk], in_=PS[2][0:64, 0:nblk]), [psk(2)], ["KM"])
                A(lambda e: e.copy(out=KM[64:128, kvh, 16:16 + nblk], in_=PS[2][64:128, 0:nblk]), [psk(2)], ["KM"])

        def compute_q(t):
            for hf in range(2):
                for kc_ in range(8):
                    S.mm(lambda e: e.matmul(PS[hf][:], lhsT=hT[:, kc_, :], rhs=wq[:, kc_, hf * 512:(hf + 1) * 512], start=(kc_ == 0), stop=(kc_ == 7)),
                         ["hT", "awq"], [psk(hf)], last=(kc_ == 7))
                A(lambda e: e.copy(out=qf[:, hf * 512:(hf + 1) * 512], in_=PS[hf][:]), [psk(hf)], ["qf"])
            q3 = qf[:].rearrange("p (h d) -> p h d", d=64)
            V(lambda e: e.tensor_tensor(out=tmp[:], in0=qf[:], in1=qf[:], op=ALU.mult), ["qf"], ["tmp"])
            V(lambda e: e.tensor_reduce(out=qst[:, 0:16], in_=tmp[:].rearrange("p (h d) -> p h d", d=64), axis=AX.X, op=ALU.add), ["tmp"], ["qst"])
            A(lambda e: e.activation(out=qst[:, 16:32], in_=qst[:, 0:16], func=AF.Sqrt, scale=1.0 / 64, bias=EPS), ["qst"], ["qst"])
            V(lambda e: e.reciprocal(out=qst[:, 32:48], in_=qst[:, 16:32]), ["qst"], ["qst"])
            V(lambda e: e.tensor_tensor(out=q3, in0=q3, in1=qst[:, 32:48].unsqueeze(2).to_broadcast([128, 16, 64]), op=ALU.mult), ["qf", "qst"], ["qf"])
            V(lambda e: e.tensor_tensor(out=q3, in0=q3, in1=gq64[:].unsqueeze(1).to_broadcast([128, 16, 64]), op=ALU.mult), ["qf", "gq64"], ["qf"])
            S.dma("sp", cs[:], cosT[t], writes=["acs"]); S.dma("sp", sn[:], sinT[t], writes=["asn"])
            cb = cs[:].unsqueeze(1).to_broadcast([128, 16, 8]); snb = sn[:].unsqueeze(1).to_broadcast([128, 16, 8])
            V(lambda e: e.tensor_tensor(out=r1[:], in0=q3[:, :, 0:8], in1=cb, op=ALU.mult), ["qf", "acs"], ["ar1"])
            V(lambda e: e.tensor_tensor(out=r2[:], in0=q3[:, :, 8:16], in1=snb, op=ALU.mult), ["qf", "asn"], ["ar2"])
            V(lambda e: e.tensor_tensor(out=r3[:], in0=q3[:, :, 8:16], in1=cb, op=ALU.mult), ["qf", "acs"], ["ar3"])
            V(lambda e: e.tensor_tensor(out=r4[:], in0=q3[:, :, 0:8], in1=snb, op=ALU.mult), ["qf", "asn"], ["ar4"])
            V(lambda e: e.tensor_tensor(out=q3[:, :, 0:8], in0=r1[:], in1=r2[:], op=ALU.subtract), ["ar1", "ar2", "qf"], ["qf"])
            V(lambda e: e.tensor_tensor(out=q3[:, :, 8:16], in0=r3[:], in1=r4[:], op=ALU.add), ["ar3", "ar4", "qf"], ["qf"])
            V(lambda e: e.tensor_copy(out=qb[:], in_=qf[:]), ["qf"], ["qb"])
            for k in range(8):
                S.mm(lambda e: e.transpose(out=PSB[:, k * 128:(k + 1) * 128], in_=qb[:, k * 128:(k + 1) * 128], identity=idb[:]),
                     ["qb", "idb"], ["psT"], last=(k == 7))
            A(lambda e: e.copy(out=qT[:].rearrange("p k t -> p (k t)"), in_=PSB[:]), ["psT"], ["qTa"])

        def gates(nblk, nvalid, rowbias):
            for pr in range(8):
                S.mm(lambda e: e.matmul(PS[2][:, pr * 32:pr * 32 + 32], lhsT=qT[:, pr, :], rhs=KM[:, pr // 2, :], start=True, stop=True),
                     ["qTa", "KM"], [psk(2)], last=(pr == 7))
            V(lambda e: e.memset(gm[:], NEG), [], ["gm"])
            V(lambda e: e.tensor_copy(out=gm[:, :, 0:nvalid],
                                      in_=PS[2][:, 0:256].rearrange("p (pr two n) -> p (pr two) n", two=2, n=16)[:, :, 0:nvalid]),
              [psk(2)], ["gm"])
            for h in range(16):
                V(lambda e: e.max(out=m8[:, h, :], in_=gm[:, h, :]), ["gm"], ["m8"])
            V(lambda e: e.tensor_tensor(out=mb[:, :, 0:nvalid], in0=gm[:, :, 0:nvalid], in1=m8[:, :, 2:3].to_broadcast([128, 16, nvalid]), op=ALU.is_ge),
              ["gm", "m8"], ["mb"])
            V(lambda e: e.tensor_scalar(out=mb[:, :, 0:nvalid], in0=mb[:, :, 0:nvalid], scalar1=-1.0, scalar2=-MNEG, op0=ALU.add, op1=ALU.mult),
              ["mb"], ["mb"])
            if rowbias is not None:
                V(lambda e: e.tensor_scalar(out=mb[:, :, 0:nvalid], in0=mb[:, :, 0:nvalid], scalar1=rowbias, scalar2=None, op0=ALU.add), ["mb", "rowm"], ["mb"])

        def attend_head(h, blocks, dcol0, accumulate):
            pr, base, kvh = h // 2, (h % 2) * 64, h // 4
            ntile = 0
            bi = 0
            for (kt0, nkt, bias, emask) in blocks:
                bank = PS[2 + bi % 2]
                S.mm(lambda e: e.matmul(bank[:, 0:nkt * 128], lhsT=qT[base:base + 64, pr, :],
                                        rhs=KT[base:base + 64, kvh, kt0:kt0 + nkt, :].rearrange("p a k -> p (a k)"), start=True, stop=True),
                     ["qTa", "KT"], [psk(2 + bi % 2)])
                pdst = P[:, ntile * 128:(ntile + nkt) * 128]
                dcol = den[:, h, dcol0 + bi:dcol0 + bi + 1]
                if emask is not None:
                    V(lambda e: e.tensor_tensor(out=so[:, 0:nkt * 128], in0=bank[:, 0:nkt * 128], in1=emask, op=ALU.add), [psk(2 + bi % 2), "mOwn", "mOwnS"], ["so"])
                    A(lambda e: e.activation(out=pdst, in_=so[:, 0:nkt * 128], func=AF.Exp, scale=0.125, accum_out=dcol), ["so"], ["P", "den"])
                else:
                    A(lambda e: e.activation(out=pdst, in_=bank[:, 0:nkt * 128], func=AF.Exp, scale=0.125, bias=bias, accum_out=dcol),
                      [psk(2 + bi % 2), "mb"], ["P", "den"])
                ntile += nkt
                bi += 1
            for kt in range(ntile):
                S.mm(lambda e: e.transpose(out=PSB[:, (kt % 8) * 128:(kt % 8 + 1) * 128], in_=P[:, kt * 128:(kt + 1) * 128], identity=idb[:]),
                     ["P", "idb"], ["psT"], last=(kt % 8 == 7 or kt == ntile - 1))
                if kt % 8 == 7 or kt == ntile - 1:
                    k0 = (kt // 8) * 8
                    nk = kt - k0 + 1
                    V(lambda e: e.tensor_copy(out=PT[:, k0:k0 + nk, :].rearrange("p a t -> p (a t)"), in_=PSB[:, 0:nk * 128]), ["psT"], ["PT"])
            kti = 0
            for (kt0, nkt, bias, emask) in blocks:
                for a_ in range(nkt):
                    S.mm(lambda e: e.matmul(PS[6][:, 0:64], lhsT=PT[:, kti, :], rhs=VV[:, kt0 + a_, kvh * 64:(kvh + 1) * 64],
                                            start=(kti == 0), stop=(kti == ntile - 1)), ["PT", "VV"], [psk(6)], last=(kti == ntile - 1))
                    kti += 1
            if accumulate:
                V(lambda e: e.tensor_tensor(out=Osb[:, h * 64:(h + 1) * 64], in0=PS[6][:, 0:64], in1=Osb[:, h * 64:(h + 1) * 64], op=ALU.add),
                  [psk(6), "Osb"], ["Osb"])
            else:
                V(lambda e: e.tensor_copy(out=Osb[:, h * 64:(h + 1) * 64], in_=PS[6][:, 0:64]), [psk(6)], ["Osb"])

        def finish_tile(xb, xkey, Gt, gk):
            V(lambda e: e.tensor_reduce(out=dsum[:], in_=den[:], axis=AX.X, op=ALU.add), ["den"], ["dsum"])
            V(lambda e: e.tensor_scalar(out=dsum[:], in0=dsum[:], scalar1=1e-30, scalar2=None, op0=ALU.add), ["dsum"], ["dsum"])
            V(lambda e: e.reciprocal(out=drec[:], in_=dsum[:]), ["dsum"], ["drec"])
            V(lambda e: e.tensor_tensor(out=ob[:].rearrange("p (h d) -> p h d", d=64), in0=Osb[:].rearrange("p (h d) -> p h d", d=64),
                                        in1=drec[:].unsqueeze(2).to_broadcast([128, 16, 64]), op=ALU.mult), ["Osb", "drec"], ["ob"])
            for k in range(8):
                S.mm(lambda e: e.transpose(out=PSB[:, k * 128:(k + 1) * 128], in_=ob[:, k * 128:(k + 1) * 128], identity=idb[:]),
                     ["ob", "idb"], ["psT"], last=(k == 7))
            A(lambda e: e.copy(out=oT[:].rearrange("p k t -> p (k t)"), in_=PSB[:]), ["psT"], ["oT"])
            for hf in range(2):
                for c in range(8):
                    S.mm(lambda e: e.matmul(PS[4 + hf][:], lhsT=oT[:, c, :], rhs=wo[:, c, hf * 512:(hf + 1) * 512], start=(c == 0), stop=(c == 7)),
                         ["oT", "awo"], [psk(4 + hf)], last=(c == 7))
            resid_add(xb, xkey, [PS[4], PS[5]], [psk(4), psk(5)], Gt, gk)

        load_ctx(kT_d, v_d, ks_d[0:32, :], 32, True, SelP, 16)
        for i in range(16):
            xkey = "xt%d" % (i % 2)
            xb = load_x(i, False)
            modnorm_T(xb, xkey, modtok[0][:], modtok[1][:], "modtok0", "modtok1")
            compute_q(i)
            V(lambda e: e.memset(den[:], 0.0), [], ["den"])
            if i > 0:
                gates(16, i, None)
            for h in range(16):
                blocks = []
                n = 0
                while n < i:
                    if n + 1 < i:
                        blocks.append((2 * n, 2, mb[:, h, n:n + 1], None))
                        blocks.append((2 * n + 2, 2, mb[:, h, n + 1:n + 2], None))
                        n += 2
                    else:
                        blocks.append((2 * n, 2, mb[:, h, n:n + 1], None))
                        n += 1
                blocks.append((2 * i, 2, None, mOwn[:, 0:256]))
                attend_head(h, blocks, 0, False)
            finish_tile(xb, xkey, modtok[2], "modtok2")
            store_x(i)
        xkey = "xt%d" % (SMP % 2)
        xb = load_x(SMP, False)
        modnorm_T(xb, xkey, modtok[3][:], modtok[4][:], "modtok3", "modtok4")
        compute_q(SMP)
        V(lambda e: e.memset(den[:], 0.0), [], ["den"])
        for kvh in range(4):
            for base in (0, 64):
                S.dma("sp", KT[base:base + 64, kvh, 0, :], kT_d[SMP, kvh // 2, (kvh % 2) * 64:(kvh % 2) * 64 + 64, :], writes=["KT"])
        S.dma("sp", VV[:, 0, :], v_d[SMP], writes=["VV"])
        for h in range(16):
            attend_head(h, [(0, 1, None, mOwnS[:, 0:128])], 0, False)
        for sq_ in range(16):
            load_ctx(kTs_d[sq_], vS_d[sq_], ksS_d[sq_], 16, False, SelS, 8)
            gates(8, 8, rowm[:, sq_:sq_ + 1])
            for h in range(16):
                blocks = [(2 * n, 2, mb[:, h, n:n + 1], None) for n in range(8)]
                attend_head(h, blocks, 1 + sq_ * 8, True)
        finish_tile(xb, xkey, modtok[5], "modtok5")
        store_x(SMP)
        S.barrier()
        ph.close()

    first = True
    for l in range(n_layers):
        adaln(l, 0)
        if l < 2:
            gmlp_phase(l, first)
            first = False
        elif do_attn:
            attn_phase(l)
        if do_peer:
            adaln(l, 1)
            peer_phase(l)
        if l == 1:
            kv_phase()
            if do_attn and n_layers > 2:
                cache_phase()
    S.barrier()
    for t in TILES_B:
        xb = load_x(t, False)
        S.dma("sp", y_p[t] if t < SMP else y_s, xb[:], reads=["xt%d" % (t % 2)])
    S.finish()
    return nc, S


def _host_inputs(inp):
    f = np.float32
    c = lambda a: np.ascontiguousarray(a, dtype=f)
    x_prompt = inp["x_prompt"]; x_sample = inp["x_sample"]
    a_w_s = np.asarray(inp["a_w_s"], dtype=f); a_b_s = np.asarray(inp["a_b_s"], dtype=f)
    shared = {}
    for k in ("ada_w", "ada_b", "norm1_g", "norm2_g", "a_w_in", "a_b_in", "a_g_sgu", "a_w_out", "kv_norm_g", "w_kv",
              "k_norm_g", "peer_w_q", "peer_v", "b_w_q", "b_q_norm_g", "b_w_o"):
        shared[k] = c(inp[k])
    shared["cache_k"] = c(inp["cache_k"]).reshape(-1, 64)
    shared["cache_v"] = c(inp["cache_v"]).reshape(-1, 64)
    shared["peer_uT"] = c(np.transpose(inp["peer_u"], (0, 2, 1)))
    shared["skT"] = c(np.transpose(inp["peer_sub_keys"], (0, 1, 3, 2)))
    shared["wsTp"] = c(np.transpose(a_w_s, (0, 3, 1, 2)))
    wsTs = np.zeros((2, 128, 8, 128), f)
    bsS = np.zeros((2, 8, 128), f)
    for q in range(16):
        wsTs[:, q * 4:(q + 1) * 4, :, q * 4:(q + 1) * 4] = np.transpose(a_w_s[:, :, :4, :4], (0, 3, 1, 2))
        bsS[:, :, q * 4:(q + 1) * 4] = a_b_s[:, :, :4]
    shared["wsTs"] = wsTs
    shared["bsP"] = c(a_b_s)
    shared["bsS"] = bsS
    s_idx = np.arange(128)[:, None]; t_idx = np.arange(128)[None, :]
    shared["maskP"] = (s_idx <= t_idx).astype(f)
    shared["maskS"] = ((s_idx // 4 == t_idx // 4) & (s_idx % 4 <= t_idx % 4) & (s_idx < 64)).astype(f)
    shared["ident"] = np.eye(128, dtype=f)
    shared["iota"] = np.tile(np.arange(128, dtype=f)[None, :], (128, 1))
    shared["iotap"] = np.arange(128, dtype=f)[:, None].copy()
    Ep = np.zeros((32, 128), f); Ep[0, :] = 1.0
    Es = np.zeros((32, 128), f)
    for t in range(64):
        Es[1 + t // 4, t] = 1.0
    shared["Ep"] = Ep; shared["Es"] = Es
    SelP = np.zeros((32, 16), f)
    for sl in range(32):
        SelP[sl, sl % 16] = 1.0 / 256
    SelS = np.zeros((16, 8), f)
    for pg in range(16):
        SelS[pg, pg // 2] = 1.0 / 256
    shared["SelP"] = SelP; shared["SelS"] = SelS
    tq = np.arange(128)[:, None]; kk = np.arange(128)[None, :]
    shared["maskOwnS"] = np.where((kk // 4 == tq // 4) & (kk % 4 <= tq % 4) & (tq < 64) & (kk < 64), 0.0, MNEG).astype(f)
    rowmask = np.full((128, 16), MNEG, f)
    for t in range(64):
        rowmask[t, t // 4] = 0.0
    shared["rowmask"] = rowmask
    half = 8
    inv = (500000.0 ** (-np.arange(half, dtype=np.float64) / half)).astype(f)
    maps = []
    for core in range(8):
        b, p = core // 2, core % 2
        m = dict(shared)
        xt32 = x_prompt[b].reshape(32, 128, D)
        m["xp"] = c(np.concatenate([xt32[p::2], xt32[(1 - p)::2]], axis=0))
        xs = np.zeros((128, D), f); xs[:64] = x_sample[16 * core:16 * core + 16].reshape(64, D)
        m["xs"] = xs
        cT = np.zeros((D, 32), f); cT[:, 0] = inp["c_prompt"][b]; cT[:, 1:17] = inp["c_sample"][16 * core:16 * core + 16].T
        m["cT"] = cT
        m["ptab"] = np.ascontiguousarray(inp["page_table"][16 * core:16 * core + 16], dtype=np.int32).reshape(256)
        pos = np.zeros((NS, 128), f)
        for i in range(16):
            pos[i] = (2 * i + p) * 128 + np.arange(128)
            pos[16 + i] = (2 * i + 1 - p) * 128 + np.arange(128)
        pos[SMP, :64] = 2048 + (np.arange(64) % 4)
        ang = pos[:, :, None].astype(f) * inv[None, None, :]
        m["cosT"] = np.cos(ang).astype(f); m["sinT"] = np.sin(ang).astype(f)
        mo = np.zeros((128, 256), f)
        mo[:, 0:128] = np.where(kk <= tq, 0.0, MNEG)
        mo[:, 128:256] = 0.0 if p == 1 else MNEG
        m["maskOwn"] = mo
        maps.append(m)
    return maps


_CACHE = {}


def kernel(**inputs):
    inp = {k: np.asarray(v) for k, v in inputs.items()}
    maps = _host_inputs(inp)
    if "nc" not in _CACHE:
        _CACHE["nc"] = build_program()[0]
    nc = _CACHE["nc"]
    res = run_bass_kernel_spmd(nc, maps, core_ids=list(range(8)))
    R = res.results
    y_prompt = np.zeros((4, 4096, D), np.float32); y_sample = np.zeros((128, 4, D), np.float32)
    k_prompt = np.zeros((4, 32, 4, 128, 64), np.float32); v_prompt = np.zeros_like(k_prompt)
    k_sample = np.zeros((128, 4, 4, 64), np.float32); v_sample = np.zeros_like(k_sample)
    a_v = np.zeros((2, 128, 4, 2048), np.float32)
    for core in range(8):
        b, p = core // 2, core % 2
        r = R[core]
        y_prompt[b].reshape(32, 128, D)[p::2] = r["y_p"]
        y_sample[16 * core:16 * core + 16] = r["y_s"][:64].reshape(16, 4, D)
        k_prompt[b, p::2] = r["kp_o"]; v_prompt[b, p::2] = r["vp_o"]
        k_sample[16 * core:16 * core + 16] = r["ks_o"]; v_sample[16 * core:16 * core + 16] = r["vs_o"]
        a_v[:, 16 * core:16 * core + 16] = r["av_o"].reshape(2, 16, 4, 2048)
    return (y_prompt, y_sample, k_prompt, v_prompt, k_sample, v_sample, a_v)
```

```python
import numpy as np
from contextlib import ExitStack
import concourse.bass as bass
import concourse.mybir as mybir
from concourse.bass_utils import run_bass_kernel_spmd

F32 = mybir.dt.float32
BF16 = mybir.dt.bfloat16
U32 = mybir.dt.uint32
AF = mybir.ActivationFunctionType
ALU = mybir.AluOpType
AX = mybir.AxisListType

NS = 33
SMP = 32
TILES_A = list(range(33))
TILES_B = list(range(16)) + [32]
MNEG = -30000.0
D = 1024
EPS = 1e-6
NEG = -1.0e30


class Sched:
    COMPUTE = ("pe", "act", "dve", "pool")

    def __init__(self, nc, n_dma_sems=8):
        self.nc = nc
        self.eng = {"pe": nc.tensor, "act": nc.scalar, "dve": nc.vector, "pool": nc.gpsimd, "sp": nc.sync}
        self.sem = {}
        self.cnt = {}
        for e in self.COMPUTE:
            self.sem[e] = nc.alloc_semaphore("sem_" + e)
            self.cnt[e] = 0
        self.dsem = {}
        self.dcnt = {}
        self.drot = {}
        for q in ("sp", "pool"):
            self.dsem[q] = [nc.alloc_semaphore("dsem_%s_%d" % (q, i)) for i in range(n_dma_sems)]
            self.dcnt[q] = [0] * n_dma_sems
            self.drot[q] = 0
        self.seen = {e: {} for e in self.eng}
        self.regions = {}
        self.n_instr = 0
        self.n_wait = 0
        self._pend_r = []
        self._pend_w = []

    def _semh(self, key):
        return self.sem[key[1]] if key[0] == "c" else self.dsem[key[1]][key[2]]

    def _wait(self, eng, deps):
        best = {}
        for k, v in deps:
            if v > best.get(k, 0):
                best[k] = v
        for k, v in best.items():
            if eng == "pe" and k == ("c", "pe"):
                continue
            if self.seen[eng].get(k, 0) >= v:
                continue
            self.eng[eng].wait_ge(self._semh(k), v)
            self.seen[eng][k] = v
            self.n_wait += 1

    def _deps(self, reads, writes):
        deps = []
        for r in reads:
            reg = self.regions.get(r)
            if reg is None:
                continue
            deps += reg[0]
            if reg[2]:
                deps += reg[1]
        for w in writes:
            reg = self.regions.get(w)
            if reg is None:
                continue
            deps += reg[0]
            deps += reg[1]
        return deps

    def _record(self, dep, reads, writes):
        for r in reads:
            reg = self.regions.setdefault(r, [[], [], isinstance(r, str) and r.startswith("ps")])
            if reg[2]:
                reg[0] = [dep]
                reg[1] = []
            else:
                reg[1] = [d for d in reg[1] if d[0] != dep[0]] + [dep]
        for w in writes:
            reg = self.regions.setdefault(w, [[], [], isinstance(w, str) and w.startswith("ps")])
            reg[0] = [dep]
            reg[1] = []

    def op(self, eng, fn, reads=(), writes=()):
        reads = list(reads)
        writes = list(writes)
        self._wait(eng, self._deps(reads, writes))
        ins = fn(self.eng[eng])
        self.cnt[eng] += 1
        ins.then_inc(self.sem[eng], 1)
        self._record((("c", eng), self.cnt[eng]), reads, writes)
        self.n_instr += 1
        return ins

    def mm(self, fn, reads=(), writes=(), last=True):
        reads = list(reads)
        writes = list(writes)
        self._wait("pe", self._deps(reads, writes))
        ins = fn(self.eng["pe"])
        self.n_instr += 1
        if last:
            self.cnt["pe"] += 1
            ins.then_inc(self.sem["pe"], 1)
            self._record((("c", "pe"), self.cnt["pe"]), reads + self._pend_r, writes + self._pend_w)
            self._pend_r = []
            self._pend_w = []
        else:
            self._pend_r += reads
            self._pend_w += writes
        return ins

    def dma(self, q, out, in_, reads=(), writes=(), **kw):
        reads = list(reads)
        writes = list(writes)
        i = self.drot[q]
        self.drot[q] = (i + 1) % len(self.dsem[q])
        key = ("d", q, i)
        deps = self._deps(reads, writes)
        if self.dcnt[q][i] > 0:
            deps.append((key, self.dcnt[q][i]))
        self._wait(q, deps)
        ins = self.eng[q].dma_start(out=out, in_=in_, **kw)
        self.dcnt[q][i] += 16
        ins.then_inc(self.dsem[q][i], 16)
        self._record((key, self.dcnt[q][i]), reads, writes)
        self.n_instr += 1

    def idma(self, out, in_, idx_ap, reads=(), writes=()):
        q = "pool"
        reads = list(reads)
        writes = list(writes)
        i = self.drot[q]
        self.drot[q] = (i + 1) % len(self.dsem[q])
        key = ("d", q, i)
        deps = self._deps(reads, writes)
        if self.dcnt[q][i] > 0:
            deps.append((key, self.dcnt[q][i]))
        self._wait(q, deps)
        ins = self.eng[q].indirect_dma_start(out=out, out_offset=None, in_=in_,
                                             in_offset=bass.IndirectOffsetOnAxis(ap=idx_ap, axis=0))
        self.dcnt[q][i] += 16
        ins.then_inc(self.dsem[q][i], 16)
        self._record((key, self.dcnt[q][i]), reads, writes)
        self.n_instr += 1

    def _all(self):
        deps = [(("c", e), self.cnt[e]) for e in self.COMPUTE if self.cnt[e] > 0]
        for q in self.dsem:
            for i, c in enumerate(self.dcnt[q]):
                if c > 0:
                    deps.append((("d", q, i), c))
        return deps

    def barrier(self):
        deps = self._all()
        for e in self.eng:
            self._wait(e, deps)
        self.regions = {}

    finish = barrier


def build_program(n_layers=4, do_peer=True, do_attn=True):
    nc = bass.Bass("TRN2", target_bir_lowering=False)

    def din(name, shape, dt=F32):
        return nc.dram_tensor(name, list(shape), dt, kind="ExternalInput").ap()

    def dout(name, shape, dt=F32):
        return nc.dram_tensor(name, list(shape), dt, kind="ExternalOutput").ap()

    xp = din("xp", [32, 128, D]); xs = din("xs", [128, D]); cT = din("cT", [D, 32])
    Ep = din("Ep", [32, 128]); Es = din("Es", [32, 128])
    ident = din("ident", [128, 128]); iota_in = din("iota", [128, 128])
    ada_w = din("ada_w", [4, D, 6 * D]); ada_b = din("ada_b", [4, 6 * D])
    norm1_g = din("norm1_g", [4, D]); norm2_g = din("norm2_g", [4, D])
    a_w_in = din("a_w_in", [2, D, 4096]); a_b_in = din("a_b_in", [2, 4096]); a_g_sgu = din("a_g_sgu", [2, 2048])
    wsTp = din("wsTp", [2, 128, 8, 128]); wsTs = din("wsTs", [2, 128, 8, 128])
    bsP = din("bsP", [2, 8, 128]); bsS = din("bsS", [2, 8, 128])
    maskP = din("maskP", [128, 128]); maskS = din("maskS", [128, 128])
    a_w_out = din("a_w_out", [2, 2048, D])
    kv_norm_g = din("kv_norm_g", [D]); w_kv = din("w_kv", [D, 512]); k_norm_g = din("k_norm_g", [64])
    peer_w_q = din("peer_w_q", [4, D, 2048]); skT = din("skT", [4, 2, 128, 128])
    peer_uT = din("peer_uT", [4, D, 16384]); peer_v = din("peer_v", [4, 16384, D])
    cosT = din("cosT", [NS, 128, 8]); sinT = din("sinT", [NS, 128, 8])
    b_w_q = din("b_w_q", [2, D, D]); b_q_norm_g = din("b_q_norm_g", [2, 64]); b_w_o = din("b_w_o", [2, D, D])
    cache_k = din("cache_k", [2560 * 512, 64]); cache_v = din("cache_v", [2560 * 512, 64])
    ptab = din("ptab", [256], mybir.dt.int32)
    iotap_in = din("iotap", [128, 1]); SelP_in = din("SelP", [32, 16]); SelS_in = din("SelS", [16, 8])
    maskOwn_in = din("maskOwn", [128, 256]); maskOwnS_in = din("maskOwnS", [128, 128]); rowmask_in = din("rowmask", [128, 16])

    y_p = dout("y_p", [16, 128, D]); y_s = dout("y_s", [128, D])
    kp_o = dout("kp_o", [16, 4, 128, 64]); vp_o = dout("vp_o", [16, 4, 128, 64])
    ks_o = dout("ks_o", [16, 4, 4, 64]); vs_o = dout("vs_o", [16, 4, 4, 64])
    av_o = dout("av_o", [2, 64, 2048])

    xd = nc.dram_tensor("xd", [NS, 128, D], F32, kind="Internal").ap()
    kT_d = nc.dram_tensor("kT_d", [NS, 2, 128, 128], BF16, kind="Internal").ap()
    v_d = nc.dram_tensor("v_d", [NS, 128, 256], BF16, kind="Internal").ap()
    ks_d = nc.dram_tensor("ks_d", [NS, 256], F32, kind="Internal").ap()
    kTs_d = nc.dram_tensor("kTs_d", [16, 16, 2, 128, 128], BF16, kind="Internal").ap()
    vS_d = nc.dram_tensor("vS_d", [16, 16, 128, 256], BF16, kind="Internal").ap()
    ksS_d = nc.dram_tensor("ksS_d", [16, 16, 256], F32, kind="Internal").ap()
    hT_d = nc.dram_tensor("hT_d", [NS, 128, 8, 128], BF16, kind="Internal").ap()
    G_d = nc.dram_tensor("G_d", [NS, 128, 128, 128], BF16, kind="Internal").ap()

    S = Sched(nc)
    V = lambda fn, r=(), w=(): S.op("dve", fn, r, w)
    A = lambda fn, r=(), w=(): S.op("act", fn, r, w)

    def sb(name, shape, dt=F32):
        return nc.alloc_sbuf_tensor(name, list(shape), dt)

    PS = [nc.alloc_psum_tensor("psb%d" % i, [128, 512], F32) for i in range(7)]
    PSB = nc.alloc_psum_tensor("psbf", [128, 1024], BF16)
    psk = lambda i: "ps%d" % i

    idf = sb("idf", [128, 128]); idb = sb("idb", [128, 128], BF16)
    iof = sb("iof", [128, 128]); iob = sb("iob", [128, 128], BF16)
    ones1 = sb("ones1", [1, 128])
    Ept = sb("Ept", [32, 128]); Est = sb("Est", [32, 128])
    cTt = sb("cTt", [128, 8, 32]); scT = sb("scT", [128, 8, 32], BF16)
    S.dma("sp", idf[:], ident, writes=["idf"])
    S.dma("sp", iof[:], iota_in, writes=["iof"])
    S.dma("sp", Ept[:], Ep, writes=["Ept"])
    S.dma("sp", Est[:], Es, writes=["Est"])
    S.dma("sp", cTt[:], cT.rearrange("(k p) m -> p k m", p=128), writes=["cTt"])
    V(lambda e: e.tensor_copy(out=idb[:], in_=idf[:]), ["idf"], ["idb"])
    V(lambda e: e.tensor_copy(out=iob[:], in_=iof[:]), ["iof"], ["iob"])
    V(lambda e: e.memset(ones1[:], 1.0), [], ["ones1"])
    A(lambda e: e.activation(out=scT[:], in_=cTt[:], func=AF.Silu), ["cTt"], ["scT"])

    modseq = sb("modseq", [32, 3 * D])
    Aseq = sb("Aseq", [32, D])
    modtok = [sb("modtok%d" % i, [128, D]) for i in range(6)]

    uid = [0]

    def phase_locals():
        ph = ExitStack()
        uid[0] += 1
        u = uid[0]
        return ph, (lambda name, shape, dt=F32: ph.enter_context(nc.sbuf_tensor("%s_%d" % (name, u), list(shape), dt)))

    def adaln(l, half):
        S.barrier()
        ph, L = phase_locals()
        adab = L("adab", [1, 512]); gbt = L("gbt", [32, D])
        wa = [L("wa0", [128, 8, 512], BF16), L("wa1", [128, 8, 512], BF16)]
        S.dma("sp", gbt[:], (norm1_g if half == 0 else norm2_g)[l].partition_broadcast(32), writes=["gbt"])
        for n in range(6):
            col = half * 3072 + n * 512
            w = wa[n % 2]
            S.dma("pool", w[:], ada_w[l, :, col:col + 512].rearrange("(k p) n -> p k n", p=128), writes=["wa%d" % (n % 2)])
            S.dma("sp", adab[:], ada_b[l:l + 1, col:col + 512], writes=["adab"])
            bank = PS[n % 2]
            for kc in range(8):
                S.mm(lambda e: e.matmul(bank[0:32, :], lhsT=scT[:, kc, :], rhs=w[:, kc, :], start=(kc == 0), stop=False),
                     ["scT", "wa%d" % (n % 2)], [psk(n % 2)], last=False)
            S.mm(lambda e: e.matmul(bank[0:32, :], lhsT=ones1[0:1, 0:32], rhs=adab[0:1, :], start=False, stop=True),
                 ["ones1", "adab"], [psk(n % 2)])
            A(lambda e: e.copy(out=modseq[:, n * 512:(n + 1) * 512], in_=bank[0:32, :]), [psk(n % 2)], ["modseq"])
        V(lambda e: e.scalar_tensor_tensor(out=Aseq[:], in0=modseq[:, D:2 * D], scalar=1.0, in1=gbt[:],
                                           op0=ALU.add, op1=ALU.mult), ["modseq", "gbt"], ["Aseq"])
        expand_mod()
        S.barrier()
        ph.close()

    def expand_mod():
        srcs = [Aseq[:], modseq[:, 0:D], modseq[:, 2 * D:3 * D]]
        k = 0
        for gi, Et in enumerate((Ept, Est)):
            for vi in range(3):
                for hf in range(2):
                    bank = PS[k % 2]
                    S.mm(lambda e: e.matmul(bank[:], lhsT=Et[:], rhs=srcs[vi][:, hf * 512:(hf + 1) * 512],
                                            start=True, stop=True),
                         ["Ept", "Est", "Aseq", "modseq"], [psk(k % 2)])
                    A(lambda e: e.copy(out=modtok[gi * 3 + vi][:, hf * 512:(hf + 1) * 512], in_=bank[:]),
                      [psk(k % 2)], ["modtok%d" % (gi * 3 + vi)])
                    k += 1

    xt = [sb("xt0", [128, D]), sb("xt1", [128, D])]
    st = sb("stat", [128, 8])
    hb = sb("hb", [128, D], BF16)
    hT = sb("hT", [128, 8, 128], BF16)
    tmp = sb("tmp", [128, D])

    def load_x(t, first):
        xb = xt[t % 2]
        src = (xp[t] if t < SMP else xs) if first else xd[t]
        S.dma("sp", xb[:], src, reads=["xd%d" % t], writes=["xt%d" % (t % 2)])
        return xb

    def store_x(t):
        S.dma("sp", xd[t], xt[t % 2][:], reads=["xt%d" % (t % 2)], writes=["xd%d" % t])

    def modnorm_T(xb, xkey, At, Bt, akey, bkey):
        A(lambda e: e.activation(out=tmp[:], in_=xb[:], func=AF.Square, accum_out=st[:, 0:1]), [xkey], ["tmp", "st"])
        A(lambda e: e.activation(out=st[:, 1:2], in_=st[:, 0:1], func=AF.Sqrt, scale=1.0 / D, bias=EPS), ["st"], ["st"])
        V(lambda e: e.reciprocal(out=st[:, 2:3], in_=st[:, 1:2]), ["st"], ["st"])
        if Bt is not None:
            V(lambda e: e.scalar_tensor_tensor(out=tmp[:], in0=xb[:], scalar=st[:, 2:3], in1=At, op0=ALU.mult, op1=ALU.mult),
              [xkey, "st", akey], ["tmp"])
            V(lambda e: e.tensor_tensor(out=hb[:], in0=tmp[:], in1=Bt, op=ALU.add), ["tmp", bkey], ["hb"])
        else:
            V(lambda e: e.scalar_tensor_tensor(out=hb[:], in0=xb[:], scalar=st[:, 2:3], in1=At, op0=ALU.mult, op1=ALU.mult),
              [xkey, "st", akey], ["hb"])
        for k in range(8):
            S.mm(lambda e: e.transpose(out=PSB[:, k * 128:(k + 1) * 128], in_=hb[:, k * 128:(k + 1) * 128], identity=idb[:]),
                 ["hb", "idb"], ["psT"], last=(k == 7))
        A(lambda e: e.copy(out=hT[:].rearrange("p k t -> p (k t)"), in_=PSB[:]), ["psT"], ["hT"])

    def resid_add(xb, xkey, banks, bkeys, Gt, gkey):
        for hf in range(2):
            V(lambda e: e.tensor_tensor(out=tmp[:, hf * 512:(hf + 1) * 512], in0=banks[hf][:], in1=Gt[:, hf * 512:(hf + 1) * 512],
                                        op=ALU.mult), [bkeys[hf], gkey], ["tmp"])
        V(lambda e: e.tensor_tensor(out=xb[:], in0=xb[:], in1=tmp[:], op=ALU.add), ["tmp", xkey], [xkey])

    def gmlp_phase(l, first):
        S.barrier()
        ph, L = phase_locals()
        w_in = L("w_in", [128, 8, 4096], BF16)
        wo = [L("wo0", [128, 4, D], BF16), L("wo1", [128, 4, D], BF16)]
        brow = L("brow", [1, 4096])
        binu = L("binu", [128, 16])
        gsg = L("gsg", [128, 2048])
        wsPb = L("wsPb", [128, 8, 128], BF16); wsSb = L("wsSb", [128, 8, 128], BF16)
        mP = L("mP", [128, 128], BF16); mS = L("mS", [128, 128], BF16)
        vf = L("vf", [128, 2048]); vb = L("vb", [128, 2048], BF16)
        uT = L("uT", [128, 16, 128], BF16); yT = L("yT", [128, 16, 128], BF16)
        binv = brow[0:1, 0:2048]
        bsPt = brow[0:1, 2048:3072].rearrange("p (g t) -> p g t", t=128)
        bsSt = brow[0:1, 3072:4096].rearrange("p (g t) -> p g t", t=128)
        for k in range(8):
            S.dma("pool", w_in[:, k, :], a_w_in[l, k * 128:(k + 1) * 128, :], writes=["w_in"], max_dma_last_dim=8192)
        S.dma("sp", brow[0:1, 0:2048], a_b_in[l:l + 1, 2048:4096], writes=["brow"])
        S.dma("sp", brow[0:1, 2048:3072], bsP[l:l + 1].rearrange("o g t -> o (g t)"), writes=["brow"])
        S.dma("sp", brow[0:1, 3072:4096], bsS[l:l + 1].rearrange("o g t -> o (g t)"), writes=["brow"])
        S.dma("sp", binu[:], a_b_in[l, 0:2048].rearrange("(c p) -> p c", p=128), writes=["binu"], allow_slow_non_contiguous=True)
        S.dma("sp", gsg[:], a_g_sgu[l].partition_broadcast(128), writes=["gsg"])
        S.dma("pool", wsPb[:], wsTp[l], writes=["wsPb"]); S.dma("pool", wsSb[:], wsTs[l], writes=["wsSb"])
        S.dma("pool", mP[:], maskP, writes=["mP"]); S.dma("pool", mS[:], maskS, writes=["mS"])
        S.barrier()
        V(lambda e: e.tensor_tensor(out=wsPb[:], in0=wsPb[:], in1=mP[:].unsqueeze(1).to_broadcast([128, 8, 128]), op=ALU.mult),
          ["wsPb", "mP"], ["wsPb"])
        V(lambda e: e.tensor_tensor(out=wsSb[:], in0=wsSb[:], in1=mS[:].unsqueeze(1).to_broadcast([128, 8, 128]), op=ALU.mult),
          ["wsSb", "mS"], ["wsSb"])
        wo_n = [0]

        def load_wo(j):
            S.dma("pool", wo[wo_n[0] % 2][:], a_w_out[l, j * 512:(j + 1) * 512, :].rearrange("(c p) d -> p c d", p=128),
                  writes=["wo%d" % (wo_n[0] % 2)])
            wo_n[0] += 1
        for t in TILES_A:
            g = 0 if t < SMP else 1
            At, Bt, Gt = modtok[3 * g], modtok[3 * g + 1], modtok[3 * g + 2]
            ak, bk, gk = "modtok%d" % (3 * g), "modtok%d" % (3 * g + 1), "modtok%d" % (3 * g + 2)
            xkey = "xt%d" % (t % 2)
            xb = load_x(t, first)
            modnorm_T(xb, xkey, At[:], Bt[:], ak, bk)
            for n in range(4):
                for kc in range(8):
                    S.mm(lambda e: e.matmul(PS[n][:], lhsT=hT[:, kc, :], rhs=w_in[:, kc, 2048 + n * 512:2048 + (n + 1) * 512],
                                            start=(kc == 0), stop=False), ["hT", "w_in"], [psk(n)], last=False)
                S.mm(lambda e: e.matmul(PS[n][:], lhsT=ones1[0:1, :], rhs=binv[0:1, n * 512:(n + 1) * 512], start=False, stop=True),
                     ["ones1", "brow"], [psk(n)])
                A(lambda e: e.activation(out=vf[:, n * 512:(n + 1) * 512], in_=PS[n][:], func=AF.Gelu_apprx_tanh), [psk(n)], ["vf"])
            A(lambda e: e.activation(out=tmp[:], in_=vf[:, 0:1024], func=AF.Square, accum_out=st[:, 3:4]), ["vf"], ["tmp", "st"])
            A(lambda e: e.activation(out=tmp[:], in_=vf[:, 1024:2048], func=AF.Square, accum_out=st[:, 4:5]), ["vf"], ["tmp", "st"])
            V(lambda e: e.tensor_tensor(out=st[:, 3:4], in0=st[:, 3:4], in1=st[:, 4:5], op=ALU.add), ["st"], ["st"])
            A(lambda e: e.activation(out=st[:, 4:5], in_=st[:, 3:4], func=AF.Sqrt, scale=1.0 / 2048, bias=EPS), ["st"], ["st"])
            V(lambda e: e.reciprocal(out=st[:, 5:6], in_=st[:, 4:5]), ["st"], ["st"])
            V(lambda e: e.scalar_tensor_tensor(out=vf[:], in0=vf[:], scalar=st[:, 5:6], in1=gsg[:], op0=ALU.mult, op1=ALU.mult),
              ["vf", "st", "gsg"], ["vf"])
            V(lambda e: e.tensor_copy(out=vb[:], in_=vf[:]), ["vf"], ["vb"])
            if t == SMP:
                S.dma("sp", av_o[l], vf[0:64, :], reads=["vf"])
            for c in range(16):
                bank = PS[3 + c // 4]
                for kc in range(8):
                    S.mm(lambda e: e.matmul(bank[:, (c % 4) * 128:(c % 4 + 1) * 128], lhsT=w_in[:, kc, c * 128:(c + 1) * 128],
                                            rhs=hT[:, kc, :], start=(kc == 0), stop=(kc == 7)),
                         ["hT", "w_in"], [psk(3 + c // 4)], last=(kc == 7))
                A(lambda e: e.activation(out=uT[:, c, :], in_=bank[:, (c % 4) * 128:(c % 4 + 1) * 128], func=AF.Gelu_apprx_tanh,
                                         bias=binu[:, c:c + 1]), [psk(3 + c // 4), "binu"], ["uT"])
            wsb = wsPb if t < SMP else wsSb
            bst = bsPt if t < SMP else bsSt
            for c in range(16):
                bank = PS[c // 4]
                gi = c // 2
                S.mm(lambda e: e.matmul(bank[:, (c % 4) * 128:(c % 4 + 1) * 128], lhsT=vb[:, c * 128:(c + 1) * 128],
                                        rhs=wsb[:, gi, :], start=True, stop=False), ["vb", "wsPb", "wsSb"], [psk(c // 4)], last=False)
                S.mm(lambda e: e.matmul(bank[:, (c % 4) * 128:(c % 4 + 1) * 128], lhsT=ones1[0:1, :], rhs=bst[0:1, gi, :],
                                        start=False, stop=True), ["ones1", "brow"], [psk(c // 4)], last=(c % 4 == 3))
                if c % 4 == 3:
                    cb = c // 4
                    V(lambda e: e.tensor_tensor(out=yT[:, cb * 4:(cb + 1) * 4, :].rearrange("p c t -> p (c t)"), in0=bank[:],
                                                in1=uT[:, cb * 4:(cb + 1) * 4, :].rearrange("p c t -> p (c t)"), op=ALU.mult),
                      [psk(cb), "uT"], ["yT"])
            for j in range(4):
                slot = wo_n[0] % 2
                load_wo(j)
                for cc in range(4):
                    c = j * 4 + cc
                    for hf in range(2):
                        S.mm(lambda e: e.matmul(PS[4 + hf][:], lhsT=yT[:, c, :], rhs=wo[slot][:, cc, hf * 512:(hf + 1) * 512],
                                                start=(c == 0), stop=(c == 15)), ["yT", "wo%d" % slot], [psk(4 + hf)],
                             last=(cc == 3 and hf == 1))
            resid_add(xb, xkey, [PS[4], PS[5]], [psk(4), psk(5)], Gt, gk)
            store_x(t)
        S.barrier()
        ph.close()

    def peer_phase(l):
        S.barrier()
        ph, L = phase_locals()
        wq = [L("wq0", [128, 8, 512], BF16), L("wq1", [128, 8, 512], BF16)]
        skt = L("skt", [128, 2, 128], BF16)
        RB = []
        for par in range(2):
            d = {}
            d["qT"] = L("qT%d" % par, [128, 16, 128], BF16)
            d["ssb"] = L("ssb%d" % par, [128, 16, 128]); d["ss2"] = L("ss2%d" % par, [128, 16, 128])
            d["sv"] = L("sv%d" % par, [128, 16, 16]); d["si"] = L("si%d" % par, [128, 16, 16], U32); d["sif"] = L("sif%d" % par, [128, 16, 16])
            d["fv"] = L("fv%d" % par, [128, 8, 16]); d["fi"] = L("fi%d" % par, [128, 8, 16], U32)
            d["fa"] = L("fa%d" % par, [128, 8, 16], U32); d["fb"] = L("fb%d" % par, [128, 8, 16], U32)
            d["faf"] = L("faf%d" % par, [128, 8, 16]); d["fbf"] = L("fbf%d" % par, [128, 8, 16])
            d["ex"] = L("ex%d" % par, [128, 8, 16]); d["zz"] = L("zz%d" % par, [128, 8]); d["rz"] = L("rz%d" % par, [128, 8])
            d["sel"] = L("sel%d" % par, [128, 3, 128]); d["selT"] = L("selT%d" % par, [128, 3, 128], BF16)
            RB.append(d)
        oh = L("oh", [128, 8, 16, 16])
        OIb = [L("OI0", [128, 32, 128], BF16), L("OI1", [128, 32, 128], BF16)]
        OJb = [L("OJ0", [128, 32, 128], BF16), L("OJ1", [128, 32, 128], BF16)]
        G = L("G", [128, 128, 128], BF16)
        S.dma("pool", skt[:], skT[l].rearrange("p d k -> d p k"), writes=["skt"])
        wq_n = [0]

        def load_wq(j):
            S.dma("pool", wq[wq_n[0] % 2][:], peer_w_q[l, :, j * 512:(j + 1) * 512].rearrange("(k p) n -> p k n", p=128),
                  writes=["wq%d" % (wq_n[0] % 2)])
            wq_n[0] += 1

        def tile_gen(ti, t):
            par = ti % 2
            R = RB[par]
            K = lambda name: "%s_%d" % (name, par)
            qT, ssb, ss2, sv, si, sif = R["qT"], R["ssb"], R["ss2"], R["sv"], R["si"], R["sif"]
            fv, fi, fa, fb, faf, fbf, ex, zz, rz, sel, selT = (R["fv"], R["fi"], R["fa"], R["fb"], R["faf"], R["fbf"], R["ex"],
                                                                R["zz"], R["rz"], R["sel"], R["selT"])
            cand = ssb[:].rearrange("p g k -> p (g k)").rearrange("p (h c) -> p h c", c=256)
            cand2 = ss2[:].rearrange("p g k -> p (g k)").rearrange("p (h c) -> p h c", c=256)
            g = 0 if t < SMP else 1
            At, Bt = modtok[3 * g], modtok[3 * g + 1]
            ak, bk = "modtok%d" % (3 * g), "modtok%d" % (3 * g + 1)
            xkey = "xt%d" % (t % 2)
            xb = load_x(t, False)
            modnorm_T(xb, xkey, At[:], Bt[:], ak, bk)
            S.dma("sp", hT_d[t], hT[:], reads=["hT"])
            for b4 in range(4):
                slot = wq_n[0] % 2
                load_wq(b4)
                bank = PS[2 + b4]
                for g4 in range(4):
                    for kc in range(8):
                        S.mm(lambda e: e.matmul(bank[:, g4 * 128:(g4 + 1) * 128], lhsT=wq[slot][:, kc, g4 * 128:(g4 + 1) * 128],
                                                rhs=hT[:, kc, :], start=(kc == 0), stop=(kc == 7)),
                             ["wq%d" % slot, "hT"], [psk(2 + b4)], last=(kc == 7 and g4 == 3))
                A(lambda e: e.copy(out=qT[:, b4 * 4:(b4 + 1) * 4, :].rearrange("p g t -> p (g t)"), in_=bank[:]),
                  [psk(2 + b4)], [K("qT%d" % b4)])
            yield
            SSB = [K("ssb%d" % gbk) for gbk in range(16)]
            SS2 = [K("ss2_%d" % gbk) for gbk in range(16)]
            for gbk in range(16):
                bank = PS[2 + gbk // 4]
                S.mm(lambda e: e.matmul(bank[:, (gbk % 4) * 128:(gbk % 4 + 1) * 128], lhsT=qT[:, gbk, :], rhs=skt[:, gbk % 2, :],
                                        start=True, stop=True), [K("qT%d" % (gbk // 4)), "skt"], [psk(2 + gbk // 4)], last=(gbk % 4 == 3))
                if gbk % 4 == 3:
                    b4 = gbk // 4
                    A(lambda e: e.copy(out=ssb[:, b4 * 4:(b4 + 1) * 4, :].rearrange("p g k -> p (g k)"), in_=bank[:]),
                      [psk(2 + b4)], SSB[b4 * 4:(b4 + 1) * 4])
            SVA = [K("sva%d" % gbk) for gbk in range(16)]; SVB = [K("svb%d" % gbk) for gbk in range(16)]
            SIA = [K("sia%d" % gbk) for gbk in range(16)]; SIB = [K("sib%d" % gbk) for gbk in range(16)]
            for gbk in range(16):
                V(lambda e: e.max(out=sv[:, gbk, 0:8], in_=ssb[:, gbk, :]), [SSB[gbk]], [SVA[gbk]])
            for gbk in range(16):
                V(lambda e: e.max_index(out=si[:, gbk, 0:8], in_max=sv[:, gbk, 0:8], in_values=ssb[:, gbk, :]), [SSB[gbk], SVA[gbk]], [SIA[gbk]])
            for gbk in range(16):
                V(lambda e: e.match_replace(out=ss2[:, gbk, :], in_to_replace=sv[:, gbk, 0:8], in_values=ssb[:, gbk, :], imm_value=NEG),
                  [SSB[gbk], SVA[gbk]], [SS2[gbk]])
            for gbk in range(16):
                V(lambda e: e.max(out=sv[:, gbk, 8:16], in_=ss2[:, gbk, :]), [SS2[gbk]], [SVB[gbk]])
            for gbk in range(16):
                V(lambda e: e.max_index(out=si[:, gbk, 8:16], in_max=sv[:, gbk, 8:16], in_values=ss2[:, gbk, :]), [SS2[gbk], SVB[gbk]], [SIB[gbk]])
            yield
            V(lambda e: e.tensor_copy(out=sif[:], in_=si[:]), SIA + SIB, [K("sif")])
            sv4 = sv[:].rearrange("p (h two) k -> p h two k", two=2)
            sif4 = sif[:].rearrange("p (h two) k -> p h two k", two=2)
            CND = [K("cand%d" % h) for h in range(8)]
            V(lambda e: e.tensor_tensor(out=cand.rearrange("p h (a b) -> p h a b", b=16),
                                        in0=sv4[:, :, 0, :].unsqueeze(3).to_broadcast([128, 8, 16, 16]),
                                        in1=sv4[:, :, 1, :].unsqueeze(2).to_broadcast([128, 8, 16, 16]), op=ALU.add),
              SVA + SVB, SSB + CND)
            FVA = [K("fva%d" % h) for h in range(8)]; FVB = [K("fvb%d" % h) for h in range(8)]
            FIA = [K("fia%d" % h) for h in range(8)]; FIB = [K("fib%d" % h) for h in range(8)]
            C2 = [K("c2_%d" % h) for h in range(8)]
            for h in range(8):
                V(lambda e: e.max(out=fv[:, h, 0:8], in_=cand[:, h, :]), [CND[h], SSB[2 * h], SSB[2 * h + 1]], [FVA[h]])
            for h in range(8):
                V(lambda e: e.max_index(out=fi[:, h, 0:8], in_max=fv[:, h, 0:8], in_values=cand[:, h, :]), [CND[h], FVA[h], SSB[2 * h], SSB[2 * h + 1]], [FIA[h]])
            for h in range(8):
                V(lambda e: e.match_replace(out=cand2[:, h, :], in_to_replace=fv[:, h, 0:8], in_values=cand[:, h, :], imm_value=NEG),
                  [CND[h], FVA[h], SSB[2 * h], SSB[2 * h + 1]], [C2[h], SS2[2 * h], SS2[2 * h + 1]])
            for h in range(8):
                V(lambda e: e.max(out=fv[:, h, 8:16], in_=cand2[:, h, :]), [C2[h], SS2[2 * h], SS2[2 * h + 1]], [FVB[h]])
            for h in range(8):
                V(lambda e: e.max_index(out=fi[:, h, 8:16], in_max=fv[:, h, 8:16], in_values=cand2[:, h, :]), [C2[h], FVB[h], SS2[2 * h], SS2[2 * h + 1]], [FIB[h]])
            yield
            V(lambda e: e.tensor_tensor(out=ex[:], in0=fv[:], in1=fv[:, :, 0:1].to_broadcast([128, 8, 16]), op=ALU.subtract),
              FVA + FVB, [K("ex")])
            A(lambda e: e.activation(out=ex[:], in_=ex[:], func=AF.Exp), [K("ex")], [K("ex")])
            V(lambda e: e.tensor_reduce(out=zz[:], in_=ex[:], axis=AX.X, op=ALU.add), [K("ex")], [K("zz")])
            V(lambda e: e.reciprocal(out=rz[:], in_=zz[:]), [K("zz")], [K("rz")])
            V(lambda e: e.tensor_tensor(out=sel[:, 2, :].rearrange("p (h k) -> p h k", k=16), in0=ex[:],
                                        in1=rz[:].unsqueeze(2).to_broadcast([128, 8, 16]), op=ALU.mult), [K("ex"), K("rz")], [K("sel2")])
            V(lambda e: e.tensor_single_scalar(out=fa[:], in_=fi[:], scalar=4, op=ALU.logical_shift_right), FIA + FIB, [K("fa")])
            V(lambda e: e.tensor_single_scalar(out=fb[:], in_=fi[:], scalar=15, op=ALU.bitwise_and), FIA + FIB, [K("fb")])
            V(lambda e: e.tensor_copy(out=faf[:], in_=fa[:]), [K("fa")], [K("faf")])
            V(lambda e: e.tensor_copy(out=fbf[:], in_=fb[:]), [K("fb")], [K("fbf")])
            io16 = iof[:, 0:16].unsqueeze(1).unsqueeze(1).to_broadcast([128, 8, 16, 16])
            for which, (ff, fk) in enumerate(((faf, K("faf")), (fbf, K("fbf")))):
                V(lambda e: e.tensor_tensor(out=oh[:], in0=ff[:].unsqueeze(3).to_broadcast([128, 8, 16, 16]), in1=io16, op=ALU.is_equal),
                  [fk, "iof"], ["oh"])
                V(lambda e: e.tensor_tensor(out=oh[:], in0=oh[:], in1=sif4[:, :, which, :].unsqueeze(2).to_broadcast([128, 8, 16, 16]),
                                            op=ALU.mult), ["oh", K("sif")], ["oh"])
                V(lambda e: e.tensor_reduce(out=sel[:, which, :].rearrange("p (h k) -> p h k", k=16), in_=oh[:], axis=AX.X, op=ALU.add),
                  ["oh"], [K("sel%d" % which)])
            for w3 in range(3):
                S.mm(lambda e: e.transpose(out=PS[6][:, w3 * 128:(w3 + 1) * 128], in_=sel[:, w3, :], identity=idf[:]),
                     [K("sel%d" % w3), "idf"], [psk(6)], last=(w3 == 2))
            A(lambda e: e.copy(out=selT[:].rearrange("p w t -> p (w t)"), in_=PS[6][:, 0:384]), [psk(6)], [K("selT")])
            yield
            for hf in range(4):
                t0 = hf * 32
                OI, OJ = OIb[hf % 2], OJb[hf % 2]
                ki, kj = "OI%d" % (hf % 2), "OJ%d" % (hf % 2)
                V(lambda e: e.tensor_tensor(out=OI[:], in0=iob[:].unsqueeze(1).to_broadcast([128, 32, 128]),
                                            in1=selT[:, 0, t0:t0 + 32].unsqueeze(2).to_broadcast([128, 32, 128]), op=ALU.is_equal),
                  ["iob", K("selT")], [ki])
                V(lambda e: e.tensor_tensor(out=OJ[:], in0=iob[:].unsqueeze(1).to_broadcast([128, 32, 128]),
                                            in1=selT[:, 1, t0:t0 + 32].unsqueeze(2).to_broadcast([128, 32, 128]), op=ALU.is_equal),
                  ["iob", K("selT")], [kj])
                S.op("pool", lambda e: e.tensor_tensor(out=OJ[:], in0=OJ[:], in1=selT[:, 2, t0:t0 + 32].unsqueeze(2).to_broadcast([128, 32, 128]),
                                                       op=ALU.mult), [kj, K("selT")], [kj])
                for q4 in range(8):
                    bank = PS[q4 % 2]
                    for tt in range(4):
                        tl = q4 * 4 + tt
                        S.mm(lambda e: e.matmul(bank[:, tt * 128:(tt + 1) * 128], lhsT=OJ[:, tl, :], rhs=OI[:, tl, :], start=True, stop=True),
                             [ki, kj], [psk(q4 % 2)], last=(tt == 3))
                    tg = t0 + q4 * 4
                    A(lambda e: e.copy(out=G[:, :, tg:tg + 4].rearrange("p i t -> p t i"),
                                       in_=bank[:].rearrange("p (t i) -> p t i", i=128)), [psk(q4 % 2)], ["G"])
                yield
            S.dma("sp", G_d[t], G[:], reads=["G"])

        prev = None
        for ti, t in enumerate(TILES_A if l < 2 else TILES_B):
            cur = tile_gen(ti, t)
            for step in range(4):
                next(cur)
                if prev is not None:
                    next(prev)
            if prev is not None:
                for _ in prev:
                    pass
            prev = cur
        for _ in prev:
            pass
        S.barrier()
        ph.close()

        tiles = TILES_A if l < 2 else TILES_B
        groups = []
        k0 = 0
        while k0 < len(tiles):
            gsz = 9 if (len(tiles) - k0) % 8 == 1 else 8
            groups.append(tiles[k0:k0 + gsz])
            k0 += gsz
        ph, L = phase_locals()
        hTg = L("hTg", [128, 8, 9 * 128], BF16)
        Yacc = L("Yacc", [128, 9, D])
        Gc = [L("Gc0", [128, 4, 9 * 128], BF16), L("Gc1", [128, 4, 9 * 128], BF16)]
        Ub = [L("Ub%d" % i_, [128, 8, 512], BF16) for i_ in range(3)]
        Vb = [L("Vb%d" % i_, [128, 4, D], BF16) for i_ in range(3)]
        ga = [L("ga0", [128, 512], BF16), L("ga1", [128, 512], BF16)]
        WT = L("WT", [128, 4, 512], BF16)
        cnt = [0, 0, 0]
        steps = [(gi, sc) for gi in range(len(groups)) for sc in range(32)]

        def prefetch(k):
            if k >= len(steps):
                return
            gi, sc = steps[k]
            su, sg = k % 3, k % 2
            S.dma("pool", Ub[su][:], peer_uT[l, :, sc * 512:(sc + 1) * 512].rearrange("(k p) e -> p k e", p=128), writes=["Ub%d" % su])
            S.dma("pool", Vb[su][:], peer_v[l, sc * 512:(sc + 1) * 512, :].rearrange("(c p) d -> p c d", p=128), writes=["Vb%d" % su])
            for a_, t in enumerate(groups[gi]):
                S.dma("sp", Gc[sg][:, :, a_ * 128:(a_ + 1) * 128], G_d[t][:, sc * 4:(sc + 1) * 4, :], writes=["Gc%d" % sg])

        prefetch(0)
        for gi, grp in enumerate(groups):
            ntl = len(grp)
            for a_, t in enumerate(grp):
                S.dma("sp", hTg[:, :, a_ * 128:(a_ + 1) * 128], hT_d[t], writes=["hTg"])
            V(lambda e: e.memset(Yacc[:], 0.0), [], ["Yacc"])
            for sc in range(32):
                k = cnt[0]
                cnt[0] += 1
                s_ = k % 3
                sg_ = k % 2
                prefetch(k + 1)
                for sub in range(0, ntl, 4):
                    nsub = min(4, ntl - sub)
                    N = nsub * 128
                    tok0 = sub * 128
                    for ci in range(4):
                        bi = cnt[1] % 2
                        cnt[1] += 1
                        bank = PS[2 + bi]
                        for kc in range(8):
                            S.mm(lambda e: e.matmul(bank[:, 0:N], lhsT=Ub[s_][:, kc, ci * 128:(ci + 1) * 128], rhs=hTg[:, kc, tok0:tok0 + N],
                                                    start=(kc == 0), stop=(kc == 7)), ["Ub%d" % s_, "hTg"], [psk(2 + bi)], last=(kc == 7))
                        A(lambda e: e.activation(out=ga[bi][:, 0:N], in_=bank[:, 0:N], func=AF.Gelu_apprx_tanh), [psk(2 + bi)], ["ga%d" % bi])
                        S.op("pool", lambda e: e.tensor_tensor(out=WT[:, ci, 0:N], in0=ga[bi][:, 0:N], in1=Gc[sg_][:, ci, tok0:tok0 + N], op=ALU.mult),
                             ["ga%d" % bi, "Gc%d" % sg_], ["WT%d" % ci])
                    for a_ in range(nsub):
                        yb = cnt[2] % 2
                        cnt[2] += 1
                        ybank = [PS[0], PS[1]] if yb == 0 else [PS[4], PS[5]]
                        ykey = [psk(0), psk(1)] if yb == 0 else [psk(4), psk(5)]
                        for ci in range(4):
                            for hf in range(2):
                                S.mm(lambda e: e.matmul(ybank[hf][:], lhsT=WT[:, ci, a_ * 128:(a_ + 1) * 128], rhs=Vb[s_][:, ci, hf * 512:(hf + 1) * 512],
                                                        start=(ci == 0), stop=(ci == 3)), ["WT%d" % ci, "Vb%d" % s_], [ykey[hf]],
                                     last=(ci == 3 and hf == 1))
                        ta = sub + a_
                        for hf in range(2):
                            V(lambda e: e.tensor_tensor(out=Yacc[:, ta, hf * 512:(hf + 1) * 512], in0=ybank[hf][:], in1=Yacc[:, ta, hf * 512:(hf + 1) * 512],
                                                        op=ALU.add), [ykey[hf], "Yacc"], ["Yacc"])
            for a_, t in enumerate(grp):
                g = 0 if t < SMP else 1
                Gt, gk = modtok[3 * g + 2], "modtok%d" % (3 * g + 2)
                xkey = "xt%d" % (t % 2)
                xb = load_x(t, False)
                V(lambda e: e.tensor_tensor(out=tmp[:], in0=Yacc[:, a_, :], in1=Gt[:], op=ALU.mult), ["Yacc", gk], ["tmp"])
                V(lambda e: e.tensor_tensor(out=xb[:], in0=xb[:], in1=tmp[:], op=ALU.add), ["tmp", xkey], [xkey])
                store_x(t)
        S.barrier()
        ph.close()

    def kv_phase():
        S.barrier()
        ph, L = phase_locals()
        wkv = L("wkv", [128, 8, 512], BF16)
        gkv = L("gkv", [128, D]); gk64 = L("gk64", [128, 64])
        kvf = L("kvf", [128, 512]); ksq = L("ksq", [128, 256]); kst = L("kst", [128, 8])
        cs = L("cs", [128, 8]); sn = L("sn", [128, 8])
        r1 = L("r1", [128, 4, 8]); r2 = L("r2", [128, 4, 8]); r3 = L("r3", [128, 4, 8]); r4 = L("r4", [128, 4, 8])
        S.dma("pool", wkv[:], w_kv.rearrange("(k p) n -> p k n", p=128), writes=["wkv"])
        S.dma("sp", gkv[:], kv_norm_g.partition_broadcast(128), writes=["gkv"])
        S.dma("sp", gk64[:], k_norm_g.partition_broadcast(128), writes=["gk64"])
        kvb = L("kvb", [128, 512], BF16); ktl = L("ktl", [128, 2, 128], BF16); ksr = L("ksr", [1, 256])
        onesc = L("onesc", [128, 1])
        V(lambda e: e.memset(onesc[:], 1.0), [], ["onesc"])
        for t in TILES_A:
            xkey = "xt%d" % (t % 2)
            xb = load_x(t, False)
            modnorm_T(xb, xkey, gkv[:], None, "gkv", None)
            S.dma("sp", cs[:], cosT[t], writes=["cs"]); S.dma("sp", sn[:], sinT[t], writes=["sn"])
            for kc in range(8):
                S.mm(lambda e: e.matmul(PS[0][:], lhsT=hT[:, kc, :], rhs=wkv[:, kc, :], start=(kc == 0), stop=(kc == 7)),
                     ["hT", "wkv"], [psk(0)], last=(kc == 7))
            A(lambda e: e.copy(out=kvf[:], in_=PS[0][:]), [psk(0)], ["kvf"])
            k3 = kvf[:, 0:256].rearrange("p (h d) -> p h d", d=64)
            V(lambda e: e.tensor_tensor(out=ksq[:], in0=kvf[:, 0:256], in1=kvf[:, 0:256], op=ALU.mult), ["kvf"], ["ksq"])
            V(lambda e: e.tensor_reduce(out=kst[:, 0:4], in_=ksq[:].rearrange("p (h d) -> p h d", d=64), axis=AX.X, op=ALU.add), ["ksq"], ["kst"])
            A(lambda e: e.activation(out=kst[:, 4:8], in_=kst[:, 0:4], func=AF.Sqrt, scale=1.0 / 64, bias=EPS), ["kst"], ["kst"])
            V(lambda e: e.reciprocal(out=kst[:, 0:4], in_=kst[:, 4:8]), ["kst"], ["kst"])
            V(lambda e: e.tensor_tensor(out=k3, in0=k3, in1=kst[:, 0:4].unsqueeze(2).to_broadcast([128, 4, 64]), op=ALU.mult), ["kvf", "kst"], ["kvf"])
            V(lambda e: e.tensor_tensor(out=k3, in0=k3, in1=gk64[:].unsqueeze(1).to_broadcast([128, 4, 64]), op=ALU.mult), ["kvf", "gk64"], ["kvf"])
            cb = cs[:].unsqueeze(1).to_broadcast([128, 4, 8]); snb = sn[:].unsqueeze(1).to_broadcast([128, 4, 8])
            V(lambda e: e.tensor_tensor(out=r1[:], in0=k3[:, :, 0:8], in1=cb, op=ALU.mult), ["kvf", "cs"], ["r1"])
            V(lambda e: e.tensor_tensor(out=r2[:], in0=k3[:, :, 8:16], in1=snb, op=ALU.mult), ["kvf", "sn"], ["r2"])
            V(lambda e: e.tensor_tensor(out=r3[:], in0=k3[:, :, 8:16], in1=cb, op=ALU.mult), ["kvf", "cs"], ["r3"])
            V(lambda e: e.tensor_tensor(out=r4[:], in0=k3[:, :, 0:8], in1=snb, op=ALU.mult), ["kvf", "sn"], ["r4"])
            V(lambda e: e.tensor_tensor(out=k3[:, :, 0:8], in0=r1[:], in1=r2[:], op=ALU.subtract), ["r1", "r2", "kvf"], ["kvf"])
            V(lambda e: e.tensor_tensor(out=k3[:, :, 8:16], in0=r3[:], in1=r4[:], op=ALU.add), ["r3", "r4", "kvf"], ["kvf"])
            v3 = kvf[:, 256:512].rearrange("p (h d) -> p h d", d=64)
            V(lambda e: e.tensor_copy(out=kvb[:], in_=kvf[:]), ["kvf"], ["kvb"])
            for pr in range(2):
                S.mm(lambda e: e.transpose(out=PSB[:, pr * 128:(pr + 1) * 128], in_=kvb[:, pr * 128:(pr + 1) * 128], identity=idb[:]),
                     ["kvb", "idb"], ["psT"], last=(pr == 1))
            A(lambda e: e.copy(out=ktl[:].rearrange("p a t -> p (a t)"), in_=PSB[:, 0:256]), ["psT"], ["ktl"])
            S.dma("sp", kT_d[t].rearrange("a p t -> p a t"), ktl[:], reads=["ktl"])
            S.dma("sp", v_d[t], kvb[:, 256:512], reads=["kvb"])
            S.mm(lambda e: e.matmul(PS[1][0:1, 0:256], lhsT=onesc[:], rhs=kvf[:, 0:256], start=True, stop=True), ["onesc", "kvf"], [psk(1)])
            A(lambda e: e.copy(out=ksr[:], in_=PS[1][0:1, 0:256]), [psk(1)], ["ksr"])
            S.dma("sp", ks_d[t:t + 1, :], ksr[:], reads=["ksr"])
            if t >= 16 and t < SMP:
                continue
            if t < 16:
                S.dma("sp", kp_o[t].rearrange("h t d -> t h d"), k3, reads=["kvf"])
                S.dma("sp", vp_o[t].rearrange("h t d -> t h d"), v3, reads=["kvf"])
            else:
                for s_ in range(16):
                    S.dma("sp", ks_o[s_].rearrange("h t d -> t h d"), k3[s_ * 4:(s_ + 1) * 4], reads=["kvf"])
                    S.dma("sp", vs_o[s_].rearrange("h t d -> t h d"), v3[s_ * 4:(s_ + 1) * 4], reads=["kvf"])
        S.barrier()
        ph.close()

    def cache_phase():
        S.barrier()
        ph, L = phase_locals()
        I32 = mybir.dt.int32
        ptb = L("ptb", [128, 256], I32); ptf = L("ptf", [128, 256]); iop = L("iop", [128, 1])
        idxf = L("idxf", [128, 256, 4]); idxu = L("idxu", [128, 256, 4], U32)
        kc = [L("kc0", [128, 256]), L("kc1", [128, 256])]
        vc = [L("vc0", [128, 256]), L("vc1", [128, 256])]
        kcb = L("kcb", [128, 256], BF16); vcb = L("vcb", [128, 256], BF16)
        ktl = L("ktlc", [128, 2, 128], BF16); ksr = L("ksrc", [1, 256]); onesc = L("onescc", [128, 1])
        V(lambda e: e.memset(onesc[:], 1.0), [], ["onesc"])
        S.dma("sp", ptb[:], ptab.partition_broadcast(128), writes=["ptb"])
        S.dma("sp", iop[:], iotap_in, writes=["iop"])
        V(lambda e: e.tensor_copy(out=ptf[:], in_=ptb[:]), ["ptb"], ["ptf"])
        for kvh in range(4):
            V(lambda e: e.tensor_scalar(out=idxf[:, :, kvh], in0=ptf[:], scalar1=512.0, scalar2=float(kvh * 128), op0=ALU.mult, op1=ALU.add),
              ["ptf"], ["idxf"])
        V(lambda e: e.tensor_scalar(out=idxf[:], in0=idxf[:], scalar1=iop[:, 0:1], scalar2=None, op0=ALU.add), ["idxf", "iop"], ["idxf"])
        V(lambda e: e.tensor_copy(out=idxu[:], in_=idxf[:]), ["idxf"], ["idxu"])
        S.barrier()
        n = 0
        for sq_ in range(16):
            for pg in range(16):
                col = sq_ * 16 + pg
                sl = n % 2
                for kvh in range(4):
                    S.idma(kc[sl][:, kvh * 64:(kvh + 1) * 64], cache_k, idxu[:, col, kvh:kvh + 1], ["idxu"], ["kc%d" % sl])
                    S.idma(vc[sl][:, kvh * 64:(kvh + 1) * 64], cache_v, idxu[:, col, kvh:kvh + 1], ["idxu"], ["vc%d" % sl])
                V(lambda e: e.tensor_copy(out=kcb[:], in_=kc[sl][:]), ["kc%d" % sl], ["kcb"])
                A(lambda e: e.copy(out=vcb[:], in_=vc[sl][:]), ["vc%d" % sl], ["vcb"])
                for pr in range(2):
                    S.mm(lambda e: e.transpose(out=PSB[:, pr * 128:(pr + 1) * 128], in_=kcb[:, pr * 128:(pr + 1) * 128], identity=idb[:]),
                         ["kcb", "idb"], ["psT"], last=(pr == 1))
                A(lambda e: e.copy(out=ktl[:].rearrange("p a t -> p (a t)"), in_=PSB[:, 0:256]), ["psT"], ["ktl"])
                S.dma("sp", kTs_d[sq_, pg].rearrange("a p t -> p a t"), ktl[:], reads=["ktl"])
                S.dma("sp", vS_d[sq_, pg], vcb[:], reads=["vcb"])
                S.mm(lambda e: e.matmul(PS[1][0:1, 0:256], lhsT=onesc[:], rhs=kc[sl][:], start=True, stop=True), ["onesc", "kc%d" % sl], [psk(1)])
                A(lambda e: e.copy(out=ksr[:], in_=PS[1][0:1, 0:256]), [psk(1)], ["ksr"])
                S.dma("sp", ksS_d[sq_, pg:pg + 1, :], ksr[:], reads=["ksr"])
                n += 1
        S.barrier()
        ph.close()

    def attn_phase(l):
        j = l - 2
        S.barrier()
        ph, L = phase_locals()
        wq = L("awq", [128, 8, D], BF16); wo = L("awo", [128, 8, D], BF16)
        KT = L("KT", [128, 4, 32, 128], BF16)
        VV = L("VV", [128, 32, 256], BF16)
        KM = L("KM", [128, 4, 32], BF16)
        kss = L("kss", [32, 256]); ks2 = L("ks2", [32, 4, 2, 64])
        SelP = L("SelP", [32, 16]); SelS = L("SelS", [16, 8])
        gq64 = L("gq64", [128, 64]); mOwn = L("mOwn", [128, 256]); mOwnS = L("mOwnS", [128, 128]); rowm = L("rowm", [128, 16])
        qf = L("qf", [128, D]); qb = L("qb", [128, D], BF16); qT = L("qTa", [128, 8, 128], BF16)
        qst = L("qst", [128, 48])
        cs = L("acs", [128, 8]); sn = L("asn", [128, 8])
        r1 = L("ar1", [128, 16, 8]); r2 = L("ar2", [128, 16, 8]); r3 = L("ar3", [128, 16, 8]); r4 = L("ar4", [128, 16, 8])
        gate = L("gate", [128, 16, 16]); gm = L("gm", [128, 16, 16]); m8 = L("m8", [128, 16, 8]); mb = L("mb", [128, 16, 16])
        P = L("P", [128, 4096], BF16); PT = L("PT", [128, 32, 128], BF16)
        so = L("so", [128, 256])
        den = L("den", [128, 16, 136]); dsum = L("dsum", [128, 16]); drec = L("drec", [128, 16])
        Osb = L("Osb", [128, D]); ob = L("ob", [128, D], BF16); oT = L("oT", [128, 8, 128], BF16)
        S.dma("pool", wq[:], b_w_q[j].rearrange("(k p) n -> p k n", p=128), writes=["awq"])
        S.dma("pool", wo[:], b_w_o[j].rearrange("(k p) n -> p k n", p=128), writes=["awo"])
        S.dma("sp", gq64[:], b_q_norm_g[j].partition_broadcast(128), writes=["gq64"])
        S.dma("sp", mOwn[:], maskOwn_in, writes=["mOwn"]); S.dma("sp", mOwnS[:], maskOwnS_in, writes=["mOwnS"])
        S.dma("sp", rowm[:], rowmask_in, writes=["rowm"])
        S.dma("sp", SelP[:], SelP_in, writes=["SelP"]); S.dma("sp", SelS[:], SelS_in, writes=["SelS"])
        S.barrier()

        def load_ctx(kT_src, v_src, ks_src, nslots, interleave, Sel, nblk):
            parts = [(0, 16, 0, 2), (16, 32, 1, 2)] if interleave else [(0, nslots, 0, 1)]
            for (s0, s1, k0, kstep) in parts:
                nk = s1 - s0
                for kvh in range(4):
                    for base in (0, 64):
                        S.dma("sp", KT[base:base + 64, kvh, k0:k0 + kstep * (nk - 1) + 1:kstep, :],
                              kT_src[s0:s1, kvh // 2, (kvh % 2) * 64:(kvh % 2) * 64 + 64, :].rearrange("s d t -> d s t"),
                              reads=["kscr"], writes=["KT"])
                S.dma("sp", VV[:, k0:k0 + kstep * (nk - 1) + 1:kstep, :], v_src[s0:s1].rearrange("s k c -> k s c"), reads=["kscr"], writes=["VV"])
            S.dma("sp", kss[0:nslots, :], ks_src, reads=["kscr"], writes=["kss"])
            k3s = kss[0:nslots, :].rearrange("p (h d) -> p h d", d=64)
            V(lambda e: e.tensor_copy(out=ks2[0:nslots, :, 0, :], in_=k3s), ["kss"], ["ks2"])
            V(lambda e: e.tensor_copy(out=ks2[0:nslots, :, 1, :], in_=k3s), ["kss"], ["ks2"])
            V(lambda e: e.memset(KM[:], 0.0), [], ["KM"])
            for kvh in range(4):
                S.mm(lambda e: e.matmul(PS[2][:, 0:nblk], lhsT=ks2[0:nslots, kvh, :, :].rearrange("p a d -> p (a d)"), rhs=Sel[0:nslots, 0:nblk],
                                        start=True, stop=True), ["ks2", "SelP", "SelS"], [psk(2)])
                A(lambda e: e.copy(out=KM[0:64, kvh, 0:nblk], in_=PS[2][0:64, 0:nblk]), [psk(2)], ["KM"])
                A(lambda e: e.copy(out=KM[64:128, kvh, 16:16 + nblk], in_=PS[2][64:128, 0:nblk]), [psk(2)], ["KM"])

        def compute_q(t):
            for hf in range(2):
                for kc_ in range(8):
                    S.mm(lambda e: e.matmul(PS[hf][:], lhsT=hT[:, kc_, :], rhs=wq[:, kc_, hf * 512:(hf + 1) * 512], start=(kc_ == 0), stop=(kc_ == 7)),
                         ["hT", "awq"], [psk(hf)], last=(kc_ == 7))
                A(lambda e: e.copy(out=qf[:, hf * 512:(hf + 1) * 512], in_=PS[hf][:]), [psk(hf)], ["qf"])
            q3 = qf[:].rearrange("p (h d) -> p h d", d=64)
            V(lambda e: e.tensor_tensor(out=tmp[:], in0=qf[:], in1=qf[:], op=ALU.mult), ["qf"], ["tmp"])
            V(lambda e: e.tensor_reduce(out=qst[:, 0:16], in_=tmp[:].rearrange("p (h d) -> p h d", d=64), axis=AX.X, op=ALU.add), ["tmp"], ["qst"])
            A(lambda e: e.activation(out=qst[:, 16:32], in_=qst[:, 0:16], func=AF.Sqrt, scale=1.0 / 64, bias=EPS), ["qst"], ["qst"])
            V(lambda e: e.reciprocal(out=qst[:, 32:48], in_=qst[:, 16:32]), ["qst"], ["qst"])
            V(lambda e: e.tensor_tensor(out=q3, in0=q3, in1=qst[:, 32:48].unsqueeze(2).to_broadcast([128, 16, 64]), op=ALU.mult), ["qf", "qst"], ["qf"])
            V(lambda e: e.tensor_tensor(out=q3, in0=q3, in1=gq64[:].unsqueeze(1).to_broadcast([128, 16, 64]), op=ALU.mult), ["qf", "gq64"], ["qf"])
            S.dma("sp", cs[:], cosT[t], writes=["acs"]); S.dma("sp", sn[:], sinT[t], writes=["asn"])
            cb = cs[:].unsqueeze(1).to_broadcast([128, 16, 8]); snb = sn[:].unsqueeze(1).to_broadcast([128, 16, 8])
            V(lambda e: e.tensor_tensor(out=r1[:], in0=q3[:, :, 0:8], in1=cb, op=ALU.mult), ["qf", "acs"], ["ar1"])
            V(lambda e: e.tensor_tensor(out=r2[:], in0=q3[:, :, 8:16], in1=snb, op=ALU.mult), ["qf", "asn"], ["ar2"])
            V(lambda e: e.tensor_tensor(out=r3[:], in0=q3[:, :, 8:16], in1=cb, op=ALU.mult), ["qf", "acs"], ["ar3"])
            V(lambda e: e.tensor_tensor(out=r4[:], in0=q3[:, :, 0:8], in1=snb, op=ALU.mult), ["qf", "asn"], ["ar4"])
            V(lambda e: e.tensor_tensor(out=q3[:, :, 0:8], in0=r1[:], in1=r2[:], op=ALU.subtract), ["ar1", "ar2", "qf"], ["qf"])
            V(lambda e: e.tensor_tensor(out=q3[:, :, 8:16], in0=r3[:], in1=r4[:], op=ALU.add), ["ar3", "ar4", "qf"], ["qf"])
            V(lambda e: e.tensor_copy(out=qb[:], in_=qf[:]), ["qf"], ["qb"])
            for k in range(8):
                S.mm(lambda e: e.transpose(out=PSB[:, k * 128:(k + 1) * 128], in_=qb[:, k * 128:(k + 1) * 128], identity=idb[:]),
                     ["qb", "idb"], ["psT"], last=(k == 7))
            A(lambda e: e.copy(out=qT[:].rearrange("p k t -> p (k t)"), in_=PSB[:]), ["psT"], ["qTa"])

        def gates(nblk, nvalid, rowbias):
            for pr in range(8):
                S.mm(lambda e: e.matmul(PS[2][:, pr * 32:pr * 32 + 32], lhsT=qT[:, pr, :], rhs=KM[:, pr // 2, :], start=True, stop=True),
                     ["qTa", "KM"], [psk(2)], last=(pr == 7))
            V(lambda e: e.memset(gm[:], NEG), [], ["gm"])
            V(lambda e: e.tensor_copy(out=gm[:, :, 0:nvalid],
                                      in_=PS[2][:, 0:256].rearrange("p (pr two n) -> p (pr two) n", two=2, n=16)[:, :, 0:nvalid]),
              [psk(2)], ["gm"])
            for h in range(16):
                V(lambda e: e.max(out=m8[:, h, :], in_=gm[:, h, :]), ["gm"], ["m8"])
            V(lambda e: e.tensor_tensor(out=mb[:, :, 0:nvalid], in0=gm[:, :, 0:nvalid], in1=m8[:, :, 2:3].to_broadcast([128, 16, nvalid]), op=ALU.is_ge),
              ["gm", "m8"], ["mb"])
            V(lambda e: e.tensor_scalar(out=mb[:, :, 0:nvalid], in0=mb[:, :, 0:nvalid], scalar1=-1.0, scalar2=-MNEG, op0=ALU.add, op1=ALU.mult),
              ["mb"], ["mb"])
            if rowbias is not None:
                V(lambda e: e.tensor_scalar(out=mb[:, :, 0:nvalid], in0=mb[:, :, 0:nvalid], scalar1=rowbias, scalar2=None, op0=ALU.add), ["mb", "rowm"], ["mb"])

        def attend_head(h, blocks, dcol0, accumulate):
            pr, base, kvh = h // 2, (h % 2) * 64, h // 4
            ntile = 0
            bi = 0
            for (kt0, nkt, bias, emask) in blocks:
                bank = PS[2 + bi % 2]
                S.mm(lambda e: e.matmul(bank[:, 0:nkt * 128], lhsT=qT[base:base + 64, pr, :],
                                        rhs=KT[base:base + 64, kvh, kt0:kt0 + nkt, :].rearrange("p a k -> p (a k)"), start=True, stop=True),
                     ["qTa", "KT"], [psk(2 + bi % 2)])
                pdst = P[:, ntile * 128:(ntile + nkt) * 128]
                dcol = den[:, h, dcol0 + bi:dcol0 + bi + 1]
                if emask is not None:
                    V(lambda e: e.tensor_tensor(out=so[:, 0:nkt * 128], in0=bank[:, 0:nkt * 128], in1=emask, op=ALU.add), [psk(2 + bi % 2), "mOwn", "mOwnS"], ["so"])
                    A(lambda e: e.activation(out=pdst, in_=so[:, 0:nkt * 128], func=AF.Exp, scale=0.125, accum_out=dcol), ["so"], ["P", "den"])
                else:
                    A(lambda e: e.activation(out=pdst, in_=bank[:, 0:nkt * 128], func=AF.Exp, scale=0.125, bias=bias, accum_out=dcol),
                      [psk(2 + bi % 2), "mb"], ["P", "den"])
                ntile += nkt
                bi += 1
            for kt in range(ntile):
                S.mm(lambda e: e.transpose(out=PSB[:, (kt % 8) * 128:(kt % 8 + 1) * 128], in_=P[:, kt * 128:(kt + 1) * 128], identity=idb[:]),
                     ["P", "idb"], ["psT"], last=(kt % 8 == 7 or kt == ntile - 1))
                if kt % 8 == 7 or kt == ntile - 1:
                    k0 = (kt // 8) * 8
                    nk = kt - k0 + 1
                    V(lambda e: e.tensor_copy(out=PT[:, k0:k0 + nk, :].rearrange("p a t -> p (a t)"), in_=PSB[:, 0:nk * 128]), ["psT"], ["PT"])
            kti = 0
            for (kt0, nkt, bias, emask) in blocks:
                for a_ in range(nkt):
                    S.mm(lambda e: e.matmul(PS[6][:, 0:64], lhsT=PT[:, kti, :], rhs=VV[:, kt0 + a_, kvh * 64:(kvh + 1) * 64],
                                            start=(kti == 0), stop=(kti == ntile - 1)), ["PT", "VV"], [psk(6)], last=(kti == ntile - 1))
                    kti += 1
            if accumulate:
                V(lambda e: e.tensor_tensor(out=Osb[:, h * 64:(h + 1) * 64], in0=PS[6][:, 0:64], in1=Osb[:, h * 64:(h + 1) * 64], op=ALU.add),
                  [psk(6), "Osb"], ["Osb"])
            else:
                V(lambda e: e.tensor_copy(out=Osb[:, h * 64:(h + 1) * 64], in_=PS[6][:, 0:64]), [psk(6)], ["Osb"])

        def finish_tile(xb, xkey, Gt, gk):
            V(lambda e: e.tensor_reduce(out=dsum[:], in_=den[:], axis=AX.X, op=ALU.add), ["den"], ["dsum"])
            V(lambda e: e.tensor_scalar(out=dsum[:], in0=dsum[:], scalar1=1e-30, scalar2=None, op0=ALU.add), ["dsum"], ["dsum"])
            V(lambda e: e.reciprocal(out=drec[:], in_=dsum[:]), ["dsum"], ["drec"])
            V(lambda e: e.tensor_tensor(out=ob[:].rearrange("p (h d) -> p h d", d=64), in0=Osb[:].rearrange("p (h d) -> p h d", d=64),
                                        in1=drec[:].unsqueeze(2).to_broadcast([128, 16, 64]), op=ALU.mult), ["Osb", "drec"], ["ob"])
            for k in range(8):
                S.mm(lambda e: e.transpose(out=PSB[:, k * 128:(k + 1) * 128], in_=ob[:, k * 128:(k + 1) * 128], identity=idb[:]),
                     ["ob", "idb"], ["psT"], last=(k == 7))
            A(lambda e: e.copy(out=oT[:].rearrange("p k t -> p (k t)"), in_=PSB[:]), ["psT"], ["oT"])
            for hf in range(2):
                for c in range(8):
                    S.mm(lambda e: e.matmul(PS[4 + hf][:], lhsT=oT[:, c, :], rhs=wo[:, c, hf * 512:(hf + 1) * 512], start=(c == 0), stop=(c == 7)),
                         ["oT", "awo"], [psk(4 + hf)], last=(c == 7))
            resid_add(xb, xkey, [PS[4], PS[5]], [psk(4), psk(5)], Gt, gk)

        load_ctx(kT_d, v_d, ks_d[0:32, :], 32, True, SelP, 16)
        for i in range(16):
            xkey = "xt%d" % (i % 2)
            xb = load_x(i, False)
            modnorm_T(xb, xkey, modtok[0][:], modtok[1][:], "modtok0", "modtok1")
            compute_q(i)
            V(lambda e: e.memset(den[:], 0.0), [], ["den"])
            if i > 0:
                gates(16, i, None)
            for h in range(16):
                blocks = []
                n = 0
                while n < i:
                    if n + 1 < i:
                        blocks.append((2 * n, 2, mb[:, h, n:n + 1], None))
                        blocks.append((2 * n + 2, 2, mb[:, h, n + 1:n + 2], None))
                        n += 2
                    else:
                        blocks.append((2 * n, 2, mb[:, h, n:n + 1], None))
                        n += 1
                blocks.append((2 * i, 2, None, mOwn[:, 0:256]))
                attend_head(h, blocks, 0, False)
            finish_tile(xb, xkey, modtok[2], "modtok2")
            store_x(i)
        xkey = "xt%d" % (SMP % 2)
        xb = load_x(SMP, False)
        modnorm_T(xb, xkey, modtok[3][:], modtok[4][:], "modtok3", "modtok4")
        compute_q(SMP)
        V(lambda e: e.memset(den[:], 0.0), [], ["den"])
        for kvh in range(4):
            for base in (0, 64):
                S.dma("sp", KT[base:base + 64, kvh, 0, :], kT_d[SMP, kvh // 2, (kvh % 2) * 64:(kvh % 2) * 64 + 64, :], writes=["KT"])
        S.dma("sp", VV[:, 0, :], v_d[SMP], writes=["VV"])
        for h in range(16):
            attend_head(h, [(0, 1, None, mOwnS[:, 0:128])], 0, False)
        for sq_ in range(16):
            load_ctx(kTs_d[sq_], vS_d[sq_], ksS_d[sq_], 16, False, SelS, 8)
            gates(8, 8, rowm[:, sq_:sq_ + 1])
            for h in range(16):
                blocks = [(2 * n, 2, mb[:, h, n:n + 1], None) for n in range(8)]
                attend_head(h, blocks, 1 + sq_ * 8, True)
        finish_tile(xb, xkey, modtok[5], "modtok5")
        store_x(SMP)
        S.barrier()
        ph.close()

    first = True
    for l in range(n_layers):
        adaln(l, 0)
        if l < 2:
            gmlp_phase(l, first)
            first = False
        elif do_attn:
            attn_phase(l)
        if do_peer:
            adaln(l, 1)
            peer_phase(l)
        if l == 1:
            kv_phase()
            if do_attn and n_layers > 2:
                cache_phase()
    S.barrier()
    for t in TILES_B:
        xb = load_x(t, False)
        S.dma("sp", y_p[t] if t < SMP else y_s, xb[:], reads=["xt%d" % (t % 2)])
    S.finish()
    return nc, S


def _host_inputs(inp):
    f = np.float32
    c = lambda a: np.ascontiguousarray(a, dtype=f)
    x_prompt = inp["x_prompt"]; x_sample = inp["x_sample"]
    a_w_s = np.asarray(inp["a_w_s"], dtype=f); a_b_s = np.asarray(inp["a_b_s"], dtype=f)
    shared = {}
    for k in ("ada_w", "ada_b", "norm1_g", "norm2_g", "a_w_in", "a_b_in", "a_g_sgu", "a_w_out", "kv_norm_g", "w_kv",
              "k_norm_g", "peer_w_q", "peer_v", "b_w_q", "b_q_norm_g", "b_w_o"):
        shared[k] = c(inp[k])
    shared["cache_k"] = c(inp["cache_k"]).reshape(-1, 64)
    shared["cache_v"] = c(inp["cache_v"]).reshape(-1, 64)
    shared["peer_uT"] = c(np.transpose(inp["peer_u"], (0, 2, 1)))
    shared["skT"] = c(np.transpose(inp["peer_sub_keys"], (0, 1, 3, 2)))
    shared["wsTp"] = c(np.transpose(a_w_s, (0, 3, 1, 2)))
    wsTs = np.zeros((2, 128, 8, 128), f)
    bsS = np.zeros((2, 8, 128), f)
    for q in range(16):
        wsTs[:, q * 4:(q + 1) * 4, :, q * 4:(q + 1) * 4] = np.transpose(a_w_s[:, :, :4, :4], (0, 3, 1, 2))
        bsS[:, :, q * 4:(q + 1) * 4] = a_b_s[:, :, :4]
    shared["wsTs"] = wsTs
    shared["bsP"] = c(a_b_s)
    shared["bsS"] = bsS
    s_idx = np.arange(128)[:, None]; t_idx = np.arange(128)[None, :]
    shared["maskP"] = (s_idx <= t_idx).astype(f)
    shared["maskS"] = ((s_idx // 4 == t_idx // 4) & (s_idx % 4 <= t_idx % 4) & (s_idx < 64)).astype(f)
    shared["ident"] = np.eye(128, dtype=f)
    shared["iota"] = np.tile(np.arange(128, dtype=f)[None, :], (128, 1))
    shared["iotap"] = np.arange(128, dtype=f)[:, None].copy()
    Ep = np.zeros((32, 128), f); Ep[0, :] = 1.0
    Es = np.zeros((32, 128), f)
    for t in range(64):
        Es[1 + t // 4, t] = 1.0
    shared["Ep"] = Ep; shared["Es"] = Es
    SelP = np.zeros((32, 16), f)
    for sl in range(32):
        SelP[sl, sl % 16] = 1.0 / 256
    SelS = np.zeros((16, 8), f)
    for pg in range(16):
        SelS[pg, pg // 2] = 1.0 / 256
    shared["SelP"] = SelP; shared["SelS"] = SelS
    tq = np.arange(128)[:, None]; kk = np.arange(128)[None, :]
    shared["maskOwnS"] = np.where((kk // 4 == tq // 4) & (kk % 4 <= tq % 4) & (tq < 64) & (kk < 64), 0.0, MNEG).astype(f)
    rowmask = np.full((128, 16), MNEG, f)
    for t in range(64):
        rowmask[t, t // 4] = 0.0
    shared["rowmask"] = rowmask
    half = 8
    inv = (500000.0 ** (-np.arange(half, dtype=np.float64) / half)).astype(f)
    maps = []
    for core in range(8):
        b, p = core // 2, core % 2
        m = dict(shared)
        xt32 = x_prompt[b].reshape(32, 128, D)
        m["xp"] = c(np.concatenate([xt32[p::2], xt32[(1 - p)::2]], axis=0))
        xs = np.zeros((128, D), f); xs[:64] = x_sample[16 * core:16 * core + 16].reshape(64, D)
        m["xs"] = xs
        cT = np.zeros((D, 32), f); cT[:, 0] = inp["c_prompt"][b]; cT[:, 1:17] = inp["c_sample"][16 * core:16 * core + 16].T
        m["cT"] = cT
        m["ptab"] = np.ascontiguousarray(inp["page_table"][16 * core:16 * core + 16], dtype=np.int32).reshape(256)
        pos = np.zeros((NS, 128), f)
        for i in range(16):
            pos[i] = (2 * i + p) * 128 + np.arange(128)
            pos[16 + i] = (2 * i + 1 - p) * 128 + np.arange(128)
        pos[SMP, :64] = 2048 + (np.arange(64) % 4)
        ang = pos[:, :, None].astype(f) * inv[None, None, :]
        m["cosT"] = np.cos(ang).astype(f); m["sinT"] = np.sin(ang).astype(f)
        mo = np.zeros((128, 256), f)
        mo[:, 0:128] = np.where(kk <= tq, 0.0, MNEG)
        mo[:, 128:256] = 0.0 if p == 1 else MNEG
        m["maskOwn"] = mo
        maps.append(m)
    return maps


_CACHE = {}


def kernel(**inputs):
    inp = {k: np.asarray(v) for k, v in inputs.items()}
    maps = _host_inputs(inp)
    if "nc" not in _CACHE:
        _CACHE["nc"] = build_program()[0]
    nc = _CACHE["nc"]
    res = run_bass_kernel_spmd(nc, maps, core_ids=list(range(8)))
    R = res.results
    y_prompt = np.zeros((4, 4096, D), np.float32); y_sample = np.zeros((128, 4, D), np.float32)
    k_prompt = np.zeros((4, 32, 4, 128, 64), np.float32); v_prompt = np.zeros_like(k_prompt)
    k_sample = np.zeros((128, 4, 4, 64), np.float32); v_sample = np.zeros_like(k_sample)
    a_v = np.zeros((2, 128, 4, 2048), np.float32)
    for core in range(8):
        b, p = core // 2, core % 2
        r = R[core]
        y_prompt[b].reshape(32, 128, D)[p::2] = r["y_p"]
        y_sample[16 * core:16 * core + 16] = r["y_s"][:64].reshape(16, 4, D)
        k_prompt[b, p::2] = r["kp_o"]; v_prompt[b, p::2] = r["vp_o"]
        k_sample[16 * core:16 * core + 16] = r["ks_o"]; v_sample[16 * core:16 * core + 16] = r["vs_o"]
        a_v[:, 16 * core:16 * core + 16] = r["av_o"].reshape(2, 16, 4, 2048)
    return (y_prompt, y_sample, k_prompt, v_prompt, k_sample, v_sample, a_v)
```
